# Optimizing a Trainium2 kernel written in Bass

```python
import math
import jax, jax.numpy as jnp
from jax import lax
import numpy as np

D_MODEL = 1024
BATCH = 8
SEQ = 8192
DEPTH = 2

MEM_LEN = 256
BLK = 128
N_BRANCH = 4
BRANCH_W = D_MODEL // 2
HEAD_DIM = 64
SGU_GROUPS = 4
SGU_CHUNK = 128
SGU_GW = BRANCH_W // SGU_GROUPS
LRU_W = BRANCH_W
LRU_HEADS = 4
LRU_HW = LRU_W // LRU_HEADS
CONV_W = 4
LRU_C = 8.0
SWA_HEADS = BRANCH_W // HEAD_DIM
SWA_KV = SWA_HEADS // 4
SWA_WINDOW = 128
FOX_HEADS = BRANCH_W // HEAD_DIM
X_HEADS = 4
X_HEAD_DIM = D_MODEL // 8
D_FF = ((8 * D_MODEL // 3 + 127) // 128) * 128
N_EXPERTS = 8
TOP_K = 2
EPS = 1e-6
IN_SIZES = (BRANCH_W, BRANCH_W, LRU_W, LRU_W, SWA_HEADS * HEAD_DIM, SWA_KV * HEAD_DIM, SWA_KV * HEAD_DIM, FOX_HEADS * HEAD_DIM, FOX_HEADS * HEAD_DIM, FOX_HEADS * HEAD_DIM, FOX_HEADS)
D_IN = sum(IN_SIZES)
N_DENSE = (DEPTH + 1) // 2
N_MOE = DEPTH // 2

kernel_name = 'hybrid_gated_four_mixer_moe_block'


def rms_norm(x, g):
    x32 = x.astype(jnp.float32)
    y = x32 * lax.rsqrt(jnp.mean(x32 * x32, axis=-1, keepdims=True) + EPS)
    return (y * g.astype(jnp.float32)).astype(x.dtype)


def alibi_slopes(n):
    return 2.0 ** (-(8.0 / n) * jnp.arange(1, n + 1, dtype=jnp.float32))


def sgu_mixer(u, v, g_norm, w_s, b_s):
    bsz, seq, _ = v.shape
    v = rms_norm(v, g_norm)
    causal = jnp.tril(jnp.ones((SGU_CHUNK, SGU_CHUNK), dtype=bool))
    w = jnp.where(causal[None], w_s, 0).astype(v.dtype)
    vc = v.reshape(bsz, seq // SGU_CHUNK, SGU_CHUNK, SGU_GROUPS, SGU_GW)
    mixed = jnp.einsum('gts,bcsgd->bctgd', w, vc) + b_s.T[None, None, :, :, None]
    return u * mixed.reshape(bsz, seq, BRANCH_W)


def causal_depthwise_conv(x, w, b):
    ch = x.shape[-1]
    y = lax.conv_general_dilated(x, w[:, None, :].astype(x.dtype), window_strides=(1,), padding=[(CONV_W - 1, 0)], dimension_numbers=('NWC', 'WIO', 'NWC'), feature_group_count=ch)
    return y + b


def rg_lru(x, wa, ba, wx, bx, lam):
    bsz, seq, width = x.shape
    xh = x.reshape(bsz, seq, LRU_HEADS, LRU_HW)
    r = jax.nn.sigmoid(jnp.einsum('bshi,hio->bsho', xh, wa).reshape(bsz, seq, width) + ba)
    i = jax.nn.sigmoid(jnp.einsum('bshi,hio->bsho', xh, wx).reshape(bsz, seq, width) + bx)
    log_a = (-LRU_C * r.astype(jnp.float32)) * jax.nn.softplus(-lam.astype(jnp.float32))
    a = jnp.exp(log_a)
    mult = jnp.sqrt(-jnp.expm1(2.0 * log_a))
    inp = (x * i).astype(jnp.float32) * mult

    def combine(left, right):
        a1, b1 = left
        a2, b2 = right
        return a1 * a2, a2 * b1 + b2

    _, h = lax.associative_scan(combine, (a, inp), axis=1)
    return h.astype(x.dtype)


def swa_sink_attention(q, k, v, sinks):
    bsz, seq, nh, dh = q.shape
    grp = nh // SWA_KV
    nb = seq // BLK
    kp = jnp.pad(k, ((0, 0), (BLK, 0), (0, 0), (0, 0)))
    vp = jnp.pad(v, ((0, 0), (BLK, 0), (0, 0), (0, 0)))
    slopes = alibi_slopes(nh).reshape(SWA_KV, grp)
    sink = sinks.astype(jnp.float32).reshape(SWA_KV, grp)
    s_idx = jnp.arange(2 * BLK)[None, :]
    dist = (jnp.arange(BLK)[:, None] + BLK) - s_idx
    in_win = (dist >= 0) & (dist < SWA_WINDOW)
    alibi = slopes[:, :, None, None] * dist.astype(jnp.float32)
    scale = dh ** -0.5

    def block(i):
        qb = lax.dynamic_slice_in_dim(q, i * BLK, BLK, axis=1).reshape(bsz, BLK, SWA_KV, grp, dh)
        kb = lax.dynamic_slice_in_dim(kp, i * BLK, 2 * BLK, axis=1)
        vb = lax.dynamic_slice_in_dim(vp, i * BLK, 2 * BLK, axis=1)
        s = jnp.einsum('bqkgd,bskd->bkgqs', qb, kb).astype(jnp.float32) * scale - alibi
        valid = in_win & (s_idx + (i - 1) * BLK >= 0)
        s = jnp.where(valid, s, -jnp.inf)
        sink_col = jnp.broadcast_to(sink[None, :, :, None, None], s.shape[:-1] + (1,))
        p = jax.nn.softmax(jnp.concatenate([s, sink_col], axis=-1), axis=-1)[..., :-1]
        o = jnp.einsum('bkgqs,bskd->bqkgd', p.astype(vb.dtype), vb)
        return o.reshape(bsz, BLK, nh * dh)

    out = lax.map(block, jnp.arange(nb))
    return out.transpose(1, 0, 2, 3).reshape(bsz, seq, nh * dh)


def forgetting_attention(q, k, v, f_logit):
    bsz, seq, nh, dh = q.shape
    nb = seq // BLK
    cum = jnp.cumsum(jax.nn.log_sigmoid(f_logit.astype(jnp.float32)), axis=1).transpose(0, 2, 1)
    k_pos = jnp.arange(seq)
    scale = dh ** -0.5

    def block(i):
        qb = lax.dynamic_slice_in_dim(q, i * BLK, BLK, axis=1)
        cq = lax.dynamic_slice_in_dim(cum, i * BLK, BLK, axis=2)
        s = jnp.einsum('bqhd,bkhd->bhqk', qb, k).astype(jnp.float32) * scale + cq[..., None] - cum[:, :, None, :]
        q_pos = i * BLK + jnp.arange(BLK)
        s = jnp.where(k_pos[None, :] <= q_pos[:, None], s, -jnp.inf)
        p = jax.nn.softmax(s, axis=-1)
        o = jnp.einsum('bhqk,bkhd->bqhd', p.astype(v.dtype), v)
        return o.reshape(bsz, BLK, nh * dh)

    out = lax.map(block, jnp.arange(nb))
    return out.transpose(1, 0, 2, 3).reshape(bsz, seq, nh * dh)


def hybrid_mixer(xn, w_in, sgu_g, sgu_w, sgu_b, conv_w, conv_b, rg_wa, rg_ba, rg_wx, rg_bx, rg_lambda, swa_sinks, fox_bf, w_branch, w_gate, b_gate, w_out):
    bsz, seq, _ = xn.shape
    proj = xn @ w_in
    a_u, a_v, b_x, b_y, c_q, c_k, c_v, d_q, d_k, d_v, d_f = jnp.split(proj, np.cumsum(IN_SIZES)[:-1].tolist(), axis=-1)
    o_a = sgu_mixer(jax.nn.gelu(a_u), jax.nn.gelu(a_v), sgu_g, sgu_w, sgu_b)
    o_b = rg_lru(causal_depthwise_conv(b_x, conv_w, conv_b), rg_wa, rg_ba, rg_wx, rg_bx, rg_lambda) * jax.nn.gelu(b_y)
    o_c = swa_sink_attention(c_q.reshape(bsz, seq, SWA_HEADS, HEAD_DIM), c_k.reshape(bsz, seq, SWA_KV, HEAD_DIM), c_v.reshape(bsz, seq, SWA_KV, HEAD_DIM), swa_sinks)
    o_d = forgetting_attention(d_q.reshape(bsz, seq, FOX_HEADS, HEAD_DIM), d_k.reshape(bsz, seq, FOX_HEADS, HEAD_DIM), d_v.reshape(bsz, seq, FOX_HEADS, HEAD_DIM), d_f + fox_bf)
    merged = jnp.zeros_like(xn)
    for br, o in enumerate((o_a, o_b, o_c, o_d)):
        gate = jax.nn.sigmoid(xn @ w_gate[br] + b_gate[br])
        merged = merged + gate * (o @ w_branch[br])
    return merged @ w_out


def memory_cross_attention(hn, mem, g_mem, wq, wkv, wo):
    bsz, seq, _ = hn.shape
    m_len = mem.shape[1]
    q = (hn @ wq).reshape(bsz, seq, X_HEADS, X_HEAD_DIM)
    k, v = jnp.split(rms_norm(mem, g_mem) @ wkv, 2, axis=-1)
    k = k.reshape(bsz, m_len, X_HEADS, X_HEAD_DIM)
    v = v.reshape(bsz, m_len, X_HEADS, X_HEAD_DIM)
    s = jnp.einsum('bshd,bmhd->bhsm', q, k).astype(jnp.float32) * (X_HEAD_DIM ** -0.5)
    p = jax.nn.softmax(s, axis=-1)
    o = jnp.einsum('bhsm,bmhd->bshd', p.astype(v.dtype), v).reshape(bsz, seq, X_HEADS * X_HEAD_DIM)
    return o @ wo


def swiglu(x, w13, w2):
    g, u = jnp.split(x @ w13, 2, axis=-1)
    return (jax.nn.silu(g) * u) @ w2


def moe_ffn(x, w_r, b_r, w13, w2):
    logits = (x @ w_r).astype(jnp.float32) + b_r.astype(jnp.float32)
    top_v, top_i = lax.top_k(logits, TOP_K)
    top_w = jax.nn.softmax(top_v, axis=-1)
    combine = jnp.einsum('bsk,bske->bse', top_w, jax.nn.one_hot(top_i, N_EXPERTS, dtype=jnp.float32)).astype(x.dtype)
    out = jnp.zeros_like(x)
    for e in range(N_EXPERTS):
        out = out + combine[..., e:e + 1] * swiglu(x, w13[e], w2[e])
    return out


def setup_inputs(seed: int = 0) -> dict:
    key = jax.random.key(seed)
    ks = iter(jax.random.split(key, 48))
    f32 = jnp.float32
    L = DEPTH

    def nrm(shape, scale):
        return jax.random.normal(next(ks), shape, f32) * scale

    def gain(shape):
        return 1.0 + 0.05 * jax.random.normal(next(ks), shape, f32)

    x = nrm((BATCH, SEQ, D_MODEL), 1.0)
    mem = nrm((BATCH, MEM_LEN, D_MODEL), 1.0)
    norm_mix = gain((L, D_MODEL))
    w_in = nrm((L, D_MODEL, D_IN), D_MODEL ** -0.5)
    sgu_g = gain((L, BRANCH_W))
    sgu_w = nrm((L, SGU_GROUPS, SGU_CHUNK, SGU_CHUNK), SGU_CHUNK ** -0.5)
    sgu_b = 1.0 + nrm((L, SGU_GROUPS, SGU_CHUNK), 0.1)
    conv_w = nrm((L, CONV_W, LRU_W), CONV_W ** -0.5)
    conv_b = nrm((L, LRU_W), 0.01)
    rg_wa = nrm((L, LRU_HEADS, LRU_HW, LRU_HW), LRU_HW ** -0.5)
    rg_ba = nrm((L, LRU_W), 0.1)
    rg_wx = nrm((L, LRU_HEADS, LRU_HW, LRU_HW), LRU_HW ** -0.5)
    rg_bx = nrm((L, LRU_W), 0.1)
    a_c = jax.random.uniform(next(ks), (L, LRU_W), f32, 0.9, 0.999)
    p_a = a_c ** (1.0 / LRU_C)
    rg_lambda = jnp.log(p_a) - jnp.log1p(-p_a)
    swa_sinks = nrm((L, SWA_HEADS), 0.5)
    fox_bf = jax.random.uniform(next(ks), (L, FOX_HEADS), f32, 2.0, 4.0)
    w_branch = nrm((L, N_BRANCH, BRANCH_W, D_MODEL), BRANCH_W ** -0.5)
    w_gate = nrm((L, N_BRANCH, D_MODEL, D_MODEL), D_MODEL ** -0.5)
    b_gate = nrm((L, N_BRANCH, D_MODEL), 0.1)
    w_out = nrm((L, D_MODEL, D_MODEL), D_MODEL ** -0.5)
    norm_cross = gain((L, D_MODEL))
    norm_mem = gain((L, D_MODEL))
    wq_c = nrm((L, D_MODEL, X_HEADS * X_HEAD_DIM), D_MODEL ** -0.5)
    wkv_c = nrm((L, D_MODEL, 2 * X_HEADS * X_HEAD_DIM), D_MODEL ** -0.5)
    wo_c = nrm((L, X_HEADS * X_HEAD_DIM, D_MODEL), (X_HEADS * X_HEAD_DIM) ** -0.5)
    norm_ffn = gain((L, D_MODEL))
    dense_w13 = nrm((N_DENSE, D_MODEL, 2 * D_FF), D_MODEL ** -0.5)
    dense_w2 = nrm((N_DENSE, D_FF, D_MODEL), D_FF ** -0.5)
    router_w = nrm((N_MOE, D_MODEL, N_EXPERTS), D_MODEL ** -0.5)
    router_b = nrm((N_MOE, N_EXPERTS), 0.01)
    moe_w13 = nrm((N_MOE, N_EXPERTS, D_MODEL, 2 * D_FF), D_MODEL ** -0.5)
    moe_w2 = nrm((N_MOE, N_EXPERTS, D_FF, D_MODEL), D_FF ** -0.5)
    norm_final = gain((D_MODEL,))
    return dict(x=x, mem=mem, norm_mix=norm_mix, w_in=w_in, sgu_g=sgu_g, sgu_w=sgu_w, sgu_b=sgu_b, conv_w=conv_w, conv_b=conv_b, rg_wa=rg_wa, rg_ba=rg_ba, rg_wx=rg_wx, rg_bx=rg_bx, rg_lambda=rg_lambda, swa_sinks=swa_sinks, fox_bf=fox_bf, w_branch=w_branch, w_gate=w_gate, b_gate=b_gate, w_out=w_out, norm_cross=norm_cross, norm_mem=norm_mem, wq_c=wq_c, wkv_c=wkv_c, wo_c=wo_c, norm_ffn=norm_ffn, dense_w13=dense_w13, dense_w2=dense_w2, router_w=router_w, router_b=router_b, moe_w13=moe_w13, moe_w2=moe_w2, norm_final=norm_final)


def reference(x, mem, norm_mix, w_in, sgu_g, sgu_w, sgu_b, conv_w, conv_b, rg_wa, rg_ba, rg_wx, rg_bx, rg_lambda, swa_sinks, fox_bf, w_branch, w_gate, b_gate, w_out, norm_cross, norm_mem, wq_c, wkv_c, wo_c, norm_ffn, dense_w13, dense_w2, router_w, router_b, moe_w13, moe_w2, norm_final):
    h = x
    for l in range(DEPTH):
        h = h + hybrid_mixer(rms_norm(h, norm_mix[l]), w_in[l], sgu_g[l], sgu_w[l], sgu_b[l], conv_w[l], conv_b[l], rg_wa[l], rg_ba[l], rg_wx[l], rg_bx[l], rg_lambda[l], swa_sinks[l], fox_bf[l], w_branch[l], w_gate[l], b_gate[l], w_out[l])
        h = h + memory_cross_attention(rms_norm(h, norm_cross[l]), mem, norm_mem[l], wq_c[l], wkv_c[l], wo_c[l])
        hn = rms_norm(h, norm_ffn[l])
        if l % 2 == 0:
            h = h + swiglu(hn, dense_w13[l // 2], dense_w2[l // 2])
        else:
            h = h + moe_ffn(hn, router_w[l // 2], router_b[l // 2], moe_w13[l // 2], moe_w2[l // 2])
    return rms_norm(h, norm_final)
```

```python
import numpy as np
import concourse.bass as bass
import concourse.mybir as mybir
from concourse.bass_utils import run_bass_kernel_spmd

F32 = mybir.dt.float32
BF16 = mybir.dt.bfloat16
AF = mybir.ActivationFunctionType
ALU = mybir.AluOpType
AX = mybir.AxisListType

ENGS = ("pe", "act", "dve", "pool", "sp")
SAME_ENGINE_SYNC = True


class Prog:
    def __init__(self, nc):
        self.nc = nc
        self.ops = []
        self.stack = []
        self.uid = 0

    def sb(self, name, shape, dt=F32):
        self.uid += 1
        g = self.nc.sbuf_tensor(f"{name}_{self.uid}", list(shape), dt)
        t = g.__enter__()
        self.stack.append(g)
        return t

    def ps(self, name, shape, dt=F32):
        g = self.nc.psum_tensor(name, list(shape), dt)
        t = g.__enter__()
        self.stack.append(g)
        return t

    def mark(self):
        return len(self.stack)

    def release(self, mk):
        while len(self.stack) > mk:
            self.stack.pop().__exit__(None, None, None)

    @staticmethod
    def _tok(x):
        if isinstance(x, (str, tuple)):
            return x
        return x.name

    def op(self, eng, fn, reads, writes, dmakey=None):
        r = tuple(self._tok(x) for x in reads if x is not None and not isinstance(x, (int, float)))
        w = tuple(self._tok(x) for x in writes if x is not None)
        self.ops.append((eng, fn, r, w, dmakey))

    def barrier(self):
        for e in ENGS:
            self.ops.append((e, None, (), (), None))

    def mm(self, out, lhsT, rhs, start=True, stop=True, sgc=False):
        if sgc:
            self.op("pe", lambda e: e.matmul(out, lhsT, rhs, start=start, stop=stop, skip_group_check=True),
                    [lhsT, rhs], [out])
        else:
            self.op("pe", lambda e: e.matmul(out, lhsT, rhs, start=start, stop=stop),
                    [lhsT, rhs], [out])

    def tr(self, out, in_, ident):
        self.op("pe", lambda e: e.transpose(out, in_, ident), [in_, ident], [out])

    def act(self, out, in_, func, bias=None, scale=None, accum_out=None):
        kw = {}
        if bias is not None:
            kw["bias"] = bias
        if scale is not None:
            kw["scale"] = scale
        if accum_out is not None:
            kw["accum_out"] = accum_out
        self.op("act", lambda e: e.activation(out, in_, func, **kw),
                [in_, bias, scale], [out, accum_out])

    def tt(self, out, in0, in1, op, eng="dve"):
        self.op(eng, lambda e: e.tensor_tensor(out, in0, in1, op), [in0, in1], [out])

    def ts(self, out, in0, s1, s2=None, op0=ALU.mult, op1=None, eng="dve"):
        kw = {}
        if op1 is not None:
            kw["op1"] = op1
        self.op(eng, lambda e: e.tensor_scalar(out, in0, s1, s2, op0, **kw), [in0, s1, s2], [out])

    def stt(self, out, in0, scalar, in1, op0, op1, eng="dve"):
        eng = "dve"
        self.op(eng, lambda e: e.scalar_tensor_tensor(out, in0, scalar, in1, op0, op1), [in0, scalar, in1], [out])

    def copy(self, out, in_, eng="dve"):
        self.op(eng, lambda e: e.tensor_copy(out, in_), [in_], [out])

    def memset(self, out, val, eng="dve"):
        self.op(eng, lambda e: e.memset(out, val), [], [out])

    def scan(self, out, d0, d1, init, op0, op1):
        self.op("dve", lambda e: e.tensor_tensor_scan(out, d0, d1, init, op0, op1), [d0, d1, init], [out])

    def recip(self, out, in_):
        self.op("dve", lambda e: e.reciprocal(out, in_), [in_], [out])

    def rsum(self, out, in_):
        self.op("dve", lambda e: e.reduce_sum(out, in_, AX.X), [in_], [out])

    def rmax(self, out, in_):
        self.op("dve", lambda e: e.reduce_max(out, in_, AX.X), [in_], [out])

    def dma(self, out, in_, eng="sp", key=None, reads=None, writes=None):
        r = [in_] if reads is None else list(reads)
        w = [out] if writes is None else list(writes)
        if key is None:
            key = in_.name if "dram" in str(type(out.tensor)).lower() else out.name
            key = key.rsplit("_", 1)[0]
        self.op(eng, lambda e: e.dma_start(out=out, in_=in_), r, w, dmakey=("dma", key))

    def emit(self):
        nc = self.nc
        ops = self.ops
        n = len(ops)
        last_w, readers = {}, {}
        last_eng, last_dma = {}, {}
        deps = [None] * n
        i = 0
        while i < n:
            eng, fn, r, w, dk = ops[i]
            if fn is None:
                snap = set(last_eng.values()) | set(last_dma.values())
                for k in range(len(ENGS)):
                    deps[i + k] = set(snap)
                last_w, readers = {}, {}
                i += len(ENGS)
                continue
            d = set()
            for t in r:
                if t in last_w:
                    d.add(last_w[t])
            for t in w:
                if t in last_w:
                    d.add(last_w[t])
                for j in readers.get(t, ()):
                    d.add(j)
            d.discard(i)
            best = {}
            for j in d:
                sk = ops[j][4] if ops[j][4] is not None else ops[j][0]
                if best.get(sk, -1) < j:
                    best[sk] = j
            d = set(best.values())
            deps[i] = d
            for t in w:
                last_w[t] = i
                readers[t] = []
            for t in r:
                if t not in w:
                    readers.setdefault(t, []).append(i)
            if dk is not None:
                last_dma[dk] = i
            else:
                last_eng[eng] = i
            i += 1

        def needs_wait(i, j):
            ei, ej = ops[i][0], ops[j][0]
            if ops[j][4] is not None:
                return True
            if ei != ej:
                return True
            if ei == "pe":
                return False
            return SAME_ENGINE_SYNC

        waited = [False] * n
        for i in range(n):
            for j in deps[i]:
                if needs_wait(i, j):
                    waited[j] = True
        sems, counts = {}, {}
        semval = [None] * n

        def get_sem(k):
            if k not in sems:
                g = nc.semaphore("s_" + "_".join(str(x) for x in k))
                sems[k] = g.__enter__()
                self.stack.append(g)
                counts[k] = 0
            return sems[k]

        for i, (eng, fn, r, w, dk) in enumerate(ops):
            if fn is None:
                continue
            if dk is not None:
                get_sem(dk)
                counts[dk] += 16
                semval[i] = (dk, counts[dk])
            elif waited[i]:
                k = ("eng", eng)
                get_sem(k)
                counts[k] += 1
                semval[i] = (k, counts[k])
        streams = {e: [] for e in ENGS}
        seen = {e: {} for e in ENGS}
        for i, (eng, fn, r, w, dk) in enumerate(ops):
            need = {}
            for j in deps[i]:
                if not needs_wait(i, j):
                    continue
                k, v = semval[j]
                if need.get(k, 0) < v:
                    need[k] = v
            waits = []
            for k, v in need.items():
                if seen[eng].get(k, 0) < v:
                    seen[eng][k] = v
                    waits.append((k, v))
            streams[eng].append((i, waits))
        self.counts = dict(counts)
        engobj = {"pe": "tensor", "act": "scalar", "dve": "vector", "pool": "gpsimd", "sp": "sync"}
        tail = [(k, v) for k, v in counts.items()]
        with nc.Block() as block:
            for ename in ENGS:
                def body(e, ename=ename):
                    for i, waits in streams[ename]:
                        for k, v in waits:
                            e.wait_ge(sems[k], v)
                        if ops[i][1] is None:
                            continue
                        ins = ops[i][1](e)
                        if semval[i] is not None:
                            k, v = semval[i]
                            ins.then_inc(sems[k], 16 if k[0] == "dma" else 1)
                    if ename == "sp":
                        for k, v in tail:
                            e.wait_ge(sems[k], v)
                getattr(block, engobj[ename])(body)

    def close(self):
        self.release(0)


S, D, T, NG, L = 8192, 1024, 512, 16, 2
DFF = 2816
NFC = DFF // 128
EPS = 1e-6
O_AU, O_AV, O_BX, O_BY, O_CQ, O_CK, O_CV, O_DQ, O_DK, O_DV, O_DF = 0, 512, 1024, 1536, 2048, 2560, 2688, 2816, 3328, 3840, 4352


def build(nlayers=2, nphase=99, dbg=False, ng=NG):
    nc = bass.Bass("TRN2", target_bir_lowering=False)
    P = Prog(nc)

    def din(name, shape):
        return nc.dram_tensor(name, list(shape), F32, kind="ExternalInput").ap()

    def dscr(name, shape, dt):
        kind = "ExternalOutput" if dbg else "Internal"
        return nc.dram_tensor(name, list(shape), dt, kind=kind).ap()

    x = din("x", [S, D]); mem = din("mem", [256, D])
    w_in = din("w_in", [L, D, 4360]); sgu_w = din("sgu_w", [L, 4, 128, 128])
    rg_wa = din("rg_wa", [L, 4, 128, 128]); rg_wx = din("rg_wx", [L, 4, 128, 128])
    w_branch = din("w_branch", [L, 4, 512, D]); w_gate = din("w_gate", [L, 4, D, D]); w_out = din("w_out", [L, D, D])
    wq_c = din("wq_c", [L, D, 512]); wkv_c = din("wkv_c", [L, D, 1024]); wo_c = din("wo_c", [L, 512, D])
    dense_w13 = din("dense_w13", [1, D, 2 * DFF]); dense_w2 = din("dense_w2", [1, DFF, D])
    router_w = din("router_w", [1, D, 8])
    moe_w13 = din("moe_w13", [1, 8, D, 2 * DFF]); moe_w2 = din("moe_w2", [1, 8, DFF, D])
    g_mix = din("g_mix", [128, L, D]); g_cross = din("g_cross", [128, L, D]); g_ffn = din("g_ffn", [128, L, D])
    g_mem = din("g_mem", [128, L, D]); g_final = din("g_final", [128, D])
    sgu_g_b = din("sgu_g_b", [128, L, 512]); sgu_b_b = din("sgu_b_b", [128, L, 512])
    sinks_b = din("sinks_b", [128, L, 8]); bf_b = din("bf_b", [128, L, 8]); rb_b = din("rb_b", [128, 8])
    conv_w_p = din("conv_w_p", [128, L, 4, 4]); conv_b_p = din("conv_b_p", [128, L, 4])
    ba_p = din("ba_p", [128, L, 4]); bx_p = din("bx_p", [128, L, 4]); lam_p = din("lam_p", [128, L, 4])
    bgate_p = din("bgate_p", [128, L, 4, 8])
    pidx_d = din("pidx", [128, 1])
    ident_d = din("ident", [128, 128]); tri_d = din("tri", [128, 128]); mexp_d = din("mexp", [128, 8, 2, 128])
    y = nc.dram_tensor("y", [S, D], F32, kind="ExternalOutput").ap()

    XNT = dscr("XNT", [128, 8, S], BF16)
    QT = dscr("QT", [128, 8, S], BF16)
    KT = dscr("KT", [128, 5, S], BF16)
    VV = dscr("VV", [128, 64, 640], BF16)
    OA = dscr("OA", [128, 4, S], BF16)
    OB = dscr("OB", [128, 4, S], BF16)
    OC = dscr("OC", [128, 64, 512], BF16)
    OD = dscr("OD", [128, 64, 512], BF16)
    H1 = dscr("H1", [S, D], F32)
    H3 = dscr("H3", [S, D], F32)
    I32 = mybir.dt.int32
    NBLK = 40
    WSL = {}
    for sbi in range(6):
        for t_ in range(2):
            WSL[(sbi, t_)] = nc.dram_tensor(f"WSL{sbi}_{t_}", [1024, 8, 512 if sbi < 5 else 256], BF16, kind="Internal").ap()
    W2H = [nc.dram_tensor(f"W2H{hf}", [1024, NFC, 512], BF16, kind="Internal").ap() for hf in range(2)]
    XS = nc.dram_tensor("XS", [NBLK * 512, D], BF16, kind="Internal").ap()
    YS = nc.dram_tensor("YS", [NBLK * 512, D], F32, kind="Internal").ap()
    XN2 = nc.dram_tensor("XN2", [S, D], BF16, kind="Internal").ap()
    W13B = [nc.dram_tensor(f"W13B{e}", [D, 2 * DFF], BF16, kind="Internal").ap() for e in range(1)]
    W2B = [nc.dram_tensor(f"W2B{e}", [DFF, D], BF16, kind="Internal").ap() for e in range(1)]

    ps = [P.ps(f"ps{i}", [128, 512]) for i in range(6)]
    pbs = [P.ps(f"pb{i}", [128, 1024], BF16) for i in range(2)]
    ring = {"i": 0, "b": 0}

    def nps(n=6):
        ring["i"] = (ring["i"] + 1) % n
        return ps[ring["i"]]

    def npb():
        ring["b"] = (ring["b"] + 1) % 2
        return pbs[ring["b"]]

    ident = P.sb("ident", [128, 128], BF16)
    tri_f = P.sb("tri_f", [128, 128]); tri_b = P.sb("tri_b", [128, 128], BF16)
    ones_f = P.sb("ones_f", [128, 128]); ones_b = P.sb("ones_b", [128, 128], BF16)
    CK = P.sb("CK", [128, 64, 8]); CREF = P.sb("CREF", [128, 16, 8])
    epsc = P.sb("epsc", [128, 1])
    P.dma(ident[:], ident_d, eng="pool")
    P.dma(tri_f[:], tri_d)
    P.dma(tri_b[:], tri_d, eng="pool")
    P.memset(ones_f[:], 1.0); P.memset(ones_b[:], 1.0); P.memset(epsc[:], EPS)

    def prep_ffn(e_idx, w13src, w2src):
        for kc in range(8):
            P.dma(W13B[e_idx][kc * 128:(kc + 1) * 128, :], w13src[kc * 128:(kc + 1) * 128, :], eng="pool",
                  key=f"prep{kc % 4}", writes=[f"W13B{e_idx}"])
        for fc in range(0, NFC, 2):
            P.dma(W2B[e_idx][fc * 128:(fc + 2) * 128, :], w2src[fc * 128:(fc + 2) * 128, :], eng="pool",
                  key=f"prep{(fc // 2) % 4}", writes=[f"W2B{e_idx}"])

    def prep_moe(e):
        w13v = moe_w13[0, e].rearrange("(k p) n -> p k n", p=128)
        w2v = moe_w2[0, e].rearrange("(k p) n -> p k n", p=128)
        i = 0
        for sbi in range(6):
            ncol = 512 if sbi < 5 else 256
            for t_ in range(2):
                c0 = t_ * DFF + sbi * 512
                P.dma(WSL[(sbi, t_)][e * 128:(e + 1) * 128, :, :], w13v[:, :, c0:c0 + ncol], eng="pool",
                      key=f"prep{i % 4}", writes=["WP"])
                i += 1
        for hf in range(2):
            for f0 in range(0, NFC, 11):
                P.dma(W2H[hf][e * 128:(e + 1) * 128, f0:f0 + 11, :], w2v[:, f0:f0 + 11, hf * 512:(hf + 1) * 512], eng="pool",
                      key=f"prep{i % 4}", writes=["WP"])
                i += 1

    def rms_p1(hT, gain, xn_tok, junk, ss, rstd):
        for tt in range(4):
            P.act(junk[:], hT[:, tt, :], AF.Square, accum_out=ss[:, tt:tt + 1])
        P.act(rstd[:], ss[:], AF.Sqrt, bias=epsc[:], scale=1.0 / D)
        P.recip(rstd[:], rstd[:])
        for tt in range(4):
            P.stt(xn_tok[:, tt, :], hT[:, tt, :], rstd[:, tt:tt + 1], gain, ALU.mult, ALU.mult,
                  eng="dve" if tt % 2 == 0 else "pool")

    def rms_p2(xn_tok, xT):
        for tt in range(4):
            pb = npb()
            for kc in range(8):
                P.tr(pb[:, kc * 128:(kc + 1) * 128], xn_tok[:, tt, kc * 128:(kc + 1) * 128], ident[:])
            src = pb[:, :].rearrange("p (a b) -> p a b", a=8)
            dst = xT[:, :, tt * 128:(tt + 1) * 128]
            if tt % 2 == 0:
                P.copy(dst, src)
            else:
                P.act(dst, src, AF.Copy)

    def rms_to_T(hT, gain, xn_tok, xT, junk, ss, rstd):
        rms_p1(hT, gain, xn_tok, junk, ss, rstd)
        rms_p2(xn_tok, xT)

    def load_h(hT, hsrc, hname, g):
        P.dma(hT[:], hsrc[g * T:(g + 1) * T, :].rearrange("(t p) d -> p t d", p=128), reads=[(hname, g)])

    def phase_A(l, hsrc, hname):
        mk = P.mark()
        Win = P.sb("Win", [128, 8, 4360], BF16)
        for kc in range(8):
            P.dma(Win[:, kc, :], w_in[l, kc * 128:(kc + 1) * 128, :], eng="pool")
        gain = P.sb("gainA", [128, D]); P.dma(gain[:], g_mix[:, l, :])
        sgug = P.sb("sgug", [128, 512]); P.dma(sgug[:], sgu_g_b[:, l, :])
        sgub = P.sb("sgub", [128, 512]); P.dma(sgub[:], sgu_b_b[:, l, :])
        bfb = P.sb("bfb", [128, 8]); P.dma(bfb[:], bf_b[:, l, :])
        cw = P.sb("cw", [128, 4, 4]); P.dma(cw[:], conv_w_p[:, l])
        cb = P.sb("cb", [128, 4]); P.dma(cb[:], conv_b_p[:, l, :])
        bap = P.sb("bap", [128, 4]); P.dma(bap[:], ba_p[:, l, :])
        bxp = P.sb("bxp", [128, 4]); P.dma(bxp[:], bx_p[:, l, :])
        lam = P.sb("lam", [128, 4]); P.dma(lam[:], lam_p[:, l, :])
        cc = P.sb("cc", [128, 4])
        P.act(cc[:], lam[:], AF.Exp, scale=-1.0)
        P.act(cc[:], cc[:], AF.Ln, bias=1.0)
        P.ts(cc[:], cc[:], -8.0, None, op0=ALU.mult)
        Wa = P.sb("Wa", [128, 4, 128], BF16); Wx = P.sb("Wx", [128, 4, 128], BF16)
        P.dma(Wa[:], rg_wa[l].rearrange("h i o -> i h o"), eng="pool")
        P.dma(Wx[:], rg_wx[l].rearrange("h i o -> i h o"), eng="pool")
        wsb = P.sb("wsb", [128, 4, 128], BF16); WsT = P.sb("WsT", [128, 4, 128], BF16)
        P.dma(wsb[:], sgu_w[l].rearrange("g t s -> t g s"), eng="pool")
        pb = npb()
        for gc in range(4):
            P.tr(pb[:, gc * 128:(gc + 1) * 128], wsb[:, gc, :], ident[:])
        for gc in range(4):
            P.tt(WsT[:, gc, :], pb[:, gc * 128:(gc + 1) * 128], tri_b[:], ALU.mult)

        hT = P.sb("hT", [128, 4, D]); xn_tok = P.sb("xn_tok", [128, 4, D], BF16); xnT = P.sb("xnT", [128, 8, T], BF16)
        junk = P.sb("junk", [128, D]); ss = P.sb("ss", [128, 4]); rstd = P.sb("rstd", [128, 4])
        uT = P.sb("uT", [128, 4, T], BF16); vg = P.sb("vg", [128, 512]); v_tok = P.sb("v_tok", [128, 4, 512], BF16)
        ssv = P.sb("ssv", [128, 1]); rsv = P.sb("rsv", [128, 1])
        bx = P.sb("bx", [128, 4, T + 3]); gy = P.sb("gy", [128, 4, T])
        QTst = P.sb("QTst", [128, 8, T], BF16); KTst = P.sb("KTst", [128, 5, T], BF16); Vst = P.sb("Vst", [128, 4, 640], BF16)
        oaT = P.sb("oaT", [128, 4, T], BF16); obT = P.sb("obT", [128, 4, T], BF16)
        tmpa = P.sb("tmpa", [128, 4, 128])
        xf = P.sb("xf", [128, 8]); ls = P.sb("ls", [128, 8]); carry = P.sb("carry", [128, 8]); hcar = P.sb("hcar", [128, 4])
        xc4 = P.sb("xc4", [128, 4, T]); xcb4 = P.sb("xcb4", [128, 4, T], BF16); rr = P.sb("rr", [128, T]); ii = P.sb("ii", [128, T])
        aa = P.sb("aa", [128, T]); a2 = P.sb("a2", [128, T]); inp = P.sb("inp", [128, T]); hh = P.sb("hh", [128, T])
        P.memset(bx[:], 0.0); P.memset(carry[:], 0.0); P.memset(hcar[:], 0.0)

        xnTs = [xnT, P.sb("xnT2", [128, 8, T], BF16)]
        load_h(hT, hsrc, hname, 0)
        rms_p1(hT, gain[:], xn_tok, junk, ss, rstd)
        if ng > 1:
            load_h(hT, hsrc, hname, 1)
        rms_p2(xn_tok, xnTs[0])
        for g in range(ng):
            xnT = xnTs[g % 2]
            P.dma(XNT[:, :, g * T:(g + 1) * T], xnT[:], writes=[("XNT", g)])
            if g + 1 < ng:
                rms_p1(hT, gain[:], xn_tok, junk, ss, rstd)
                if g + 2 < ng:
                    load_h(hT, hsrc, hname, g + 2)

            def proj(col0, nch, epi):
                for c in range(nch):
                    p_ = nps()
                    for kc in range(8):
                        P.mm(p_[:], Win[:, kc, col0 + c * 128:col0 + (c + 1) * 128], xnT[:, kc, :], start=kc == 0, stop=kc == 7)
                    epi(c, p_)
            proj(O_AU, 4, lambda c, p_: P.act(uT[:, c, :], p_[:], AF.Gelu_apprx_tanh))
            proj(O_BX, 4, lambda c, p_: P.copy(bx[:, c, 3:T + 3], p_[:]))
            proj(O_BY, 4, lambda c, p_: P.act(gy[:, c, :], p_[:], AF.Gelu_apprx_tanh))
            for c in range(4):
                P.act(xc4[:, c, :], bx[:, c, 3:T + 3], AF.Identity, bias=cb[:, c:c + 1], scale=cw[:, 3, c:c + 1])
                for k in range(3):
                    P.stt(xc4[:, c, :], bx[:, c, k:k + T], cw[:, k, c:c + 1], xc4[:, c, :], ALU.mult, ALU.add)
                P.copy(xcb4[:, c, :], xc4[:, c, :], eng="pool")
                P.copy(bx[:, c, 0:3], bx[:, c, T:T + 3], eng="pool")
            proj(O_CQ, 4, lambda c, p_: P.copy(QTst[:, c, :], p_[:]))
            proj(O_DQ, 4, lambda c, p_: P.act(QTst[:, 4 + c, :], p_[:], AF.Copy))
            proj(O_CK, 1, lambda c, p_: P.copy(KTst[:, 0, :], p_[:]))
            proj(O_DK, 4, lambda c, p_: P.act(KTst[:, 1 + c, :], p_[:], AF.Copy))
            if g + 1 < ng:
                rms_p2(xn_tok, xnTs[(g + 1) % 2])
            for tt in range(4):
                tok = slice(tt * 128, (tt + 1) * 128)
                p_ = nps()
                for kc in range(8):
                    P.mm(p_[:], xnT[:, kc, tok], Win[:, kc, O_AV:O_AV + 512], start=kc == 0, stop=kc == 7)
                P.act(vg[:], p_[:], AF.Gelu_apprx_tanh)
                P.act(junk[:, 0:512], vg[:], AF.Square, accum_out=ssv[:])
                P.act(rsv[:], ssv[:], AF.Sqrt, bias=epsc[:], scale=1.0 / 512)
                P.recip(rsv[:], rsv[:])
                P.stt(v_tok[:, tt, :], vg[:], rsv[:, 0:1], sgug[:], ALU.mult, ALU.mult)
                p1 = nps()
                for kc in range(8):
                    P.mm(p1[:], xnT[:, kc, tok], Win[:, kc, O_DV:O_DV + 512], start=kc == 0, stop=kc == 7)
                P.copy(Vst[:, tt, 128:640], p1[:])
                p2 = nps()
                for kc in range(8):
                    P.mm(p2[:, 0:128], xnT[:, kc, tok], Win[:, kc, O_CV:O_CV + 128], start=kc == 0, stop=kc == 7)
                for kc in range(8):
                    P.mm(p2[:, 128:136], xnT[:, kc, tok], Win[:, kc, O_DF:O_DF + 8], start=kc == 0, stop=kc == 7)
                P.act(Vst[:, tt, 0:128], p2[:, 0:128], AF.Copy)
                P.tt(xf[:], p2[:, 128:136], bfb[:], ALU.add)
                P.act(xf[:], xf[:], AF.Exp, scale=-1.0)
                P.act(xf[:], xf[:], AF.Ln, bias=1.0)
                P.ts(ls[:], xf[:], -1.0, None, op0=ALU.mult)
                p3 = nps()
                P.mm(p3[:, 0:8], tri_f[:], ls[:])
                P.mm(p3[:, 8:16], ones_f[:], ls[:])
                kt = 4 * g + tt
                P.tt(CK[:, kt, :], p3[:, 0:8], carry[:], ALU.add)
                P.tt(carry[:], p3[:, 8:16], carry[:], ALU.add)
                if tt == 1:
                    P.copy(CREF[:, g, :], carry[:])
                p4 = nps()
                for gc in range(4):
                    P.mm(p4[:, gc * 128:(gc + 1) * 128], v_tok[:, tt, gc * 128:(gc + 1) * 128], WsT[:, gc, :])
                P.tt(tmpa[:], p4[:, :].rearrange("p (a b) -> p a b", a=4), sgub[:, :].rearrange("p (a b) -> p a b", a=4), ALU.add)
                P.tt(oaT[:, :, tok], tmpa[:], uT[:, :, tok], ALU.mult, eng="pool")
                c = tt
                pr = nps(); P.mm(pr[:], Wa[:, c, :], xcb4[:, c, :])
                pi = nps(); P.mm(pi[:], Wx[:, c, :], xcb4[:, c, :])
                P.act(rr[:], pr[:], AF.Sigmoid, bias=bap[:, c:c + 1])
                P.act(ii[:], pi[:], AF.Sigmoid, bias=bxp[:, c:c + 1])
                P.act(aa[:], rr[:], AF.Exp, scale=cc[:, c:c + 1])
                P.tt(a2[:], aa[:], aa[:], ALU.mult, eng="pool")
                P.act(a2[:], a2[:], AF.Sqrt, bias=1.0, scale=-1.0)
                P.tt(inp[:], xc4[:, c, :], ii[:], ALU.mult)
                P.tt(inp[:], inp[:], a2[:], ALU.mult, eng="pool")
                P.scan(hh[:], aa[:], inp[:], hcar[:, c:c + 1], ALU.mult, ALU.add)
                P.copy(hcar[:, c:c + 1], hh[:, T - 1:T])
                P.tt(obT[:, c, :], hh[:], gy[:, c, :], ALU.mult, eng="pool")
            gs = slice(g * T, (g + 1) * T)
            P.dma(OA[:, :, gs], oaT[:], writes=[("OA", g)])
            P.dma(OB[:, :, gs], obT[:], writes=[("OB", g)])
            P.dma(QT[:, :, gs], QTst[:], writes=[("QT", g)])
            P.dma(KT[:, :, gs], KTst[:], writes=[("KT", g)])
            P.dma(VV[:, 4 * g:4 * g + 4, :], Vst[:], writes=[("VV", g)])
        P.barrier()
        P.release(mk)

    def phase_B(l):
        mk = P.mark()
        if l == 0 and nlayers > 1:
            for e in range(8):
                prep_moe(e)
        mexp = P.sb("mexp", [128, 8, 2, 128]); P.dma(mexp[:], mexp_d)
        snk = P.sb("snk", [128, 8]); P.dma(snk[:], sinks_b[:, l, :])
        P.act(snk[:], snk[:], AF.Exp)
        qh = [P.sb(f"qh{i}", [64, S], BF16) for i in range(2)]
        kh = [P.sb(f"kh{i}", [64, S], BF16) for i in range(2)]
        vh = [P.sb(f"vh{i}", [128, 64, 65], BF16) for i in range(2)]
        for i in range(2):
            P.memset(vh[i][:, :, 64:65], 1.0)
        biasgs = [P.sb(f"biasg{i}", [128, 64]) for i in range(2)]
        pts = [P.sb(f"pt{i}", [128, 512], BF16) for i in range(6)]
        pfs = [P.sb(f"pf{i}", [128, 512]) for i in range(4)]
        dens = [P.sb(f"den{i}", [128, 4]) for i in range(2)]; ods = [P.sb(f"od{i}", [128, 4, 64], BF16) for i in range(2)]
        cnt = 0
        allg = list(range(ng))
        def load_head(hd):
            fox = hd >= 8
            h = hd % 8
            b = hd % 2
            q_, k_, v_ = qh[b], kh[b], vh[b]
            Sg = ng * T
            if fox:
                P.dma(q_[:, 0:Sg], QT[(h % 2) * 64:(h % 2) * 64 + 64, 4 + h // 2, 0:Sg], reads=[("QT", g) for g in allg])
                P.dma(k_[:, 0:Sg], KT[(h % 2) * 64:(h % 2) * 64 + 64, 1 + h // 2, 0:Sg], reads=[("KT", g) for g in allg])
                voff = 128 + h * 64
            else:
                kv = h // 4
                P.dma(q_[:, 0:Sg], QT[(h % 2) * 64:(h % 2) * 64 + 64, h // 2, 0:Sg], reads=[("QT", g) for g in allg])
                P.dma(k_[:, 0:Sg], KT[kv * 64:kv * 64 + 64, 0, 0:Sg], reads=[("KT", g) for g in allg])
                voff = kv * 64
            for k0 in range(0, 4 * ng, 16):
                k1 = min(4 * ng, k0 + 16)
                P.dma(v_[:, k0:k1, 0:64], VV[:, k0:k1, voff:voff + 64], reads=[("VV", g) for g in allg])

        load_head(0)
        for hd in range(16):
            fox = hd >= 8
            h = hd % 8
            b = hd % 2
            q_, k_, v_ = qh[b], kh[b], vh[b]
            if hd + 1 < 16:
                load_head(hd + 1)
            items = []
            for g in range(ng):
                kts = list(range(0, 4 * g + 4)) if fox else list(range(max(0, 4 * g - 1), 4 * g + 4))
                for kt in kts:
                    items.append((g, kt, kts))
            state = {}

            def stage1(it):
                g, kt, kts = it
                if kt == kts[0] and fox:
                    bg_ = biasgs[g % 2]
                    P.ts(bg_[:, 0:4 * g + 4], CK[:, 0:4 * g + 4, h], -1.0, CREF[:, g, h:h + 1], op0=ALU.mult, op1=ALU.add)
                j = kt - 4 * g
                lo = max(0, j)
                hi = 3 if fox else min(3, j + 1)
                cs = slice(lo * 128, (hi + 1) * 128)
                p_ = nps(4)
                P.mm(p_[:, cs], k_[:, kt * 128:(kt + 1) * 128], q_[:, g * T + lo * 128:g * T + (hi + 1) * 128])
                state["cnt"] = state.get("cnt", 0) + 1
                pt = pts[state["cnt"] % 6]
                if fox:
                    P.act(pt[:, cs], p_[:, cs], AF.Exp, bias=biasgs[g % 2][:, kt:kt + 1], scale=0.125)
                    if j >= 0:
                        P.tt(pt[:, j * 128:(j + 1) * 128], pt[:, j * 128:(j + 1) * 128], tri_b[:], ALU.mult)
                else:
                    pf = pfs[state["cnt"] % 4]
                    P.act(pf[:, cs], p_[:, cs], AF.Exp, scale=0.125)
                    for qs in range(lo, hi + 1):
                        idx = 0 if kt == 4 * g + qs else 1
                        P.tt(pt[:, qs * 128:(qs + 1) * 128], pf[:, qs * 128:(qs + 1) * 128], mexp[:, h, idx, :], ALU.mult)
                return (pt, lo, hi)

            def stage2(it, s1):
                g, kt, kts = it
                pt, lo, hi = s1
                po = ps[4 + (g % 2)]
                pov = po[:, 0:260].rearrange("p (a b) -> p a b", a=4)
                for qs in range(lo, hi + 1):
                    last = 4 * g + qs
                    P.mm(pov[:, qs, :], pt[:, qs * 128:(qs + 1) * 128], v_[:, kt, :],
                         start=(kt == kts[0] and qs == lo), stop=kt == last, sgc=True)
                if kt == kts[-1]:
                    od = ods[g % 2]
                    dn = dens[g % 2]
                    if fox:
                        P.copy(dn[:], pov[:, :, 64])
                    else:
                        P.ts(dn[:], pov[:, :, 64], snk[:, h:h + 1], None, op0=ALU.add)
                    P.recip(dn[:], dn[:])
                    for qs in range(4):
                        P.ts(od[:, qs, :], pov[:, qs, 0:64], dn[:, qs:qs + 1], None, op0=ALU.mult)
                    dst = OD if fox else OC
                    P.dma(dst[:, 4 * g:4 * g + 4, h * 64:(h + 1) * 64], od[:], writes=[("OD" if fox else "OC", g, h)])

            LOOK = 3
            s1res = {}
            n_it = len(items)
            for i in range(n_it + LOOK):
                if i < n_it:
                    s1res[i] = stage1(items[i])
                if i - LOOK >= 0:
                    stage2(items[i - LOOK], s1res.pop(i - LOOK))
        P.barrier()
        P.release(mk)

    def phase_C(l, hsrc, hname):
        mk = P.mark()
        Wg = P.sb("Wg", [128, 8, 4096], BF16)
        for br in range(4):
            for kc in range(0, 8, 4):
                P.dma(Wg[:, kc:kc + 4, br * 1024:(br + 1) * 1024],
                      w_gate[l, br, kc * 128:(kc + 4) * 128, :].rearrange("(k p) n -> p k n", p=128), eng="pool")
        Wb = P.sb("Wb", [128, 4, 4, D], BF16)
        for br in range(4):
            P.dma(Wb[:, br], w_branch[l, br].rearrange("(k p) n -> p k n", p=128), eng="pool")
        Wo = P.sb("Wo", [128, 8, D], BF16)
        for kc in range(0, 8, 4):
            P.dma(Wo[:, kc:kc + 4, :], w_out[l, kc * 128:(kc + 4) * 128, :].rearrange("(k p) n -> p k n", p=128), eng="pool")
        bg = P.sb("bg", [128, 4, 8]); P.dma(bg[:], bgate_p[:, l])
        xnT = P.sb("xnT", [128, 8, T], BF16)
        brT = [P.sb(f"brT{i}", [128, 4, T], BF16) for i in range(2)]
        brX = [[P.sb(f"brX{j}_{i}", [128, 4, T], BF16) for i in range(2)] for j in range(2)]
        otokX = [[P.sb(f"otok{j}_{i}", [128, 4, 512], BF16) for i in range(2)] for j in range(2)]

        def prep_cd(g):
            par = g % 2
            P.dma(otokX[par][0][:], OC[:, 4 * g:4 * g + 4, :], reads=[("OC", g, h) for h in range(8)])
            P.dma(otokX[par][1][:], OD[:, 4 * g:4 * g + 4, :], reads=[("OD", g, h) for h in range(8)])
            for i in range(2):
                for tt in range(4):
                    pb = npb()
                    for c in range(4):
                        P.tr(pb[:, c * 128:(c + 1) * 128], otokX[par][i][:, tt, c * 128:(c + 1) * 128], ident[:])
                    P.copy(brX[par][i][:, :, tt * 128:(tt + 1) * 128], pb[:, 0:512].rearrange("p (a b) -> p a b", a=4))
        mT = P.sb("mT", [128, 8, T], BF16)
        gate = [P.sb(f"gate{i}", [128, T]) for i in range(2)]
        macc = P.sb("macc", [128, T]); mtmp = [P.sb(f"mtmp{i}", [128, T]) for i in range(2)]
        hT = P.sb("hT", [128, 4, D])
        for g in range(ng):
            gs = slice(g * T, (g + 1) * T)
            P.dma(xnT[:], XNT[:, :, gs], reads=[("XNT", g)])
            P.dma(brT[0][:], OA[:, :, gs], reads=[("OA", g)])
            P.dma(brT[1][:], OB[:, :, gs], reads=[("OB", g)])
            load_h(hT, hsrc, hname, g)
            if g == 0:
                prep_cd(0)
            brs = [brT[0], brT[1], brX[g % 2][0], brX[g % 2][1]]
            for oc in range(8):
                if oc == 4 and g + 1 < ng:
                    prep_cd(g + 1)
                for br in range(4):
                    pg = nps()
                    for kc in range(8):
                        P.mm(pg[:], Wg[:, kc, br * 1024 + oc * 128:br * 1024 + (oc + 1) * 128], xnT[:, kc, :], start=kc == 0, stop=kc == 7)
                    pr = nps()
                    for kc in range(4):
                        P.mm(pr[:], Wb[:, br, kc, oc * 128:(oc + 1) * 128], brs[br][:, kc, :], start=kc == 0, stop=kc == 3)
                    gt = gate[br % 2]
                    P.act(gt[:], pg[:], AF.Sigmoid, bias=bg[:, br, oc:oc + 1])
                    if br == 0:
                        P.tt(macc[:], gt[:], pr[:], ALU.mult)
                    else:
                        mt_ = mtmp[br % 2]
                        P.tt(mt_[:], gt[:], pr[:], ALU.mult)
                        if br < 3:
                            P.tt(macc[:], macc[:], mt_[:], ALU.add, eng="pool")
                        else:
                            P.tt(mT[:, oc, :], macc[:], mt_[:], ALU.add, eng="pool")
            for tt in range(4):
                for half in range(2):
                    p_ = nps()
                    for kc in range(8):
                        P.mm(p_[:], mT[:, kc, tt * 128:(tt + 1) * 128], Wo[:, kc, half * 512:(half + 1) * 512], start=kc == 0, stop=kc == 7)
                    P.tt(hT[:, tt, half * 512:(half + 1) * 512], hT[:, tt, half * 512:(half + 1) * 512], p_[:], ALU.add)
            P.dma(H1[gs, :].rearrange("(t p) d -> p t d", p=128), hT[:], writes=[("H1", g)])
        P.barrier()
        P.release(mk)

    def phase_D(l, moe, last):
        mk = P.mark()
        gx = P.sb("gx", [128, D]); P.dma(gx[:], g_cross[:, l, :])
        gf = P.sb("gf", [128, D]); P.dma(gf[:], g_ffn[:, l, :])
        Wq = P.sb("Wq", [128, 8, 512], BF16)
        P.dma(Wq[:], wq_c[l].rearrange("(k p) n -> p k n", p=128), eng="pool")
        Woc = P.sb("Woc", [128, 4, D], BF16)
        P.dma(Woc[:], wo_c[l].rearrange("(k p) n -> p k n", p=128), eng="pool")
        kxT = P.sb("kxT", [128, 4, 256], BF16); vx = P.sb("vx", [128, 2, 512], BF16)
        hT = P.sb("hT", [128, 4, D]); xn_tok = P.sb("xn_tok", [128, 4, D], BF16); xT = P.sb("xT", [128, 8, T], BF16)
        junk = P.sb("junk", [128, D]); ss = P.sb("ss", [128, 4]); rstd = P.sb("rstd", [128, 4])
        mk2 = P.mark()
        gm = P.sb("gm", [128, D]); P.dma(gm[:], g_mem[:, l, :])
        Wkv = P.sb("Wkv", [128, 8, D], BF16)
        for kc in range(0, 8, 4):
            P.dma(Wkv[:, kc:kc + 4, :], wkv_c[l, kc * 128:(kc + 4) * 128, :].rearrange("(k p) n -> p k n", p=128), eng="pool")
        mt_ = P.sb("memt", [128, 2, D]); P.dma(mt_[:], mem.rearrange("(t p) d -> p t d", p=128))
        mn = P.sb("memn", [128, 2, D], BF16); memT = P.sb("memT", [128, 8, 256], BF16)
        for t2 in range(2):
            P.act(junk[:], mt_[:, t2, :], AF.Square, accum_out=ss[:, t2:t2 + 1])
        P.act(rstd[:, 0:2], ss[:, 0:2], AF.Sqrt, bias=epsc[:], scale=1.0 / D)
        P.recip(rstd[:, 0:2], rstd[:, 0:2])
        for t2 in range(2):
            P.stt(mn[:, t2, :], mt_[:, t2, :], rstd[:, t2:t2 + 1], gm[:], ALU.mult, ALU.mult)
            pb = npb()
            for kc in range(8):
                P.tr(pb[:, kc * 128:(kc + 1) * 128], mn[:, t2, kc * 128:(kc + 1) * 128], ident[:])
            P.copy(memT[:, :, t2 * 128:(t2 + 1) * 128], pb[:, :].rearrange("p (a b) -> p a b", a=8))
        for hd in range(4):
            p_ = nps()
            for kc in range(8):
                P.mm(p_[:, 0:256], Wkv[:, kc, hd * 128:(hd + 1) * 128], memT[:, kc, :], start=kc == 0, stop=kc == 7)
            P.copy(kxT[:, hd, :], p_[:, 0:256])
        for t2 in range(2):
            p_ = nps()
            for kc in range(8):
                P.mm(p_[:], memT[:, kc, t2 * 128:(t2 + 1) * 128], Wkv[:, kc, 512:1024], start=kc == 0, stop=kc == 7)
            P.copy(vx[:, t2, :], p_[:])
        P.barrier()
        P.release(mk2)
        qxT = P.sb("qxT", [128, 4, T], BF16); ptm = [P.sb(f"ptm{i}", [128, T], BF16) for i in range(2)]
        rden = P.sb("rden", [128, T]); oxT = P.sb("oxT", [128, 4, T], BF16)
        hid = P.sb("hid", [128, NFC, T], BF16)
        w1g = [P.sb(f"w1g{i}", [128, 8, 512], BF16) for i in range(2)]
        w1u = [P.sb(f"w1u{i}", [128, 8, 512], BF16) for i in range(2)]
        w2h = [P.sb(f"w2h{i}", [128, NFC, 512], BF16) for i in range(2)]
        sg = [P.sb(f"sg{i}", [128, T]) for i in range(2)]
        ne = 8 if moe else 1
        if moe:
            Wr = P.sb("Wr", [128, 8, 8], BF16)
            P.dma(Wr[:], router_w[0].rearrange("(k p) n -> p k n", p=128), eng="pool")
            rb = P.sb("rb", [128, 8]); P.dma(rb[:], rb_b)
            lg = P.sb("lg", [128, 8]); lg2 = P.sb("lg2", [128, 8]); eq1 = P.sb("eq1", [128, 8]); eq2 = P.sb("eq2", [128, 8])
            m1 = P.sb("m1", [128, 1]); m2 = P.sb("m2", [128, 1]); dd = P.sb("dd", [128, 1]); w1_ = P.sb("w1_", [128, 1]); w2_ = P.sb("w2_", [128, 1])
            comb = P.sb("comb", [128, 4, 8])
        if last:
            gfin = P.sb("gfin", [128, D]); P.dma(gfin[:], g_final)
            yo = P.sb("yo", [128, 4, D])
        slab_i = 0
        hTs = [hT, P.sb("hT2", [128, 4, D])]
        for g in range(ng):
            gs = slice(g * T, (g + 1) * T)
            hT = hTs[g % 2]
            if g == 0:
                load_h(hT, H1, "H1", g)
                rms_to_T(hT, gx[:], xn_tok, xT, junk, ss, rstd)
            if g + 1 < ng:
                load_h(hTs[(g + 1) % 2], H1, "H1", g + 1)
            for hd in range(4):
                p_ = nps()
                for kc in range(8):
                    P.mm(p_[:], Wq[:, kc, hd * 128:(hd + 1) * 128], xT[:, kc, :], start=kc == 0, stop=kc == 7)
                if hd % 2 == 0:
                    P.copy(qxT[:, hd, :], p_[:])
                else:
                    P.act(qxT[:, hd, :], p_[:], AF.Copy)
            for hd in range(4):
                po = nps(); pd = nps()
                for m_ in range(2):
                    p_ = nps()
                    P.mm(p_[:], kxT[:, hd, m_ * 128:(m_ + 1) * 128], qxT[:, hd, :])
                    pt = ptm[m_]
                    P.act(pt[:], p_[:], AF.Exp, scale=128 ** -0.5)
                    P.mm(po[:], vx[:, m_, hd * 128:(hd + 1) * 128], pt[:], start=m_ == 0, stop=m_ == 1)
                    P.mm(pd[:], ones_b[:], pt[:], start=m_ == 0, stop=m_ == 1)
                P.recip(rden[:], pd[:])
                P.tt(oxT[:, hd, :], po[:], rden[:], ALU.mult)
            for tt in range(4):
                for half in range(2):
                    p_ = nps()
                    for hd in range(4):
                        P.mm(p_[:], oxT[:, hd, tt * 128:(tt + 1) * 128], Woc[:, hd, half * 512:(half + 1) * 512], start=hd == 0, stop=hd == 3)
                    P.tt(hT[:, tt, half * 512:(half + 1) * 512], hT[:, tt, half * 512:(half + 1) * 512], p_[:], ALU.add)
            rms_to_T(hT, gf[:], xn_tok, xT, junk, ss, rstd)
            if moe:
                for tt in range(4):
                    p_ = nps()
                    for kc in range(8):
                        P.mm(p_[:, 0:8], xT[:, kc, tt * 128:(tt + 1) * 128], Wr[:, kc, :], start=kc == 0, stop=kc == 7)
                    P.tt(lg[:], p_[:, 0:8], rb[:], ALU.add)
                    P.rmax(m1[:], lg[:])
                    P.ts(eq1[:], lg[:], m1[:, 0:1], None, op0=ALU.is_equal)
                    P.stt(lg2[:], eq1[:], -1e30, lg[:], ALU.mult, ALU.add)
                    P.rmax(m2[:], lg2[:])
                    P.ts(eq2[:], lg2[:], m2[:, 0:1], None, op0=ALU.is_equal)
                    P.tt(dd[:], m2[:], m1[:], ALU.subtract)
                    P.act(w2_[:], dd[:], AF.Sigmoid)
                    P.act(w1_[:], dd[:], AF.Sigmoid, scale=-1.0)
                    P.ts(eq1[:], eq1[:], w1_[:, 0:1], None, op0=ALU.mult)
                    P.stt(comb[:, tt, :], eq2[:], w2_[:, 0:1], eq1[:], ALU.mult, ALU.add)
            for e in range(ne):
                ei = (1 + e) if moe else 0
                w13 = W13B[ei]; w2 = W2B[ei]
                for sbi in range(6):
                    nfc = 4 if sbi < 5 else 2
                    bi = slab_i % 2; slab_i += 1
                    P.dma(w1g[bi][:, :, 0:nfc * 128], w13[:, sbi * 512:sbi * 512 + nfc * 128].rearrange("(k p) n -> p k n", p=128), reads=[f"W13B{ei}"])
                    P.dma(w1u[bi][:, :, 0:nfc * 128], w13[:, DFF + sbi * 512:DFF + sbi * 512 + nfc * 128].rearrange("(k p) n -> p k n", p=128), reads=[f"W13B{ei}"])
                    if sbi == 0:
                        for half in range(2):
                            P.dma(w2h[half][:], w2[:, half * 512:(half + 1) * 512].rearrange("(k p) n -> p k n", p=128), reads=[f"W2B{ei}"])
                    for fl in range(nfc):
                        fc = sbi * 4 + fl
                        pg = nps(); pu = nps()
                        for kc in range(8):
                            P.mm(pg[:], w1g[bi][:, kc, fl * 128:(fl + 1) * 128], xT[:, kc, :], start=kc == 0, stop=kc == 7)
                        for kc in range(8):
                            P.mm(pu[:], w1u[bi][:, kc, fl * 128:(fl + 1) * 128], xT[:, kc, :], start=kc == 0, stop=kc == 7)
                        s_ = sg[fc % 2]
                        P.act(s_[:], pg[:], AF.Silu)
                        P.tt(hid[:, fc, :], s_[:], pu[:], ALU.mult)
                if e == ne - 1 and g + 1 < ng:
                    rms_to_T(hTs[(g + 1) % 2], gx[:], xn_tok, xT, junk, ss, rstd)
                for half in range(2):
                    for tt in range(4):
                        p_ = nps()
                        for fc in range(NFC):
                            P.mm(p_[:], hid[:, fc, tt * 128:(tt + 1) * 128], w2h[half][:, fc, :], start=fc == 0, stop=fc == NFC - 1)
                        hv = hT[:, tt, half * 512:(half + 1) * 512]
                        if moe:
                            P.stt(hv, p_[:], comb[:, tt, e:e + 1], hv, ALU.mult, ALU.add)
                        else:
                            P.tt(hv, hv, p_[:], ALU.add)
            if last:
                for tt in range(4):
                    P.act(junk[:], hT[:, tt, :], AF.Square, accum_out=ss[:, tt:tt + 1])
                P.act(rstd[:], ss[:], AF.Sqrt, bias=epsc[:], scale=1.0 / D)
                P.recip(rstd[:], rstd[:])
                for tt in range(4):
                    P.stt(yo[:, tt, :], hT[:, tt, :], rstd[:, tt:tt + 1], gfin[:], ALU.mult, ALU.mult, eng="dve" if tt % 2 == 0 else "pool")
                P.dma(y[gs, :].rearrange("(t p) d -> p t d", p=128), yo[:], writes=[("y", g)])
            else:
                P.dma(H3[gs, :].rearrange("(t p) d -> p t d", p=128), hT[:], writes=[("H3", g)])
        P.barrier()
        P.release(mk)

    def phase_D_moe(l):
        mk = P.mark()
        nkt = 4 * ng
        gx = P.sb("gx", [128, D]); P.dma(gx[:], g_cross[:, l, :])
        gf = P.sb("gf", [128, D]); P.dma(gf[:], g_ffn[:, l, :])
        EQ1 = P.sb("EQ1", [128, 64, 8]); EQ2 = P.sb("EQ2", [128, 64, 8])
        RK1 = P.sb("RK1", [128, 64]); RK2 = P.sb("RK2", [128, 64]); WW1 = P.sb("WW1", [128, 64]); WW2 = P.sb("WW2", [128, 64])
        carryM = P.sb("carryM", [128, 8]); P.memset(carryM[:], 0.0)
        SL1 = P.sb("SL1", [128, 64], I32); SL2 = P.sb("SL2", [128, 64], I32); IDXW = P.sb("IDXW", [128, NBLK], I32)
        pidx = P.sb("pidx", [128, 1]); P.dma(pidx[:], pidx_d)
        junk = P.sb("junk", [128, D]); ss = P.sb("ss", [128, 4]); rstd = P.sb("rstd", [128, 4])
        mk1 = P.mark()
        Wq = P.sb("Wq", [128, 8, 512], BF16)
        P.dma(Wq[:], wq_c[l].rearrange("(k p) n -> p k n", p=128), eng="pool")
        Woc = P.sb("Woc", [128, 4, D], BF16)
        P.dma(Woc[:], wo_c[l].rearrange("(k p) n -> p k n", p=128), eng="pool")
        kxT = P.sb("kxT", [128, 4, 256], BF16); vx = P.sb("vx", [128, 2, 512], BF16)
        hT = P.sb("hT", [128, 4, D]); xn_tok = P.sb("xn_tok", [128, 4, D], BF16); xT = P.sb("xT", [128, 8, T], BF16)
        zt = P.sb("zt", [128, 4, D], BF16); P.memset(zt[:], 0.0, eng="pool")
        for b in range(NBLK):
            P.dma(XS[b * 512:(b + 1) * 512, :].rearrange("(t p) d -> p t d", p=128), zt[:], writes=[("XS", b)])
        mk2 = P.mark()
        gm = P.sb("gm", [128, D]); P.dma(gm[:], g_mem[:, l, :])
        Wkv = P.sb("Wkv", [128, 8, D], BF16)
        for kc in range(0, 8, 4):
            P.dma(Wkv[:, kc:kc + 4, :], wkv_c[l, kc * 128:(kc + 4) * 128, :].rearrange("(k p) n -> p k n", p=128), eng="pool")
        mt_ = P.sb("memt", [128, 2, D]); P.dma(mt_[:], mem.rearrange("(t p) d -> p t d", p=128))
        mn = P.sb("memn", [128, 2, D], BF16); memT = P.sb("memT", [128, 8, 256], BF16)
        for t2 in range(2):
            P.act(junk[:], mt_[:, t2, :], AF.Square, accum_out=ss[:, t2:t2 + 1])
        P.act(rstd[:, 0:2], ss[:, 0:2], AF.Sqrt, bias=epsc[:], scale=1.0 / D)
        P.recip(rstd[:, 0:2], rstd[:, 0:2])
        for t2 in range(2):
            P.stt(mn[:, t2, :], mt_[:, t2, :], rstd[:, t2:t2 + 1], gm[:], ALU.mult, ALU.mult)
            pb = npb()
            for kc in range(8):
                P.tr(pb[:, kc * 128:(kc + 1) * 128], mn[:, t2, kc * 128:(kc + 1) * 128], ident[:])
            P.copy(memT[:, :, t2 * 128:(t2 + 1) * 128], pb[:, :].rearrange("p (a b) -> p a b", a=8))
        for hd in range(4):
            p_ = nps()
            for kc in range(8):
                P.mm(p_[:, 0:256], Wkv[:, kc, hd * 128:(hd + 1) * 128], memT[:, kc, :], start=kc == 0, stop=kc == 7)
            P.copy(kxT[:, hd, :], p_[:, 0:256])
        for t2 in range(2):
            p_ = nps()
            for kc in range(8):
                P.mm(p_[:], memT[:, kc, t2 * 128:(t2 + 1) * 128], Wkv[:, kc, 512:1024], start=kc == 0, stop=kc == 7)
            P.copy(vx[:, t2, :], p_[:])
        P.barrier()
        P.release(mk2)
        qxT = P.sb("qxT", [128, 4, T], BF16); ptm = [P.sb(f"ptm{i}", [128, T], BF16) for i in range(2)]
        rden = P.sb("rden", [128, T]); oxT = P.sb("oxT", [128, 4, T], BF16)
        Wr = P.sb("Wr", [128, 8, 8], BF16)
        P.dma(Wr[:], router_w[0].rearrange("(k p) n -> p k n", p=128), eng="pool")
        rb = P.sb("rb", [128, 8]); P.dma(rb[:], rb_b)
        lg = P.sb("lg", [128, 8]); lg2 = P.sb("lg2", [128, 8]); msk = P.sb("msk", [128, 8]); rk = P.sb("rk", [128, 8]); t8 = P.sb("t8", [128, 8])
        m1 = P.sb("m1", [128, 1]); m2 = P.sb("m2", [128, 1]); dd = P.sb("dd", [128, 1])
        for g in range(ng):
            gs = slice(g * T, (g + 1) * T)
            load_h(hT, H1, "H1", g)
            rms_to_T(hT, gx[:], xn_tok, xT, junk, ss, rstd)
            for hd in range(4):
                p_ = nps()
                for kc in range(8):
                    P.mm(p_[:], Wq[:, kc, hd * 128:(hd + 1) * 128], xT[:, kc, :], start=kc == 0, stop=kc == 7)
                if hd % 2 == 0:
                    P.copy(qxT[:, hd, :], p_[:])
                else:
                    P.act(qxT[:, hd, :], p_[:], AF.Copy)
            for hd in range(4):
                po = nps(); pd = nps()
                for m_ in range(2):
                    p_ = nps()
                    P.mm(p_[:], kxT[:, hd, m_ * 128:(m_ + 1) * 128], qxT[:, hd, :])
                    pt = ptm[m_]
                    P.act(pt[:], p_[:], AF.Exp, scale=128 ** -0.5)
                    P.mm(po[:], vx[:, m_, hd * 128:(hd + 1) * 128], pt[:], start=m_ == 0, stop=m_ == 1)
                    P.mm(pd[:], ones_b[:], pt[:], start=m_ == 0, stop=m_ == 1)
                P.recip(rden[:], pd[:])
                P.tt(oxT[:, hd, :], po[:], rden[:], ALU.mult)
            for tt in range(4):
                for half in range(2):
                    p_ = nps()
                    for hd in range(4):
                        P.mm(p_[:], oxT[:, hd, tt * 128:(tt + 1) * 128], Woc[:, hd, half * 512:(half + 1) * 512], start=hd == 0, stop=hd == 3)
                    P.tt(hT[:, tt, half * 512:(half + 1) * 512], hT[:, tt, half * 512:(half + 1) * 512], p_[:], ALU.add)
            P.dma(H3[gs, :].rearrange("(t p) d -> p t d", p=128), hT[:], writes=[("H3", g)])
            rms_to_T(hT, gf[:], xn_tok, xT, junk, ss, rstd)
            P.dma(XN2[gs, :].rearrange("(t p) d -> p t d", p=128), xn_tok[:], writes=[("XN2", g)])
            for tt in range(4):
                kt = 4 * g + tt
                p_ = nps()
                for kc in range(8):
                    P.mm(p_[:, 0:8], xT[:, kc, tt * 128:(tt + 1) * 128], Wr[:, kc, :], start=kc == 0, stop=kc == 7)
                P.tt(lg[:], p_[:, 0:8], rb[:], ALU.add)
                P.rmax(m1[:], lg[:])
                P.ts(EQ1[:, kt, :], lg[:], m1[:, 0:1], None, op0=ALU.is_equal)
                P.stt(lg2[:], EQ1[:, kt, :], -1e30, lg[:], ALU.mult, ALU.add)
                P.rmax(m2[:], lg2[:])
                P.ts(EQ2[:, kt, :], lg2[:], m2[:, 0:1], None, op0=ALU.is_equal)
                P.tt(dd[:], m2[:], m1[:], ALU.subtract)
                P.act(WW2[:, kt:kt + 1], dd[:], AF.Sigmoid)
                P.act(WW1[:, kt:kt + 1], dd[:], AF.Sigmoid, scale=-1.0)
                P.tt(msk[:], EQ1[:, kt, :], EQ2[:, kt, :], ALU.add)
                p3 = nps()
                P.mm(p3[:, 0:8], tri_f[:], msk[:])
                P.mm(p3[:, 8:16], ones_f[:], msk[:])
                P.tt(rk[:], p3[:, 0:8], carryM[:], ALU.add)
                P.tt(rk[:], rk[:], msk[:], ALU.subtract)
                P.tt(carryM[:], p3[:, 8:16], carryM[:], ALU.add)
                P.tt(t8[:], EQ1[:, kt, :], rk[:], ALU.mult)
                P.rsum(RK1[:, kt:kt + 1], t8[:])
                P.tt(t8[:], EQ2[:, kt, :], rk[:], ALU.mult)
                P.rsum(RK2[:, kt:kt + 1], t8[:])
        P.barrier()
        P.release(mk1)
        nblk = P.sb("nblk", [128, 8]); pad = P.sb("pad", [128, 8]); incl = P.sb("incl", [128, 8]); base = P.sb("base", [128, 8])
        t8 = P.sb("t8b", [128, 8]); eb = P.sb("eb", [128, NBLK]); sf1 = P.sb("sf1", [128, 64]); sf2 = P.sb("sf2", [128, 64])
        P.memset(nblk[:], 0.0)
        for j in range(16):
            P.stt(nblk[:], carryM[:], float(512 * j), nblk[:], ALU.is_gt, ALU.add)
        P.ts(pad[:], nblk[:], 512.0, None, op0=ALU.mult)
        P.scan(incl[:], ones_f[:, 0:8], pad[:], 0.0, ALU.mult, ALU.add)
        P.tt(base[:], incl[:], pad[:], ALU.subtract)
        for b in range(NBLK):
            P.ts(t8[:], incl[:], float(512 * b), None, op0=ALU.is_le)
            P.rsum(eb[:, b:b + 1], t8[:])
        P.ts(eb[:], eb[:], 7.0, None, op0=ALU.min)
        P.ts(eb[:], eb[:], 128.0, pidx[:, 0:1], op0=ALU.mult, op1=ALU.add)
        P.copy(IDXW[:], eb[:])
        P.copy(sf1[:], RK1[:]); P.copy(sf2[:], RK2[:])
        for e in range(8):
            P.stt(sf1[:, 0:nkt], EQ1[:, 0:nkt, e], base[:, e:e + 1], sf1[:, 0:nkt], ALU.mult, ALU.add)
            P.stt(sf2[:, 0:nkt], EQ2[:, 0:nkt, e], base[:, e:e + 1], sf2[:, 0:nkt], ALU.mult, ALU.add)
        P.copy(SL1[:], sf1[:]); P.copy(SL2[:], sf2[:])
        xr = [P.sb(f"xr{i}", [128, D], BF16) for i in range(2)]
        XSB = NBLK * 512 - 1
        for kt in range(nkt):
            x_ = xr[kt % 2]
            P.dma(x_[:], XN2[kt * 128:(kt + 1) * 128, :], reads=[("XN2", kt // 4)])
            for SL in (SL1, SL2):
                P.op("pool", lambda e, x_=x_, SL=SL, kt=kt: e.indirect_dma_start(
                    out=XS, out_offset=bass.IndirectOffsetOnAxis(ap=SL[:, kt:kt + 1], axis=0), in_=x_[:], in_offset=None,
                    bounds_check=None), [x_, SL] + [("XS", b) for b in range(NBLK)], ["XSs"], dmakey=("dma", f"xr{kt % 2}"))
        P.barrier()
        mkb = P.mark()
        xs_toks = [P.sb(f"xs_tok{i}", [128, 4, D], BF16) for i in range(2)]; xTs = [P.sb(f"xTb{i}", [128, 8, T], BF16) for i in range(2)]

        def prep_blk(b):
            xs_tok = xs_toks[b % 2]; xT = xTs[b % 2]
            P.dma(xs_tok[:], XS[b * 512:(b + 1) * 512, :].rearrange("(t p) d -> p t d", p=128), reads=["XSs"])
            for tt in range(4):
                pb = npb()
                for kc in range(8):
                    P.tr(pb[:, kc * 128:(kc + 1) * 128], xs_tok[:, tt, kc * 128:(kc + 1) * 128], ident[:])
                src = pb[:, :].rearrange("p (a b) -> p a b", a=8)
                dst = xT[:, :, tt * 128:(tt + 1) * 128]
                if tt % 2 == 0:
                    P.copy(dst, src)
                else:
                    P.act(dst, src, AF.Copy)
        hid = P.sb("hid", [128, NFC, T], BF16)
        w1g = [P.sb(f"w1g{i}", [128, 8, 512], BF16) for i in range(2)]
        w1u = [P.sb(f"w1u{i}", [128, 8, 512], BF16) for i in range(2)]
        w2h = [P.sb(f"w2h{i}", [128, NFC, 512], BF16) for i in range(2)]
        w1gL = P.sb("w1gL", [128, 8, 256], BF16); w1uL = P.sb("w1uL", [128, 8, 256], BF16)
        sg = [P.sb(f"sg{i}", [128, T]) for i in range(2)]
        yo = P.sb("yo", [128, 4, D])
        slab_i = 0

        def wgather(dst, src, b, key):
            P.op("pool", lambda e: e.indirect_dma_start(
                out=dst, out_offset=None, in_=src, in_offset=bass.IndirectOffsetOnAxis(ap=IDXW[:, b:b + 1], axis=0),
                bounds_check=None), [IDXW, "WP"], [dst], dmakey=("dma", key))
        prep_blk(0)
        for b in range(NBLK):
            xT = xTs[b % 2]
            for sbi in range(6):
                if sbi == 4 and b + 1 < NBLK:
                    prep_blk(b + 1)
                nfc = 4 if sbi < 5 else 2
                bi = slab_i % 2; slab_i += 1
                if sbi < 5:
                    g_t, u_t, kg, ku = w1g[bi], w1u[bi], f"w1g{bi}", f"w1u{bi}"
                else:
                    g_t, u_t, kg, ku = w1gL, w1uL, "w1gL", "w1uL"
                wgather(g_t[:].rearrange("p k n -> p (k n)"), WSL[(sbi, 0)].rearrange("r k n -> r (k n)"), b, kg)
                wgather(u_t[:].rearrange("p k n -> p (k n)"), WSL[(sbi, 1)].rearrange("r k n -> r (k n)"), b, ku)
                if sbi == 0:
                    for half in range(2):
                        wgather(w2h[half][:].rearrange("p k n -> p (k n)"), W2H[half].rearrange("r k n -> r (k n)"), b, f"w2h{half}")
                for fl in range(nfc):
                    fc = sbi * 4 + fl
                    pg = nps(); pu = nps()
                    for kc in range(8):
                        P.mm(pg[:], g_t[:, kc, fl * 128:(fl + 1) * 128], xT[:, kc, :], start=kc == 0, stop=kc == 7)
                    for kc in range(8):
                        P.mm(pu[:], u_t[:, kc, fl * 128:(fl + 1) * 128], xT[:, kc, :], start=kc == 0, stop=kc == 7)
                    s_ = sg[fc % 2]
                    P.act(s_[:], pg[:], AF.Silu)
                    P.tt(hid[:, fc, :], s_[:], pu[:], ALU.mult)
            for half in range(2):
                for tt in range(4):
                    p_ = nps()
                    for fc in range(NFC):
                        P.mm(p_[:], hid[:, fc, tt * 128:(tt + 1) * 128], w2h[half][:, fc, :], start=fc == 0, stop=fc == NFC - 1)
                    if tt % 2 == 0:
                        P.copy(yo[:, tt, half * 512:(half + 1) * 512], p_[:])
                    else:
                        P.act(yo[:, tt, half * 512:(half + 1) * 512], p_[:], AF.Copy)
            P.dma(YS[b * 512:(b + 1) * 512, :].rearrange("(t p) d -> p t d", p=128), yo[:], writes=["YSs"])
        P.barrier()
        P.release(mkb)
        gfin = P.sb("gfin", [128, D]); P.dma(gfin[:], g_final)
        ya = [P.sb(f"ya{i}", [128, D]) for i in range(2)]; yb = [P.sb(f"yb{i}", [128, D]) for i in range(2)]
        h2 = [P.sb(f"h2{i}", [128, D]) for i in range(2)]; yo2 = [P.sb(f"yo2{i}", [128, D]) for i in range(2)]
        ssc = P.sb("ssc", [128, 64]); rsc = P.sb("rsc", [128, 64])
        for kt in range(nkt):
            i2 = kt % 2
            for dst_, SL, nm in ((ya[i2], SL1, "ya"), (yb[i2], SL2, "yb")):
                P.op("pool", lambda e, dst_=dst_, SL=SL, kt=kt: e.indirect_dma_start(
                    out=dst_[:], out_offset=None, in_=YS, in_offset=bass.IndirectOffsetOnAxis(ap=SL[:, kt:kt + 1], axis=0),
                    bounds_check=None), [SL, "YSs"], [dst_], dmakey=("dma", f"{nm}{i2}"))
            P.dma(h2[i2][:], H3[kt * 128:(kt + 1) * 128, :], reads=[("H3", kt // 4)])
            P.stt(h2[i2][:], ya[i2][:], WW1[:, kt:kt + 1], h2[i2][:], ALU.mult, ALU.add)
            P.stt(h2[i2][:], yb[i2][:], WW2[:, kt:kt + 1], h2[i2][:], ALU.mult, ALU.add)
            P.act(junk[:], h2[i2][:], AF.Square, accum_out=ssc[:, kt:kt + 1])
            P.act(rsc[:, kt:kt + 1], ssc[:, kt:kt + 1], AF.Sqrt, bias=epsc[:], scale=1.0 / D)
            P.recip(rsc[:, kt:kt + 1], rsc[:, kt:kt + 1])
            P.stt(yo2[i2][:], h2[i2][:], rsc[:, kt:kt + 1], gfin[:], ALU.mult, ALU.mult)
            P.dma(y[kt * 128:(kt + 1) * 128, :], yo2[i2][:], writes=[("y", kt)])
        P.barrier()
        P.release(mk)

    phases = []
    prep_ffn(0, dense_w13[0], dense_w2[0])
    np_ = 0
    for l in range(nlayers):
        hsrc, hname = (x, "x") if l == 0 else (H3, "H3")
        for ph in "ABCD":
            if np_ >= nphase:
                break
            np_ += 1
            if ph == "A":
                phase_A(l, hsrc, hname)
            elif ph == "B":
                phase_B(l)
            elif ph == "C":
                phase_C(l, hsrc, hname)
            else:
                if l % 2 == 1:
                    phase_D_moe(l)
                else:
                    phase_D(l, moe=False, last=(l == nlayers - 1))
    P.emit()
    P.close()
    return nc, P


def host_inputs(inputs, b):
    f = lambda a: np.ascontiguousarray(np.asarray(a, dtype=np.float32))
    rep = lambda a: f(np.broadcast_to(np.asarray(a)[None], (128,) + np.asarray(a).shape))
    m = {}
    m["x"] = f(inputs["x"][b]); m["mem"] = f(inputs["mem"][b])
    for k in ("w_in", "sgu_w", "rg_wa", "rg_wx", "w_branch", "w_gate", "w_out", "wq_c", "wkv_c", "wo_c",
              "dense_w13", "dense_w2", "router_w", "moe_w13", "moe_w2"):
        m[k] = f(inputs[k])
    m["g_mix"] = rep(inputs["norm_mix"]); m["g_cross"] = rep(inputs["norm_cross"]); m["g_ffn"] = rep(inputs["norm_ffn"])
    m["g_mem"] = rep(inputs["norm_mem"]); m["g_final"] = rep(inputs["norm_final"])
    m["sgu_g_b"] = rep(inputs["sgu_g"]); m["sgu_b_b"] = rep(np.asarray(inputs["sgu_b"]).reshape(L, 512))
    m["sinks_b"] = rep(inputs["swa_sinks"]); m["bf_b"] = rep(inputs["fox_bf"]); m["rb_b"] = rep(np.asarray(inputs["router_b"])[0])
    pp = lambda a: f(np.asarray(a).reshape(L, 4, 128).transpose(2, 0, 1))
    m["conv_w_p"] = f(np.asarray(inputs["conv_w"]).reshape(L, 4, 4, 128).transpose(3, 0, 1, 2))
    m["conv_b_p"] = pp(inputs["conv_b"]); m["ba_p"] = pp(inputs["rg_ba"]); m["bx_p"] = pp(inputs["rg_bx"]); m["lam_p"] = pp(inputs["rg_lambda"])
    m["bgate_p"] = f(np.asarray(inputs["b_gate"]).reshape(L, 4, 8, 128).transpose(3, 0, 1, 2))
    m["ident"] = np.eye(128, dtype=np.float32)
    m["pidx"] = np.arange(128, dtype=np.float32).reshape(128, 1)
    k = np.arange(128)[:, None]; q = np.arange(128)[None, :]
    m["tri"] = (k <= q).astype(np.float32)
    slopes = 2.0 ** (-(np.arange(1, 9, dtype=np.float64)))
    me = np.zeros((128, 8, 2, 128), np.float64)
    for h in range(8):
        me[:, h, 0, :] = np.where(k <= q, np.exp(-slopes[h] * (q - k)), 0.0)
        me[:, h, 1, :] = np.where(k > q, np.exp(-slopes[h] * (q + 128 - k)), 0.0)
    m["mexp"] = me.astype(np.float32)
    return m


_CACHE = {}


def kernel(**inputs):
    if "nc" not in _CACHE:
        _CACHE["nc"] = build()[0]
    nc = _CACHE["nc"]
    in_maps = [host_inputs(inputs, b) for b in range(8)]
    res = run_bass_kernel_spmd(nc, in_maps, core_ids=list(range(8)))
    return np.stack([np.asarray(r["y"], dtype=np.float32) for r in res.results], axis=0)
```

```python
import numpy as np
import concourse.bass as bass
import concourse.mybir as mybir
from concourse.bass_utils import run_bass_kernel_spmd

F32 = mybir.dt.float32
BF16 = mybir.dt.bfloat16
AF = mybir.ActivationFunctionType
ALU = mybir.AluOpType
AX = mybir.AxisListType

ENGS = ("pe", "act", "dve", "pool", "sp")
SAME_ENGINE_SYNC = True


class Prog:
    def __init__(self, nc):
        self.nc = nc
        self.ops = []
        self.stack = []
        self.uid = 0

    def sb(self, name, shape, dt=F32):
        self.uid += 1
        g = self.nc.sbuf_tensor(f"{name}_{self.uid}", list(shape), dt)
        t = g.__enter__()
        self.stack.append(g)
        return t

    def ps(self, name, shape, dt=F32):
        g = self.nc.psum_tensor(name, list(shape), dt)
        t = g.__enter__()
        self.stack.append(g)
        return t

    def mark(self):
        return len(self.stack)

    def release(self, mk):
        while len(self.stack) > mk:
            self.stack.pop().__exit__(None, None, None)

    @staticmethod
    def _tok(x):
        if isinstance(x, (str, tuple)):
            return x
        return x.name

    def op(self, eng, fn, reads, writes, dmakey=None):
        r = tuple(self._tok(x) for x in reads if x is not None and not isinstance(x, (int, float)))
        w = tuple(self._tok(x) for x in writes if x is not None)
        self.ops.append((eng, fn, r, w, dmakey))

    def barrier(self):
        for e in ENGS:
            self.ops.append((e, None, (), (), None))

    def mm(self, out, lhsT, rhs, start=True, stop=True, sgc=False):
        if sgc:
            self.op("pe", lambda e: e.matmul(out, lhsT, rhs, start=start, stop=stop, skip_group_check=True),
                    [lhsT, rhs], [out])
        else:
            self.op("pe", lambda e: e.matmul(out, lhsT, rhs, start=start, stop=stop),
                    [lhsT, rhs], [out])

    def tr(self, out, in_, ident):
        self.op("pe", lambda e: e.transpose(out, in_, ident), [in_, ident], [out])

    def act(self, out, in_, func, bias=None, scale=None, accum_out=None):
        kw = {}
        if bias is not None:
            kw["bias"] = bias
        if scale is not None:
            kw["scale"] = scale
        if accum_out is not None:
            kw["accum_out"] = accum_out
        self.op("act", lambda e: e.activation(out, in_, func, **kw),
                [in_, bias, scale], [out, accum_out])

    def tt(self, out, in0, in1, op, eng="dve"):
        self.op(eng, lambda e: e.tensor_tensor(out, in0, in1, op), [in0, in1], [out])

    def ts(self, out, in0, s1, s2=None, op0=ALU.mult, op1=None, eng="dve"):
        kw = {}
        if op1 is not None:
            kw["op1"] = op1
        self.op(eng, lambda e: e.tensor_scalar(out, in0, s1, s2, op0, **kw), [in0, s1, s2], [out])

    def stt(self, out, in0, scalar, in1, op0, op1, eng="dve"):
        eng = "dve"
        self.op(eng, lambda e: e.scalar_tensor_tensor(out, in0, scalar, in1, op0, op1), [in0, scalar, in1], [out])

    def copy(self, out, in_, eng="dve"):
        self.op(eng, lambda e: e.tensor_copy(out, in_), [in_], [out])

    def memset(self, out, val, eng="dve"):
        self.op(eng, lambda e: e.memset(out, val), [], [out])

    def scan(self, out, d0, d1, init, op0, op1):
        self.op("dve", lambda e: e.tensor_tensor_scan(out, d0, d1, init, op0, op1), [d0, d1, init], [out])

    def recip(self, out, in_):
        self.op("dve", lambda e: e.reciprocal(out, in_), [in_], [out])

    def rsum(self, out, in_):
        self.op("dve", lambda e: e.reduce_sum(out, in_, AX.X), [in_], [out])

    def rmax(self, out, in_):
        self.op("dve", lambda e: e.reduce_max(out, in_, AX.X), [in_], [out])

    def dma(self, out, in_, eng="sp", key=None, reads=None, writes=None):
        r = [in_] if reads is None else list(reads)
        w = [out] if writes is None else list(writes)
        if key is None:
            key = in_.name if "dram" in str(type(out.tensor)).lower() else out.name
            key = key.rsplit("_", 1)[0]
        self.op(eng, lambda e: e.dma_start(out=out, in_=in_), r, w, dmakey=("dma", key))

    def emit(self):
        nc = self.nc
        ops = self.ops
        n = len(ops)
        last_w, readers = {}, {}
        last_eng, last_dma = {}, {}
        deps = [None] * n
        i = 0
        while i < n:
            eng, fn, r, w, dk = ops[i]
            if fn is None:
                snap = set(last_eng.values()) | set(last_dma.values())
                for k in range(len(ENGS)):
                    deps[i + k] = set(snap)
                last_w, readers = {}, {}
                i += len(ENGS)
                continue
            d = set()
            for t in r:
                if t in last_w:
                    d.add(last_w[t])
            for t in w:
                if t in last_w:
                    d.add(last_w[t])
                for j in readers.get(t, ()):
                    d.add(j)
            d.discard(i)
            best = {}
            for j in d:
                sk = ops[j][4] if ops[j][4] is not None else ops[j][0]
                if best.get(sk, -1) < j:
                    best[sk] = j
            d = set(best.values())
            deps[i] = d
            for t in w:
                last_w[t] = i
                readers[t] = []
            for t in r:
                if t not in w:
                    readers.setdefault(t, []).append(i)
            if dk is not None:
                last_dma[dk] = i
            else:
                last_eng[eng] = i
            i += 1

        def needs_wait(i, j):
            ei, ej = ops[i][0], ops[j][0]
            if ops[j][4] is not None:
                return True
            if ei != ej:
                return True
            if ei == "pe":
                return False
            return SAME_ENGINE_SYNC

        waited = [False] * n
        for i in range(n):
            for j in deps[i]:
                if needs_wait(i, j):
                    waited[j] = True
        sems, counts = {}, {}
        semval = [None] * n

        def get_sem(k):
            if k not in sems:
                g = nc.semaphore("s_" + "_".join(str(x) for x in k))
                sems[k] = g.__enter__()
                self.stack.append(g)
                counts[k] = 0
            return sems[k]

        for i, (eng, fn, r, w, dk) in enumerate(ops):
            if fn is None:
                continue
            if dk is not None:
                get_sem(dk)
                counts[dk] += 16
                semval[i] = (dk, counts[dk])
            elif waited[i]:
                k = ("eng", eng)
                get_sem(k)
                counts[k] += 1
                semval[i] = (k, counts[k])
        streams = {e: [] for e in ENGS}
        seen = {e: {} for e in ENGS}
        for i, (eng, fn, r, w, dk) in enumerate(ops):
            need = {}
            for j in deps[i]:
                if not needs_wait(i, j):
                    continue
                k, v = semval[j]
                if need.get(k, 0) < v:
                    need[k] = v
            waits = []
            for k, v in need.items():
                if seen[eng].get(k, 0) < v:
                    seen[eng][k] = v
                    waits.append((k, v))
            streams[eng].append((i, waits))
        self.counts = dict(counts)
        engobj = {"pe": "tensor", "act": "scalar", "dve": "vector", "pool": "gpsimd", "sp": "sync"}
        tail = [(k, v) for k, v in counts.items()]
        with nc.Block() as block:
            for ename in ENGS:
                def body(e, ename=ename):
                    for i, waits in streams[ename]:
                        for k, v in waits:
                            e.wait_ge(sems[k], v)
                        if ops[i][1] is None:
                            continue
                        ins = ops[i][1](e)
                        if semval[i] is not None:
                            k, v = semval[i]
                            ins.then_inc(sems[k], 16 if k[0] == "dma" else 1)
                    if ename == "sp":
                        for k, v in tail:
                            e.wait_ge(sems[k], v)
                getattr(block, engobj[ename])(body)

    def close(self):
        self.release(0)


S, D, T, NG, L = 8192, 1024, 512, 16, 2
DFF = 2816
NFC = DFF // 128
EPS = 1e-6
O_AU, O_AV, O_BX, O_BY, O_CQ, O_CK, O_CV, O_DQ, O_DK, O_DV, O_DF = 0, 512, 1024, 1536, 2048, 2560, 2688, 2816, 3328, 3840, 4352


def build(nlayers=2, nphase=99, dbg=False, ng=NG):
    nc = bass.Bass("TRN2", target_bir_lowering=False)
    P = Prog(nc)

    def din(name, shape):
        return nc.dram_tensor(name, list(shape), F32, kind="ExternalInput").ap()

    def dscr(name, shape, dt):
        kind = "ExternalOutput" if dbg else "Internal"
        return nc.dram_tensor(name, list(shape), dt, kind=kind).ap()

    x = din("x", [S, D]); mem = din("mem", [256, D])
    w_in = din("w_in", [L, D, 4360]); sgu_w = din("sgu_w", [L, 4, 128, 128])
    rg_wa = din("rg_wa", [L, 4, 128, 128]); rg_wx = din("rg_wx", [L, 4, 128, 128])
    w_branch = din("w_branch", [L, 4, 512, D]); w_gate = din("w_gate", [L, 4, D, D]); w_out = din("w_out", [L, D, D])
    wq_c = din("wq_c", [L, D, 512]); wkv_c = din("wkv_c", [L, D, 1024]); wo_c = din("wo_c", [L, 512, D])
    dense_w13 = din("dense_w13", [1, D, 2 * DFF]); dense_w2 = din("dense_w2", [1, DFF, D])
    router_w = din("router_w", [1, D, 8])
    moe_w13 = din("moe_w13", [1, 8, D, 2 * DFF]); moe_w2 = din("moe_w2", [1, 8, DFF, D])
    g_mix = din("g_mix", [128, L, D]); g_cross = din("g_cross", [128, L, D]); g_ffn = din("g_ffn", [128, L, D])
    g_mem = din("g_mem", [128, L, D]); g_final = din("g_final", [128, D])
    sgu_g_b = din("sgu_g_b", [128, L, 512]); sgu_b_b = din("sgu_b_b", [128, L, 512])
    sinks_b = din("sinks_b", [128, L, 8]); bf_b = din("bf_b", [128, L, 8]); rb_b = din("rb_b", [128, 8])
    conv_w_p = din("conv_w_p", [128, L, 4, 4]); conv_b_p = din("conv_b_p", [128, L, 4])
    ba_p = din("ba_p", [128, L, 4]); bx_p = din("bx_p", [128, L, 4]); lam_p = din("lam_p", [128, L, 4])
    bgate_p = din("bgate_p", [128, L, 4, 8])
    pidx_d = din("pidx", [128, 1])
    ident_d = din("ident", [128, 128]); tri_d = din("tri", [128, 128]); mexp_d = din("mexp", [128, 8, 2, 128])
    y = nc.dram_tensor("y", [S, D], F32, kind="ExternalOutput").ap()

    XNT = dscr("XNT", [128, 8, S], BF16)
    QT = dscr("QT", [128, 8, S], BF16)
    KT = dscr("KT", [128, 5, S], BF16)
    VV = dscr("VV", [128, 64, 640], BF16)
    OA = dscr("OA", [128, 4, S], BF16)
    OB = dscr("OB", [128, 4, S], BF16)
    OC = dscr("OC", [128, 64, 512], BF16)
    OD = dscr("OD", [128, 64, 512], BF16)
    H1 = dscr("H1", [S, D], F32)
    H3 = dscr("H3", [S, D], F32)
    I32 = mybir.dt.int32
    NBLK = 40
    WSL = {}
    for sbi in range(6):
        for t_ in range(2):
            WSL[(sbi, t_)] = nc.dram_tensor(f"WSL{sbi}_{t_}", [1024, 8, 512 if sbi < 5 else 256], BF16, kind="Internal").ap()
    W2H = [nc.dram_tensor(f"W2H{hf}", [1024, NFC, 512], BF16, kind="Internal").ap() for hf in range(2)]
    XS = nc.dram_tensor("XS", [NBLK * 512, D], BF16, kind="Internal").ap()
    YS = nc.dram_tensor("YS", [NBLK * 512, D], F32, kind="Internal").ap()
    XN2 = nc.dram_tensor("XN2", [S, D], BF16, kind="Internal").ap()
    W13B = [nc.dram_tensor(f"W13B{e}", [D, 2 * DFF], BF16, kind="Internal").ap() for e in range(1)]
    W2B = [nc.dram_tensor(f"W2B{e}", [DFF, D], BF16, kind="Internal").ap() for e in range(1)]

    ps = [P.ps(f"ps{i}", [128, 512]) for i in range(6)]
    pbs = [P.ps(f"pb{i}", [128, 1024], BF16) for i in range(2)]
    ring = {"i": 0, "b": 0}

    def nps(n=6):
        ring["i"] = (ring["i"] + 1) % n
        return ps[ring["i"]]

    def npb():
        ring["b"] = (ring["b"] + 1) % 2
        return pbs[ring["b"]]

    ident = P.sb("ident", [128, 128], BF16)
    tri_f = P.sb("tri_f", [128, 128]); tri_b = P.sb("tri_b", [128, 128], BF16)
    ones_f = P.sb("ones_f", [128, 128]); ones_b = P.sb("ones_b", [128, 128], BF16)
    CK = P.sb("CK", [128, 64, 8]); CREF = P.sb("CREF", [128, 16, 8])
    epsc = P.sb("epsc", [128, 1])
    P.dma(ident[:], ident_d, eng="pool")
    P.dma(tri_f[:], tri_d)
    P.dma(tri_b[:], tri_d, eng="pool")
    P.memset(ones_f[:], 1.0); P.memset(ones_b[:], 1.0); P.memset(epsc[:], EPS)

    def prep_ffn(e_idx, w13src, w2src):
        for kc in range(8):
            P.dma(W13B[e_idx][kc * 128:(kc + 1) * 128, :], w13src[kc * 128:(kc + 1) * 128, :], eng="pool",
                  key=f"prep{kc % 4}", writes=[f"W13B{e_idx}"])
        for fc in range(0, NFC, 2):
            P.dma(W2B[e_idx][fc * 128:(fc + 2) * 128, :], w2src[fc * 128:(fc + 2) * 128, :], eng="pool",
                  key=f"prep{(fc // 2) % 4}", writes=[f"W2B{e_idx}"])

    def prep_moe(e):
        w13v = moe_w13[0, e].rearrange("(k p) n -> p k n", p=128)
        w2v = moe_w2[0, e].rearrange("(k p) n -> p k n", p=128)
        i = 0
        for sbi in range(6):
            ncol = 512 if sbi < 5 else 256
            for t_ in range(2):
                c0 = t_ * DFF + sbi * 512
                P.dma(WSL[(sbi, t_)][e * 128:(e + 1) * 128, :, :], w13v[:, :, c0:c0 + ncol], eng="pool",
                      key=f"prep{i % 4}", writes=["WP"])
                i += 1
        for hf in range(2):
            for f0 in range(0, NFC, 11):
                P.dma(W2H[hf][e * 128:(e + 1) * 128, f0:f0 + 11, :], w2v[:, f0:f0 + 11, hf * 512:(hf + 1) * 512], eng="pool",
                      key=f"prep{i % 4}", writes=["WP"])
                i += 1

    def rms_p1(hT, gain, xn_tok, junk, ss, rstd):
        for tt in range(4):
            P.act(junk[:], hT[:, tt, :], AF.Square, accum_out=ss[:, tt:tt + 1])
        P.act(rstd[:], ss[:], AF.Sqrt, bias=epsc[:], scale=1.0 / D)
        P.recip(rstd[:], rstd[:])
        for tt in range(4):
            P.stt(xn_tok[:, tt, :], hT[:, tt, :], rstd[:, tt:tt + 1], gain, ALU.mult, ALU.mult,
                  eng="dve" if tt % 2 == 0 else "pool")

    def rms_p2(xn_tok, xT):
        for tt in range(4):
            pb = npb()
            for kc in range(8):
                P.tr(pb[:, kc * 128:(kc + 1) * 128], xn_tok[:, tt, kc * 128:(kc + 1) * 128], ident[:])
            src = pb[:, :].rearrange("p (a b) -> p a b", a=8)
            dst = xT[:, :, tt * 128:(tt + 1) * 128]
            if tt % 2 == 0:
                P.copy(dst, src)
            else:
                P.act(dst, src, AF.Copy)

    def rms_to_T(hT, gain, xn_tok, xT, junk, ss, rstd):
        rms_p1(hT, gain, xn_tok, junk, ss, rstd)
        rms_p2(xn_tok, xT)

    def load_h(hT, hsrc, hname, g):
        P.dma(hT[:], hsrc[g * T:(g + 1) * T, :].rearrange("(t p) d -> p t d", p=128), reads=[(hname, g)])

    def phase_A(l, hsrc, hname):
        mk = P.mark()
        Win = P.sb("Win", [128, 8, 4360], BF16)
        for kc in range(8):
            P.dma(Win[:, kc, :], w_in[l, kc * 128:(kc + 1) * 128, :], eng="pool")
        gain = P.sb("gainA", [128, D]); P.dma(gain[:], g_mix[:, l, :])
        sgug = P.sb("sgug", [128, 512]); P.dma(sgug[:], sgu_g_b[:, l, :])
        sgub = P.sb("sgub", [128, 512]); P.dma(sgub[:], sgu_b_b[:, l, :])
        bfb = P.sb("bfb", [128, 8]); P.dma(bfb[:], bf_b[:, l, :])
        cw = P.sb("cw", [128, 4, 4]); P.dma(cw[:], conv_w_p[:, l])
        cb = P.sb("cb", [128, 4]); P.dma(cb[:], conv_b_p[:, l, :])
        bap = P.sb("bap", [128, 4]); P.dma(bap[:], ba_p[:, l, :])
        bxp = P.sb("bxp", [128, 4]); P.dma(bxp[:], bx_p[:, l, :])
        lam = P.sb("lam", [128, 4]); P.dma(lam[:], lam_p[:, l, :])
        cc = P.sb("cc", [128, 4])
        P.act(cc[:], lam[:], AF.Exp, scale=-1.0)
        P.act(cc[:], cc[:], AF.Ln, bias=1.0)
        P.ts(cc[:], cc[:], -8.0, None, op0=ALU.mult)
        Wa = P.sb("Wa", [128, 4, 128], BF16); Wx = P.sb("Wx", [128, 4, 128], BF16)
        P.dma(Wa[:], rg_wa[l].rearrange("h i o -> i h o"), eng="pool")
        P.dma(Wx[:], rg_wx[l].rearrange("h i o -> i h o"), eng="pool")
        wsb = P.sb("wsb", [128, 4, 128], BF16); WsT = P.sb("WsT", [128, 4, 128], BF16)
        P.dma(wsb[:], sgu_w[l].rearrange("g t s -> t g s"), eng="pool")
        pb = npb()
        for gc in range(4):
            P.tr(pb[:, gc * 128:(gc + 1) * 128], wsb[:, gc, :], ident[:])
        for gc in range(4):
            P.tt(WsT[:, gc, :], pb[:, gc * 128:(gc + 1) * 128], tri_b[:], ALU.mult)

        hT = P.sb("hT", [128, 4, D]); xn_tok = P.sb("xn_tok", [128, 4, D], BF16); xnT = P.sb("xnT", [128, 8, T], BF16)
        junk = P.sb("junk", [128, D]); ss = P.sb("ss", [128, 4]); rstd = P.sb("rstd", [128, 4])
        uT = P.sb("uT", [128, 4, T], BF16); vg = P.sb("vg", [128, 512]); v_tok = P.sb("v_tok", [128, 4, 512], BF16)
        ssv = P.sb("ssv", [128, 1]); rsv = P.sb("rsv", [128, 1])
        bx = P.sb("bx", [128, 4, T + 3]); gy = P.sb("gy", [128, 4, T])
        QTst = P.sb("QTst", [128, 8, T], BF16); KTst = P.sb("KTst", [128, 5, T], BF16); Vst = P.sb("Vst", [128, 4, 640], BF16)
        oaT = P.sb("oaT", [128, 4, T], BF16); obT = P.sb("obT", [128, 4, T], BF16)
        tmpa = P.sb("tmpa", [128, 4, 128])
        xf = P.sb("xf", [128, 8]); ls = P.sb("ls", [128, 8]); carry = P.sb("carry", [128, 8]); hcar = P.sb("hcar", [128, 4])
        xc4 = P.sb("xc4", [128, 4, T]); xcb4 = P.sb("xcb4", [128, 4, T], BF16); rr = P.sb("rr", [128, T]); ii = P.sb("ii", [128, T])
        aa = P.sb("aa", [128, T]); a2 = P.sb("a2", [128, T]); inp = P.sb("inp", [128, T]); hh = P.sb("hh", [128, T])
        P.memset(bx[:], 0.0); P.memset(carry[:], 0.0); P.memset(hcar[:], 0.0)

        xnTs = [xnT, P.sb("xnT2", [128, 8, T], BF16)]
        load_h(hT, hsrc, hname, 0)
        rms_p1(hT, gain[:], xn_tok, junk, ss, rstd)
        if ng > 1:
            load_h(hT, hsrc, hname, 1)
        rms_p2(xn_tok, xnTs[0])
        for g in range(ng):
            xnT = xnTs[g % 2]
            P.dma(XNT[:, :, g * T:(g + 1) * T], xnT[:], writes=[("XNT", g)])
            if g + 1 < ng:
                rms_p1(hT, gain[:], xn_tok, junk, ss, rstd)
                if g + 2 < ng:
                    load_h(hT, hsrc, hname, g + 2)

            def proj(col0, nch, epi):
                for c in range(nch):
                    p_ = nps()
                    for kc in range(8):
                        P.mm(p_[:], Win[:, kc, col0 + c * 128:col0 + (c + 1) * 128], xnT[:, kc, :], start=kc == 0, stop=kc == 7)
                    epi(c, p_)
            proj(O_AU, 4, lambda c, p_: P.act(uT[:, c, :], p_[:], AF.Gelu_apprx_tanh))
            proj(O_BX, 4, lambda c, p_: P.copy(bx[:, c, 3:T + 3], p_[:]))
            proj(O_BY, 4, lambda c, p_: P.act(gy[:, c, :], p_[:], AF.Gelu_apprx_tanh))
            for c in range(4):
                P.act(xc4[:, c, :], bx[:, c, 3:T + 3], AF.Identity, bias=cb[:, c:c + 1], scale=cw[:, 3, c:c + 1])
                for k in range(3):
                    P.stt(xc4[:, c, :], bx[:, c, k:k + T], cw[:, k, c:c + 1], xc4[:, c, :], ALU.mult, ALU.add)
                P.copy(xcb4[:, c, :], xc4[:, c, :], eng="pool")
                P.copy(bx[:, c, 0:3], bx[:, c, T:T + 3], eng="pool")
            proj(O_CQ, 4, lambda c, p_: P.copy(QTst[:, c, :], p_[:]))
            proj(O_DQ, 4, lambda c, p_: P.act(QTst[:, 4 + c, :], p_[:], AF.Copy))
            proj(O_CK, 1, lambda c, p_: P.copy(KTst[:, 0, :], p_[:]))
            proj(O_DK, 4, lambda c, p_: P.act(KTst[:, 1 + c, :], p_[:], AF.Copy))
            if g + 1 < ng:
                rms_p2(xn_tok, xnTs[(g + 1) % 2])
            for tt in range(4):
                tok = slice(tt * 128, (tt + 1) * 128)
                p_ = nps()
                for kc in range(8):
                    P.mm(p_[:], xnT[:, kc, tok], Win[:, kc, O_AV:O_AV + 512], start=kc == 0, stop=kc == 7)
                P.act(vg[:], p_[:], AF.Gelu_apprx_tanh)
                P.act(junk[:, 0:512], vg[:], AF.Square, accum_out=ssv[:])
                P.act(rsv[:], ssv[:], AF.Sqrt, bias=epsc[:], scale=1.0 / 512)
                P.recip(rsv[:], rsv[:])
                P.stt(v_tok[:, tt, :], vg[:], rsv[:, 0:1], sgug[:], ALU.mult, ALU.mult)
                p1 = nps()
                for kc in range(8):
                    P.mm(p1[:], xnT[:, kc, tok], Win[:, kc, O_DV:O_DV + 512], start=kc == 0, stop=kc == 7)
                P.copy(Vst[:, tt, 128:640], p1[:])
                p2 = nps()
                for kc in range(8):
                    P.mm(p2[:, 0:128], xnT[:, kc, tok], Win[:, kc, O_CV:O_CV + 128], start=kc == 0, stop=kc == 7)
                for kc in range(8):
                    P.mm(p2[:, 128:136], xnT[:, kc, tok], Win[:, kc, O_DF:O_DF + 8], start=kc == 0, stop=kc == 7)
                P.act(Vst[:, tt, 0:128], p2[:, 0:128], AF.Copy)
                P.tt(xf[:], p2[:, 128:136], bfb[:], ALU.add)
                P.act(xf[:], xf[:], AF.Exp, scale=-1.0)
                P.act(xf[:], xf[:], AF.Ln, bias=1.0)
                P.ts(ls[:], xf[:], -1.0, None, op0=ALU.mult)
                p3 = nps()
                P.mm(p3[:, 0:8], tri_f[:], ls[:])
                P.mm(p3[:, 8:16], ones_f[:], ls[:])
                kt = 4 * g + tt
                P.tt(CK[:, kt, :], p3[:, 0:8], carry[:], ALU.add)
                P.tt(carry[:], p3[:, 8:16], carry[:], ALU.add)
                if tt == 1:
                    P.copy(CREF[:, g, :], carry[:])
                p4 = nps()
                for gc in range(4):
                    P.mm(p4[:, gc * 128:(gc + 1) * 128], v_tok[:, tt, gc * 128:(gc + 1) * 128], WsT[:, gc, :])
                P.tt(tmpa[:], p4[:, :].rearrange("p (a b) -> p a b", a=4), sgub[:, :].rearrange("p (a b) -> p a b", a=4), ALU.add)
                P.tt(oaT[:, :, tok], tmpa[:], uT[:, :, tok], ALU.mult, eng="pool")
                c = tt
                pr = nps(); P.mm(pr[:], Wa[:, c, :], xcb4[:, c, :])
                pi = nps(); P.mm(pi[:], Wx[:, c, :], xcb4[:, c, :])
                P.act(rr[:], pr[:], AF.Sigmoid, bias=bap[:, c:c + 1])
                P.act(ii[:], pi[:], AF.Sigmoid, bias=bxp[:, c:c + 1])
                P.act(aa[:], rr[:], AF.Exp, scale=cc[:, c:c + 1])
                P.tt(a2[:], aa[:], aa[:], ALU.mult, eng="pool")
                P.act(a2[:], a2[:], AF.Sqrt, bias=1.0, scale=-1.0)
                P.tt(inp[:], xc4[:, c, :], ii[:], ALU.mult)
                P.tt(inp[:], inp[:], a2[:], ALU.mult, eng="pool")
                P.scan(hh[:], aa[:], inp[:], hcar[:, c:c + 1], ALU.mult, ALU.add)
                P.copy(hcar[:, c:c + 1], hh[:, T - 1:T])
                P.tt(obT[:, c, :], hh[:], gy[:, c, :], ALU.mult, eng="pool")
            gs = slice(g * T, (g + 1) * T)
            P.dma(OA[:, :, gs], oaT[:], writes=[("OA", g)])
            P.dma(OB[:, :, gs], obT[:], writes=[("OB", g)])
            P.dma(QT[:, :, gs], QTst[:], writes=[("QT", g)])
            P.dma(KT[:, :, gs], KTst[:], writes=[("KT", g)])
            P.dma(VV[:, 4 * g:4 * g + 4, :], Vst[:], writes=[("VV", g)])
        P.barrier()
        P.release(mk)

    def phase_B(l):
        mk = P.mark()
        if l == 0 and nlayers > 1:
            for e in range(8):
                prep_moe(e)
        mexp = P.sb("mexp", [128, 8, 2, 128]); P.dma(mexp[:], mexp_d)
        snk = P.sb("snk", [128, 8]); P.dma(snk[:], sinks_b[:, l, :])
        P.act(snk[:], snk[:], AF.Exp)
        qh = [P.sb(f"qh{i}", [64, S], BF16) for i in range(2)]
        kh = [P.sb(f"kh{i}", [64, S], BF16) for i in range(2)]
        vh = [P.sb(f"vh{i}", [128, 64, 65], BF16) for i in range(2)]
        for i in range(2):
            P.memset(vh[i][:, :, 64:65], 1.0)
        biasgs = [P.sb(f"biasg{i}", [128, 64]) for i in range(2)]
        pts = [P.sb(f"pt{i}", [128, 512], BF16) for i in range(6)]
        pfs = [P.sb(f"pf{i}", [128, 512]) for i in range(4)]
        dens = [P.sb(f"den{i}", [128, 4]) for i in range(2)]; ods = [P.sb(f"od{i}", [128, 4, 64], BF16) for i in range(2)]
        cnt = 0
        allg = list(range(ng))
        def load_head(hd):
            fox = hd >= 8
            h = hd % 8
            b = hd % 2
            q_, k_, v_ = qh[b], kh[b], vh[b]
            Sg = ng * T
            if fox:
                P.dma(q_[:, 0:Sg], QT[(h % 2) * 64:(h % 2) * 64 + 64, 4 + h // 2, 0:Sg], reads=[("QT", g) for g in allg])
                P.dma(k_[:, 0:Sg], KT[(h % 2) * 64:(h % 2) * 64 + 64, 1 + h // 2, 0:Sg], reads=[("KT", g) for g in allg])
                voff = 128 + h * 64
            else:
                kv = h // 4
                P.dma(q_[:, 0:Sg], QT[(h % 2) * 64:(h % 2) * 64 + 64, h // 2, 0:Sg], reads=[("QT", g) for g in allg])
                P.dma(k_[:, 0:Sg], KT[kv * 64:kv * 64 + 64, 0, 0:Sg], reads=[("KT", g) for g in allg])
                voff = kv * 64
            for k0 in range(0, 4 * ng, 16):
                k1 = min(4 * ng, k0 + 16)
                P.dma(v_[:, k0:k1, 0:64], VV[:, k0:k1, voff:voff + 64], reads=[("VV", g) for g in allg])

        load_head(0)
        for hd in range(16):
            fox = hd >= 8
            h = hd % 8
            b = hd % 2
            q_, k_, v_ = qh[b], kh[b], vh[b]
            if hd + 1 < 16:
                load_head(hd + 1)
            items = []
            for g in range(ng):
                kts = list(range(0, 4 * g + 4)) if fox else list(range(max(0, 4 * g - 1), 4 * g + 4))
                for kt in kts:
                    items.append((g, kt, kts))
            state = {}

            def stage1(it):
                g, kt, kts = it
                if kt == kts[0] and fox:
                    bg_ = biasgs[g % 2]
                    P.ts(bg_[:, 0:4 * g + 4], CK[:, 0:4 * g + 4, h], -1.0, CREF[:, g, h:h + 1], op0=ALU.mult, op1=ALU.add)
                j = kt - 4 * g
                lo = max(0, j)
                hi = 3 if fox else min(3, j + 1)
                cs = slice(lo * 128, (hi + 1) * 128)
                p_ = nps(4)
                P.mm(p_[:, cs], k_[:, kt * 128:(kt + 1) * 128], q_[:, g * T + lo * 128:g * T + (hi + 1) * 128])
                state["cnt"] = state.get("cnt", 0) + 1
                pt = pts[state["cnt"] % 6]
                if fox:
                    P.act(pt[:, cs], p_[:, cs], AF.Exp, bias=biasgs[g % 2][:, kt:kt + 1], scale=0.125)
                    if j >= 0:
                        P.tt(pt[:, j * 128:(j + 1) * 128], pt[:, j * 128:(j + 1) * 128], tri_b[:], ALU.mult)
                else:
                    pf = pfs[state["cnt"] % 4]
                    P.act(pf[:, cs], p_[:, cs], AF.Exp, scale=0.125)
                    for qs in range(lo, hi + 1):
                        idx = 0 if kt == 4 * g + qs else 1
                        P.tt(pt[:, qs * 128:(qs + 1) * 128], pf[:, qs * 128:(qs + 1) * 128], mexp[:, h, idx, :], ALU.mult)
                return (pt, lo, hi)

            def stage2(it, s1):
                g, kt, kts = it
                pt, lo, hi = s1
                po = ps[4 + (g % 2)]
                pov = po[:, 0:260].rearrange("p (a b) -> p a b", a=4)
                for qs in range(lo, hi + 1):
                    last = 4 * g + qs
                    P.mm(pov[:, qs, :], pt[:, qs * 128:(qs + 1) * 128], v_[:, kt, :],
                         start=(kt == kts[0] and qs == lo), stop=kt == last, sgc=True)
                if kt == kts[-1]:
                    od = ods[g % 2]
                    dn = dens[g % 2]
                    if fox:
                        P.copy(dn[:], pov[:, :, 64])
                    else:
                        P.ts(dn[:], pov[:, :, 64], snk[:, h:h + 1], None, op0=ALU.add)
                    P.recip(dn[:], dn[:])
                    for qs in range(4):
                        P.ts(od[:, qs, :], pov[:, qs, 0:64], dn[:, qs:qs + 1], None, op0=ALU.mult)
                    dst = OD if fox else OC
                    P.dma(dst[:, 4 * g:4 * g + 4, h * 64:(h + 1) * 64], od[:], writes=[("OD" if fox else "OC", g, h)])

            LOOK = 3
            s1res = {}
            n_it = len(items)
            for i in range(n_it + LOOK):
                if i < n_it:
                    s1res[i] = stage1(items[i])
                if i - LOOK >= 0:
                    stage2(items[i - LOOK], s1res.pop(i - LOOK))
        P.barrier()
        P.release(mk)

    def phase_C(l, hsrc, hname):
        mk = P.mark()
        Wg = P.sb("Wg", [128, 8, 4096], BF16)
        for br in range(4):
            for kc in range(0, 8, 4):
                P.dma(Wg[:, kc:kc + 4, br * 1024:(br + 1) * 1024],
                      w_gate[l, br, kc * 128:(kc + 4) * 128, :].rearrange("(k p) n -> p k n", p=128), eng="pool")
        Wb = P.sb("Wb", [128, 4, 4, D], BF16)
        for br in range(4):
            P.dma(Wb[:, br], w_branch[l, br].rearrange("(k p) n -> p k n", p=128), eng="pool")
        Wo = P.sb("Wo", [128, 8, D], BF16)
        for kc in range(0, 8, 4):
            P.dma(Wo[:, kc:kc + 4, :], w_out[l, kc * 128:(kc + 4) * 128, :].rearrange("(k p) n -> p k n", p=128), eng="pool")
        bg = P.sb("bg", [128, 4, 8]); P.dma(bg[:], bgate_p[:, l])
        xnT = P.sb("xnT", [128, 8, T], BF16)
        brT = [P.sb(f"brT{i}", [128, 4, T], BF16) for i in range(2)]
        brX = [[P.sb(f"brX{j}_{i}", [128, 4, T], BF16) for i in range(2)] for j in range(2)]
        otokX = [[P.sb(f"otok{j}_{i}", [128, 4, 512], BF16) for i in range(2)] for j in range(2)]

        def prep_cd(g):
            par = g % 2
            P.dma(otokX[par][0][:], OC[:, 4 * g:4 * g + 4, :], reads=[("OC", g, h) for h in range(8)])
            P.dma(otokX[par][1][:], OD[:, 4 * g:4 * g + 4, :], reads=[("OD", g, h) for h in range(8)])
            for i in range(2):
                for tt in range(4):
                    pb = npb()
                    for c in range(4):
                        P.tr(pb[:, c * 128:(c + 1) * 128], otokX[par][i][:, tt, c * 128:(c + 1) * 128], ident[:])
                    P.copy(brX[par][i][:, :, tt * 128:(tt + 1) * 128], pb[:, 0:512].rearrange("p (a b) -> p a b", a=4))
        mT = P.sb("mT", [128, 8, T], BF16)
        gate = [P.sb(f"gate{i}", [128, T]) for i in range(2)]
        macc = P.sb("macc", [128, T]); mtmp = [P.sb(f"mtmp{i}", [128, T]) for i in range(2)]
        hT = P.sb("hT", [128, 4, D])
        for g in range(ng):
            gs = slice(g * T, (g + 1) * T)
            P.dma(xnT[:], XNT[:, :, gs], reads=[("XNT", g)])
            P.dma(brT[0][:], OA[:, :, gs], reads=[("OA", g)])
            P.dma(brT[1][:], OB[:, :, gs], reads=[("OB", g)])
            load_h(hT, hsrc, hname, g)
            if g == 0:
                prep_cd(0)
            brs = [brT[0], brT[1], brX[g % 2][0], brX[g % 2][1]]
            for oc in range(8):
                if oc == 4 and g + 1 < ng:
                    prep_cd(g + 1)
                for br in range(4):
                    pg = nps()
                    for kc in range(8):
                        P.mm(pg[:], Wg[:, kc, br * 1024 + oc * 128:br * 1024 + (oc + 1) * 128], xnT[:, kc, :], start=kc == 0, stop=kc == 7)
                    pr = nps()
                    for kc in range(4):
                        P.mm(pr[:], Wb[:, br, kc, oc * 128:(oc + 1) * 128], brs[br][:, kc, :], start=kc == 0, stop=kc == 3)
                    gt = gate[br % 2]
                    P.act(gt[:], pg[:], AF.Sigmoid, bias=bg[:, br, oc:oc + 1])
                    if br == 0:
                        P.tt(macc[:], gt[:], pr[:], ALU.mult)
                    else:
                        mt_ = mtmp[br % 2]
                        P.tt(mt_[:], gt[:], pr[:], ALU.mult)
                        if br < 3:
                            P.tt(macc[:], macc[:], mt_[:], ALU.add, eng="pool")
                        else:
                            P.tt(mT[:, oc, :], macc[:], mt_[:], ALU.add, eng="pool")
            for tt in range(4):
                for half in range(2):
                    p_ = nps()
                    for kc in range(8):
                        P.mm(p_[:], mT[:, kc, tt * 128:(tt + 1) * 128], Wo[:, kc, half * 512:(half + 1) * 512], start=kc == 0, stop=kc == 7)
                    P.tt(hT[:, tt, half * 512:(half + 1) * 512], hT[:, tt, half * 512:(half + 1) * 512], p_[:], ALU.add)
            P.dma(H1[gs, :].rearrange("(t p) d -> p t d", p=128), hT[:], writes=[("H1", g)], eng="pool")
        P.barrier()
        P.release(mk)

    def phase_D(l, moe, last):
        mk = P.mark()
        gx = P.sb("gx", [128, D]); P.dma(gx[:], g_cross[:, l, :])
        gf = P.sb("gf", [128, D]); P.dma(gf[:], g_ffn[:, l, :])
        Wq = P.sb("Wq", [128, 8, 512], BF16)
        P.dma(Wq[:], wq_c[l].rearrange("(k p) n -> p k n", p=128), eng="pool")
        Woc = P.sb("Woc", [128, 4, D], BF16)
        P.dma(Woc[:], wo_c[l].rearrange("(k p) n -> p k n", p=128), eng="pool")
        kxT = P.sb("kxT", [128, 4, 256], BF16); vx = P.sb("vx", [128, 2, 512], BF16)
        hT = P.sb("hT", [128, 4, D]); xn_tok = P.sb("xn_tok", [128, 4, D], BF16); xT = P.sb("xT", [128, 8, T], BF16)
        junk = P.sb("junk", [128, D]); ss = P.sb("ss", [128, 4]); rstd = P.sb("rstd", [128, 4])
        mk2 = P.mark()
        gm = P.sb("gm", [128, D]); P.dma(gm[:], g_mem[:, l, :])
        Wkv = P.sb("Wkv", [128, 8, D], BF16)
        for kc in range(0, 8, 4):
            P.dma(Wkv[:, kc:kc + 4, :], wkv_c[l, kc * 128:(kc + 4) * 128, :].rearrange("(k p) n -> p k n", p=128), eng="pool")
        mt_ = P.sb("memt", [128, 2, D]); P.dma(mt_[:], mem.rearrange("(t p) d -> p t d", p=128))
        mn = P.sb("memn", [128, 2, D], BF16); memT = P.sb("memT", [128, 8, 256], BF16)
        for t2 in range(2):
            P.act(junk[:], mt_[:, t2, :], AF.Square, accum_out=ss[:, t2:t2 + 1])
        P.act(rstd[:, 0:2], ss[:, 0:2], AF.Sqrt, bias=epsc[:], scale=1.0 / D)
        P.recip(rstd[:, 0:2], rstd[:, 0:2])
        for t2 in range(2):
            P.stt(mn[:, t2, :], mt_[:, t2, :], rstd[:, t2:t2 + 1], gm[:], ALU.mult, ALU.mult)
            pb = npb()
            for kc in range(8):
                P.tr(pb[:, kc * 128:(kc + 1) * 128], mn[:, t2, kc * 128:(kc + 1) * 128], ident[:])
            P.copy(memT[:, :, t2 * 128:(t2 + 1) * 128], pb[:, :].rearrange("p (a b) -> p a b", a=8))
        for hd in range(4):
            p_ = nps()
            for kc in range(8):
                P.mm(p_[:, 0:256], Wkv[:, kc, hd * 128:(hd + 1) * 128], memT[:, kc, :], start=kc == 0, stop=kc == 7)
            P.copy(kxT[:, hd, :], p_[:, 0:256])
        for t2 in range(2):
            p_ = nps()
            for kc in range(8):
                P.mm(p_[:], memT[:, kc, t2 * 128:(t2 + 1) * 128], Wkv[:, kc, 512:1024], start=kc == 0, stop=kc == 7)
            P.copy(vx[:, t2, :], p_[:])
        P.barrier()
        P.release(mk2)
        qxT = P.sb("qxT", [128, 4, T], BF16); ptm = [P.sb(f"ptm{i}", [128, T], BF16) for i in range(2)]
        rden = P.sb("rden", [128, T]); oxT = P.sb("oxT", [128, 4, T], BF16)
        hid = P.sb("hid", [128, NFC, T], BF16)
        w1g = [P.sb(f"w1g{i}", [128, 8, 512], BF16) for i in range(2)]
        w1u = [P.sb(f"w1u{i}", [128, 8, 512], BF16) for i in range(2)]
        w2h = [P.sb(f"w2h{i}", [128, NFC, 512], BF16) for i in range(2)]
        sg = [P.sb(f"sg{i}", [128, T]) for i in range(2)]
        ne = 8 if moe else 1
        if moe:
            Wr = P.sb("Wr", [128, 8, 8], BF16)
            P.dma(Wr[:], router_w[0].rearrange("(k p) n -> p k n", p=128), eng="pool")
            rb = P.sb("rb", [128, 8]); P.dma(rb[:], rb_b)
            lg = P.sb("lg", [128, 8]); lg2 = P.sb("lg2", [128, 8]); eq1 = P.sb("eq1", [128, 8]); eq2 = P.sb("eq2", [128, 8])
            m1 = P.sb("m1", [128, 1]); m2 = P.sb("m2", [128, 1]); dd = P.sb("dd", [128, 1]); w1_ = P.sb("w1_", [128, 1]); w2_ = P.sb("w2_", [128, 1])
            comb = P.sb("comb", [128, 4, 8])
        if last:
            gfin = P.sb("gfin", [128, D]); P.dma(gfin[:], g_final)
            yo = P.sb("yo", [128, 4, D])
        slab_i = 0
        hTs = [hT, P.sb("hT2", [128, 4, D])]
        for g in range(ng):
            gs = slice(g * T, (g + 1) * T)
            hT = hTs[g % 2]
            if g == 0:
                load_h(hT, H1, "H1", g)
                rms_to_T(hT, gx[:], xn_tok, xT, junk, ss, rstd)
            if g + 1 < ng:
                load_h(hTs[(g + 1) % 2], H1, "H1", g + 1)
            for hd in range(4):
                p_ = nps()
                for kc in range(8):
                    P.mm(p_[:], Wq[:, kc, hd * 128:(hd + 1) * 128], xT[:, kc, :], start=kc == 0, stop=kc == 7)
                if hd % 2 == 0:
                    P.copy(qxT[:, hd, :], p_[:])
                else:
                    P.act(qxT[:, hd, :], p_[:], AF.Copy)
            for hd in range(4):
                po = nps(); pd = nps()
                for m_ in range(2):
                    p_ = nps()
                    P.mm(p_[:], kxT[:, hd, m_ * 128:(m_ + 1) * 128], qxT[:, hd, :])
                    pt = ptm[m_]
                    P.act(pt[:], p_[:], AF.Exp, scale=128 ** -0.5)
                    P.mm(po[:], vx[:, m_, hd * 128:(hd + 1) * 128], pt[:], start=m_ == 0, stop=m_ == 1)
                    P.mm(pd[:], ones_b[:], pt[:], start=m_ == 0, stop=m_ == 1)
                P.recip(rden[:], pd[:])
                P.tt(oxT[:, hd, :], po[:], rden[:], ALU.mult)
            for tt in range(4):
                for half in range(2):
                    p_ = nps()
                    for hd in range(4):
                        P.mm(p_[:], oxT[:, hd, tt * 128:(tt + 1) * 128], Woc[:, hd, half * 512:(half + 1) * 512], start=hd == 0, stop=hd == 3)
                    P.tt(hT[:, tt, half * 512:(half + 1) * 512], hT[:, tt, half * 512:(half + 1) * 512], p_[:], ALU.add)
            rms_to_T(hT, gf[:], xn_tok, xT, junk, ss, rstd)
            if moe:
                for tt in range(4):
                    p_ = nps()
                    for kc in range(8):
                        P.mm(p_[:, 0:8], xT[:, kc, tt * 128:(tt + 1) * 128], Wr[:, kc, :], start=kc == 0, stop=kc == 7)
                    P.tt(lg[:], p_[:, 0:8], rb[:], ALU.add)
                    P.rmax(m1[:], lg[:])
                    P.ts(eq1[:], lg[:], m1[:, 0:1], None, op0=ALU.is_equal)
                    P.stt(lg2[:], eq1[:], -1e30, lg[:], ALU.mult, ALU.add)
                    P.rmax(m2[:], lg2[:])
                    P.ts(eq2[:], lg2[:], m2[:, 0:1], None, op0=ALU.is_equal)
                    P.tt(dd[:], m2[:], m1[:], ALU.subtract)
                    P.act(w2_[:], dd[:], AF.Sigmoid)
                    P.act(w1_[:], dd[:], AF.Sigmoid, scale=-1.0)
                    P.ts(eq1[:], eq1[:], w1_[:, 0:1], None, op0=ALU.mult)
                    P.stt(comb[:, tt, :], eq2[:], w2_[:, 0:1], eq1[:], ALU.mult, ALU.add)
            for e in range(ne):
                ei = (1 + e) if moe else 0
                w13 = W13B[ei]; w2 = W2B[ei]
                for sbi in range(6):
                    nfc = 4 if sbi < 5 else 2
                    bi = slab_i % 2; slab_i += 1
                    P.dma(w1g[bi][:, :, 0:nfc * 128], w13[:, sbi * 512:sbi * 512 + nfc * 128].rearrange("(k p) n -> p k n", p=128), reads=[f"W13B{ei}"])
                    P.dma(w1u[bi][:, :, 0:nfc * 128], w13[:, DFF + sbi * 512:DFF + sbi * 512 + nfc * 128].rearrange("(k p) n -> p k n", p=128), reads=[f"W13B{ei}"])
                    if sbi == 0:
                        for half in range(2):
                            P.dma(w2h[half][:], w2[:, half * 512:(half + 1) * 512].rearrange("(k p) n -> p k n", p=128), reads=[f"W2B{ei}"])
                    for fl in range(nfc):
                        fc = sbi * 4 + fl
                        pg = nps(); pu = nps()
                        for kc in range(8):
                            P.mm(pg[:], w1g[bi][:, kc, fl * 128:(fl + 1) * 128], xT[:, kc, :], start=kc == 0, stop=kc == 7)
                        for kc in range(8):
                            P.mm(pu[:], w1u[bi][:, kc, fl * 128:(fl + 1) * 128], xT[:, kc, :], start=kc == 0, stop=kc == 7)
                        s_ = sg[fc % 2]
                        P.act(s_[:], pg[:], AF.Silu)
                        P.tt(hid[:, fc, :], s_[:], pu[:], ALU.mult)
                if e == ne - 1 and g + 1 < ng:
                    rms_to_T(hTs[(g + 1) % 2], gx[:], xn_tok, xT, junk, ss, rstd)
                for half in range(2):
                    for tt in range(4):
                        p_ = nps()
                        for fc in range(NFC):
                            P.mm(p_[:], hid[:, fc, tt * 128:(tt + 1) * 128], w2h[half][:, fc, :], start=fc == 0, stop=fc == NFC - 1)
                        hv = hT[:, tt, half * 512:(half + 1) * 512]
                        if moe:
                            P.stt(hv, p_[:], comb[:, tt, e:e + 1], hv, ALU.mult, ALU.add)
                        else:
                            P.tt(hv, hv, p_[:], ALU.add)
            if last:
                for tt in range(4):
                    P.act(junk[:], hT[:, tt, :], AF.Square, accum_out=ss[:, tt:tt + 1])
                P.act(rstd[:], ss[:], AF.Sqrt, bias=epsc[:], scale=1.0 / D)
                P.recip(rstd[:], rstd[:])
                for tt in range(4):
                    P.stt(yo[:, tt, :], hT[:, tt, :], rstd[:, tt:tt + 1], gfin[:], ALU.mult, ALU.mult, eng="dve" if tt % 2 == 0 else "pool")
                P.dma(y[gs, :].rearrange("(t p) d -> p t d", p=128), yo[:], writes=[("y", g)])
            else:
                P.dma(H3[gs, :].rearrange("(t p) d -> p t d", p=128), hT[:], writes=[("H3", g)])
        P.barrier()
        P.release(mk)

    def phase_D_moe(l):
        mk = P.mark()
        nkt = 4 * ng
        gx = P.sb("gx", [128, D]); P.dma(gx[:], g_cross[:, l, :])
        gf = P.sb("gf", [128, D]); P.dma(gf[:], g_ffn[:, l, :])
        EQ1 = P.sb("EQ1", [128, 64, 8]); EQ2 = P.sb("EQ2", [128, 64, 8])
        RK1 = P.sb("RK1", [128, 64]); RK2 = P.sb("RK2", [128, 64]); WW1 = P.sb("WW1", [128, 64]); WW2 = P.sb("WW2", [128, 64])
        carryM = P.sb("carryM", [128, 8]); P.memset(carryM[:], 0.0)
        SL1 = P.sb("SL1", [128, 64], I32); SL2 = P.sb("SL2", [128, 64], I32); IDXW = P.sb("IDXW", [128, NBLK], I32)
        pidx = P.sb("pidx", [128, 1]); P.dma(pidx[:], pidx_d)
        junk = P.sb("junk", [128, D]); ss = P.sb("ss", [128, 4]); rstd = P.sb("rstd", [128, 4])
        mk1 = P.mark()
        Wq = P.sb("Wq", [128, 8, 512], BF16)
        P.dma(Wq[:], wq_c[l].rearrange("(k p) n -> p k n", p=128), eng="pool")
        Woc = P.sb("Woc", [128, 4, D], BF16)
        P.dma(Woc[:], wo_c[l].rearrange("(k p) n -> p k n", p=128), eng="pool")
        kxT = P.sb("kxT", [128, 4, 256], BF16); vx = P.sb("vx", [128, 2, 512], BF16)
        hT = P.sb("hT", [128, 4, D]); xn_tok = P.sb("xn_tok", [128, 4, D], BF16); xT = P.sb("xT", [128, 8, T], BF16)
        zt = P.sb("zt", [128, 4, D], BF16); P.memset(zt[:], 0.0, eng="pool")
        for b in range(NBLK):
            P.dma(XS[b * 512:(b + 1) * 512, :].rearrange("(t p) d -> p t d", p=128), zt[:], writes=[("XS", b)])
        mk2 = P.mark()
        gm = P.sb("gm", [128, D]); P.dma(gm[:], g_mem[:, l, :])
        Wkv = P.sb("Wkv", [128, 8, D], BF16)
        for kc in range(0, 8, 4):
            P.dma(Wkv[:, kc:kc + 4, :], wkv_c[l, kc * 128:(kc + 4) * 128, :].rearrange("(k p) n -> p k n", p=128), eng="pool")
        mt_ = P.sb("memt", [128, 2, D]); P.dma(mt_[:], mem.rearrange("(t p) d -> p t d", p=128))
        mn = P.sb("memn", [128, 2, D], BF16); memT = P.sb("memT", [128, 8, 256], BF16)
        for t2 in range(2):
            P.act(junk[:], mt_[:, t2, :], AF.Square, accum_out=ss[:, t2:t2 + 1])
        P.act(rstd[:, 0:2], ss[:, 0:2], AF.Sqrt, bias=epsc[:], scale=1.0 / D)
        P.recip(rstd[:, 0:2], rstd[:, 0:2])
        for t2 in range(2):
            P.stt(mn[:, t2, :], mt_[:, t2, :], rstd[:, t2:t2 + 1], gm[:], ALU.mult, ALU.mult)
            pb = npb()
            for kc in range(8):
                P.tr(pb[:, kc * 128:(kc + 1) * 128], mn[:, t2, kc * 128:(kc + 1) * 128], ident[:])
            P.copy(memT[:, :, t2 * 128:(t2 + 1) * 128], pb[:, :].rearrange("p (a b) -> p a b", a=8))
        for hd in range(4):
            p_ = nps()
            for kc in range(8):
                P.mm(p_[:, 0:256], Wkv[:, kc, hd * 128:(hd + 1) * 128], memT[:, kc, :], start=kc == 0, stop=kc == 7)
            P.copy(kxT[:, hd, :], p_[:, 0:256])
        for t2 in range(2):
            p_ = nps()
            for kc in range(8):
                P.mm(p_[:], memT[:, kc, t2 * 128:(t2 + 1) * 128], Wkv[:, kc, 512:1024], start=kc == 0, stop=kc == 7)
            P.copy(vx[:, t2, :], p_[:])
        P.barrier()
        P.release(mk2)
        qxT = P.sb("qxT", [128, 4, T], BF16); ptm = [P.sb(f"ptm{i}", [128, T], BF16) for i in range(2)]
        rden = P.sb("rden", [128, T]); oxT = P.sb("oxT", [128, 4, T], BF16)
        Wr = P.sb("Wr", [128, 8, 8], BF16)
        P.dma(Wr[:], router_w[0].rearrange("(k p) n -> p k n", p=128), eng="pool")
        rb = P.sb("rb", [128, 8]); P.dma(rb[:], rb_b)
        lg = P.sb("lg", [128, 8]); lg2 = P.sb("lg2", [128, 8]); msk = P.sb("msk", [128, 8]); rk = P.sb("rk", [128, 8]); t8 = P.sb("t8", [128, 8])
        m1 = P.sb("m1", [128, 1]); m2 = P.sb("m2", [128, 1]); dd = P.sb("dd", [128, 1])
        for g in range(ng):
            gs = slice(g * T, (g + 1) * T)
            load_h(hT, H1, "H1", g)
            rms_to_T(hT, gx[:], xn_tok, xT, junk, ss, rstd)
            for hd in range(4):
                p_ = nps()
                for kc in range(8):
                    P.mm(p_[:], Wq[:, kc, hd * 128:(hd + 1) * 128], xT[:, kc, :], start=kc == 0, stop=kc == 7)
                if hd % 2 == 0:
                    P.copy(qxT[:, hd, :], p_[:])
                else:
                    P.act(qxT[:, hd, :], p_[:], AF.Copy)
            for hd in range(4):
                po = nps(); pd = nps()
                for m_ in range(2):
                    p_ = nps()
                    P.mm(p_[:], kxT[:, hd, m_ * 128:(m_ + 1) * 128], qxT[:, hd, :])
                    pt = ptm[m_]
                    P.act(pt[:], p_[:], AF.Exp, scale=128 ** -0.5)
                    P.mm(po[:], vx[:, m_, hd * 128:(hd + 1) * 128], pt[:], start=m_ == 0, stop=m_ == 1)
                    P.mm(pd[:], ones_b[:], pt[:], start=m_ == 0, stop=m_ == 1)
                P.recip(rden[:], pd[:])
                P.tt(oxT[:, hd, :], po[:], rden[:], ALU.mult)
            for tt in range(4):
                for half in range(2):
                    p_ = nps()
                    for hd in range(4):
                        P.mm(p_[:], oxT[:, hd, tt * 128:(tt + 1) * 128], Woc[:, hd, half * 512:(half + 1) * 512], start=hd == 0, stop=hd == 3)
                    P.tt(hT[:, tt, half * 512:(half + 1) * 512], hT[:, tt, half * 512:(half + 1) * 512], p_[:], ALU.add)
            P.dma(H3[gs, :].rearrange("(t p) d -> p t d", p=128), hT[:], writes=[("H3", g)])
            rms_to_T(hT, gf[:], xn_tok, xT, junk, ss, rstd)
            P.dma(XN2[gs, :].rearrange("(t p) d -> p t d", p=128), xn_tok[:], writes=[("XN2", g)])
            for tt in range(4):
                kt = 4 * g + tt
                p_ = nps()
                for kc in range(8):
                    P.mm(p_[:, 0:8], xT[:, kc, tt * 128:(tt + 1) * 128], Wr[:, kc, :], start=kc == 0, stop=kc == 7)
                P.tt(lg[:], p_[:, 0:8], rb[:], ALU.add)
                P.rmax(m1[:], lg[:])
                P.ts(EQ1[:, kt, :], lg[:], m1[:, 0:1], None, op0=ALU.is_equal)
                P.stt(lg2[:], EQ1[:, kt, :], -1e30, lg[:], ALU.mult, ALU.add)
                P.rmax(m2[:], lg2[:])
                P.ts(EQ2[:, kt, :], lg2[:], m2[:, 0:1], None, op0=ALU.is_equal)
                P.tt(dd[:], m2[:], m1[:], ALU.subtract)
                P.act(WW2[:, kt:kt + 1], dd[:], AF.Sigmoid)
                P.act(WW1[:, kt:kt + 1], dd[:], AF.Sigmoid, scale=-1.0)
                P.tt(msk[:], EQ1[:, kt, :], EQ2[:, kt, :], ALU.add)
                p3 = nps()
                P.mm(p3[:, 0:8], tri_f[:], msk[:])
                P.mm(p3[:, 8:16], ones_f[:], msk[:])
                P.tt(rk[:], p3[:, 0:8], carryM[:], ALU.add)
                P.tt(rk[:], rk[:], msk[:], ALU.subtract)
                P.tt(carryM[:], p3[:, 8:16], carryM[:], ALU.add)
                P.tt(t8[:], EQ1[:, kt, :], rk[:], ALU.mult)
                P.rsum(RK1[:, kt:kt + 1], t8[:])
                P.tt(t8[:], EQ2[:, kt, :], rk[:], ALU.mult)
                P.rsum(RK2[:, kt:kt + 1], t8[:])
        P.barrier()
        P.release(mk1)
        nblk = P.sb("nblk", [128, 8]); pad = P.sb("pad", [128, 8]); incl = P.sb("incl", [128, 8]); base = P.sb("base", [128, 8])
        t8 = P.sb("t8b", [128, 8]); eb = P.sb("eb", [128, NBLK]); sf1 = P.sb("sf1", [128, 64]); sf2 = P.sb("sf2", [128, 64])
        P.memset(nblk[:], 0.0)
        for j in range(16):
            P.stt(nblk[:], carryM[:], float(512 * j), nblk[:], ALU.is_gt, ALU.add)
        P.ts(pad[:], nblk[:], 512.0, None, op0=ALU.mult)
        P.scan(incl[:], ones_f[:, 0:8], pad[:], 0.0, ALU.mult, ALU.add)
        P.tt(base[:], incl[:], pad[:], ALU.subtract)
        for b in range(NBLK):
            P.ts(t8[:], incl[:], float(512 * b), None, op0=ALU.is_le)
            P.rsum(eb[:, b:b + 1], t8[:])
        P.ts(eb[:], eb[:], 7.0, None, op0=ALU.min)
        P.ts(eb[:], eb[:], 128.0, pidx[:, 0:1], op0=ALU.mult, op1=ALU.add)
        P.copy(IDXW[:], eb[:])
        P.copy(sf1[:], RK1[:]); P.copy(sf2[:], RK2[:])
        for e in range(8):
            P.stt(sf1[:, 0:nkt], EQ1[:, 0:nkt, e], base[:, e:e + 1], sf1[:, 0:nkt], ALU.mult, ALU.add)
            P.stt(sf2[:, 0:nkt], EQ2[:, 0:nkt, e], base[:, e:e + 1], sf2[:, 0:nkt], ALU.mult, ALU.add)
        P.copy(SL1[:], sf1[:]); P.copy(SL2[:], sf2[:])
        xr = [P.sb(f"xr{i}", [128, D], BF16) for i in range(2)]
        XSB = NBLK * 512 - 1
        for kt in range(nkt):
            x_ = xr[kt % 2]
            P.dma(x_[:], XN2[kt * 128:(kt + 1) * 128, :], reads=[("XN2", kt // 4)])
            for SL in (SL1, SL2):
                P.op("pool", lambda e, x_=x_, SL=SL, kt=kt: e.indirect_dma_start(
                    out=XS, out_offset=bass.IndirectOffsetOnAxis(ap=SL[:, kt:kt + 1], axis=0), in_=x_[:], in_offset=None,
                    bounds_check=None), [x_, SL] + [("XS", b) for b in range(NBLK)], ["XSs"], dmakey=("dma", f"xr{kt % 2}"))
        P.barrier()
        mkb = P.mark()
        xs_toks = [P.sb(f"xs_tok{i}", [128, 4, D], BF16) for i in range(2)]; xTs = [P.sb(f"xTb{i}", [128, 8, T], BF16) for i in range(2)]

        def prep_blk(b):
            xs_tok = xs_toks[b % 2]; xT = xTs[b % 2]
            P.dma(xs_tok[:], XS[b * 512:(b + 1) * 512, :].rearrange("(t p) d -> p t d", p=128), reads=["XSs"])
            for tt in range(4):
                pb = npb()
                for kc in range(8):
                    P.tr(pb[:, kc * 128:(kc + 1) * 128], xs_tok[:, tt, kc * 128:(kc + 1) * 128], ident[:])
                src = pb[:, :].rearrange("p (a b) -> p a b", a=8)
                dst = xT[:, :, tt * 128:(tt + 1) * 128]
                if tt % 2 == 0:
                    P.copy(dst, src)
                else:
                    P.act(dst, src, AF.Copy)
        hid = P.sb("hid", [128, NFC, T], BF16)
        w1g = [P.sb(f"w1g{i}", [128, 8, 512], BF16) for i in range(2)]
        w1u = [P.sb(f"w1u{i}", [128, 8, 512], BF16) for i in range(2)]
        w2h = [P.sb(f"w2h{i}", [128, NFC, 512], BF16) for i in range(2)]
        w1gL = P.sb("w1gL", [128, 8, 256], BF16); w1uL = P.sb("w1uL", [128, 8, 256], BF16)
        sg = [P.sb(f"sg{i}", [128, T]) for i in range(2)]
        yo = P.sb("yo", [128, 4, D])
        slab_i = 0

        def wgather(dst, src, b, key):
            P.op("pool", lambda e: e.indirect_dma_start(
                out=dst, out_offset=None, in_=src, in_offset=bass.IndirectOffsetOnAxis(ap=IDXW[:, b:b + 1], axis=0),
                bounds_check=None), [IDXW, "WP"], [dst], dmakey=("dma", key))
        prep_blk(0)
        for b in range(NBLK):
            xT = xTs[b % 2]
            for sbi in range(6):
                if sbi == 4 and b + 1 < NBLK:
                    prep_blk(b + 1)
                nfc = 4 if sbi < 5 else 2
                bi = slab_i % 2; slab_i += 1
                if sbi < 5:
                    g_t, u_t, kg, ku = w1g[bi], w1u[bi], f"w1g{bi}", f"w1u{bi}"
                else:
                    g_t, u_t, kg, ku = w1gL, w1uL, "w1gL", "w1uL"
                wgather(g_t[:].rearrange("p k n -> p (k n)"), WSL[(sbi, 0)].rearrange("r k n -> r (k n)"), b, kg)
                wgather(u_t[:].rearrange("p k n -> p (k n)"), WSL[(sbi, 1)].rearrange("r k n -> r (k n)"), b, ku)
                if sbi == 0:
                    for half in range(2):
                        wgather(w2h[half][:].rearrange("p k n -> p (k n)"), W2H[half].rearrange("r k n -> r (k n)"), b, f"w2h{half}")
                for fl in range(nfc):
                    fc = sbi * 4 + fl
                    pg = nps(); pu = nps()
                    for kc in range(8):
                        P.mm(pg[:], g_t[:, kc, fl * 128:(fl + 1) * 128], xT[:, kc, :], start=kc == 0, stop=kc == 7)
                    for kc in range(8):
                        P.mm(pu[:], u_t[:, kc, fl * 128:(fl + 1) * 128], xT[:, kc, :], start=kc == 0, stop=kc == 7)
                    s_ = sg[fc % 2]
                    P.act(s_[:], pg[:], AF.Silu)
                    P.tt(hid[:, fc, :], s_[:], pu[:], ALU.mult)
            for half in range(2):
                for tt in range(4):
                    p_ = nps()
                    for fc in range(NFC):
                        P.mm(p_[:], hid[:, fc, tt * 128:(tt + 1) * 128], w2h[half][:, fc, :], start=fc == 0, stop=fc == NFC - 1)
                    if tt % 2 == 0:
                        P.copy(yo[:, tt, half * 512:(half + 1) * 512], p_[:])
                    else:
                        P.act(yo[:, tt, half * 512:(half + 1) * 512], p_[:], AF.Copy)
            P.dma(YS[b * 512:(b + 1) * 512, :].rearrange("(t p) d -> p t d", p=128), yo[:], writes=["YSs"])
        P.barrier()
        P.release(mkb)
        gfin = P.sb("gfin", [128, D]); P.dma(gfin[:], g_final)
        ya = [P.sb(f"ya{i}", [128, D]) for i in range(2)]; yb = [P.sb(f"yb{i}", [128, D]) for i in range(2)]
        h2 = [P.sb(f"h2{i}", [128, D]) for i in range(2)]; yo2 = [P.sb(f"yo2{i}", [128, D]) for i in range(2)]
        ssc = P.sb("ssc", [128, 64]); rsc = P.sb("rsc", [128, 64])
        def fetch_c(kt):
            i2 = kt % 2
            for dst_, SL, nm in ((ya[i2], SL1, "ya"), (yb[i2], SL2, "yb")):
                P.op("pool", lambda e, dst_=dst_, SL=SL, kt=kt: e.indirect_dma_start(
                    out=dst_[:], out_offset=None, in_=YS, in_offset=bass.IndirectOffsetOnAxis(ap=SL[:, kt:kt + 1], axis=0),
                    bounds_check=None), [SL, "YSs"], [dst_], dmakey=("dma", f"{nm}{i2}"))
            P.dma(h2[i2][:], H3[kt * 128:(kt + 1) * 128, :], reads=[("H3", kt // 4)])

        fetch_c(0)
        for kt in range(nkt):
            i2 = kt % 2
            if kt + 1 < nkt:
                fetch_c(kt + 1)
            P.stt(h2[i2][:], ya[i2][:], WW1[:, kt:kt + 1], h2[i2][:], ALU.mult, ALU.add)
            P.stt(h2[i2][:], yb[i2][:], WW2[:, kt:kt + 1], h2[i2][:], ALU.mult, ALU.add)
            P.act(junk[:], h2[i2][:], AF.Square, accum_out=ssc[:, kt:kt + 1])
            P.act(rsc[:, kt:kt + 1], ssc[:, kt:kt + 1], AF.Sqrt, bias=epsc[:], scale=1.0 / D)
            P.recip(rsc[:, kt:kt + 1], rsc[:, kt:kt + 1])
            P.stt(yo2[i2][:], h2[i2][:], rsc[:, kt:kt + 1], gfin[:], ALU.mult, ALU.mult)
            P.dma(y[kt * 128:(kt + 1) * 128, :], yo2[i2][:], writes=[("y", kt)])
        P.barrier()
        P.release(mk)

    phases = []
    prep_ffn(0, dense_w13[0], dense_w2[0])
    np_ = 0
    for l in range(nlayers):
        hsrc, hname = (x, "x") if l == 0 else (H3, "H3")
        for ph in "ABCD":
            if np_ >= nphase:
                break
            np_ += 1
            if ph == "A":
                phase_A(l, hsrc, hname)
            elif ph == "B":
                phase_B(l)
            elif ph == "C":
                phase_C(l, hsrc, hname)
            else:
                if l % 2 == 1:
                    phase_D_moe(l)
                else:
                    phase_D(l, moe=False, last=(l == nlayers - 1))
    P.emit()
    P.close()
    return nc, P


def host_inputs(inputs, b):
    f = lambda a: np.ascontiguousarray(np.asarray(a, dtype=np.float32))
    rep = lambda a: f(np.broadcast_to(np.asarray(a)[None], (128,) + np.asarray(a).shape))
    m = {}
    m["x"] = f(inputs["x"][b]); m["mem"] = f(inputs["mem"][b])
    for k in ("w_in", "sgu_w", "rg_wa", "rg_wx", "w_branch", "w_gate", "w_out", "wq_c", "wkv_c", "wo_c",
              "dense_w13", "dense_w2", "router_w", "moe_w13", "moe_w2"):
        m[k] = f(inputs[k])
    m["g_mix"] = rep(inputs["norm_mix"]); m["g_cross"] = rep(inputs["norm_cross"]); m["g_ffn"] = rep(inputs["norm_ffn"])
    m["g_mem"] = rep(inputs["norm_mem"]); m["g_final"] = rep(inputs["norm_final"])
    m["sgu_g_b"] = rep(inputs["sgu_g"]); m["sgu_b_b"] = rep(np.asarray(inputs["sgu_b"]).reshape(L, 512))
    m["sinks_b"] = rep(inputs["swa_sinks"]); m["bf_b"] = rep(inputs["fox_bf"]); m["rb_b"] = rep(np.asarray(inputs["router_b"])[0])
    pp = lambda a: f(np.asarray(a).reshape(L, 4, 128).transpose(2, 0, 1))
    m["conv_w_p"] = f(np.asarray(inputs["conv_w"]).reshape(L, 4, 4, 128).transpose(3, 0, 1, 2))
    m["conv_b_p"] = pp(inputs["conv_b"]); m["ba_p"] = pp(inputs["rg_ba"]); m["bx_p"] = pp(inputs["rg_bx"]); m["lam_p"] = pp(inputs["rg_lambda"])
    m["bgate_p"] = f(np.asarray(inputs["b_gate"]).reshape(L, 4, 8, 128).transpose(3, 0, 1, 2))
    m["ident"] = np.eye(128, dtype=np.float32)
    m["pidx"] = np.arange(128, dtype=np.float32).reshape(128, 1)
    k = np.arange(128)[:, None]; q = np.arange(128)[None, :]
    m["tri"] = (k <= q).astype(np.float32)
    slopes = 2.0 ** (-(np.arange(1, 9, dtype=np.float64)))
    me = np.zeros((128, 8, 2, 128), np.float64)
    for h in range(8):
        me[:, h, 0, :] = np.where(k <= q, np.exp(-slopes[h] * (q - k)), 0.0)
        me[:, h, 1, :] = np.where(k > q, np.exp(-slopes[h] * (q + 128 - k)), 0.0)
    m["mexp"] = me.astype(np.float32)
    return m


_CACHE = {}


def kernel(**inputs):
    if "nc" not in _CACHE:
        _CACHE["nc"] = build()[0]
    nc = _CACHE["nc"]
    in_maps = [host_inputs(inputs, b) for b in range(8)]
    res = run_bass_kernel_spmd(nc, in_maps, core_ids=list(range(8)))
    return np.stack([np.asarray(r["y"], dtype=np.float32) for r in res.results], axis=0)
```

```python
import numpy as np
import concourse.bass as bass
import concourse.mybir as mybir
from concourse.bass_utils import run_bass_kernel_spmd

F32 = mybir.dt.float32
BF16 = mybir.dt.bfloat16
AF = mybir.ActivationFunctionType
ALU = mybir.AluOpType
AX = mybir.AxisListType

ENGS = ("pe", "act", "dve", "pool", "sp")
SAME_ENGINE_SYNC = True


class Prog:
    def __init__(self, nc):
        self.nc = nc
        self.ops = []
        self.stack = []
        self.uid = 0

    def sb(self, name, shape, dt=F32):
        self.uid += 1
        g = self.nc.sbuf_tensor(f"{name}_{self.uid}", list(shape), dt)
        t = g.__enter__()
        self.stack.append(g)
        return t

    def ps(self, name, shape, dt=F32):
        g = self.nc.psum_tensor(name, list(shape), dt)
        t = g.__enter__()
        self.stack.append(g)
        return t

    def mark(self):
        return len(self.stack)

    def release(self, mk):
        while len(self.stack) > mk:
            self.stack.pop().__exit__(None, None, None)

    @staticmethod
    def _tok(x):
        if isinstance(x, (str, tuple)):
            return x
        return x.name

    def op(self, eng, fn, reads, writes, dmakey=None):
        r = tuple(self._tok(x) for x in reads if x is not None and not isinstance(x, (int, float)))
        w = tuple(self._tok(x) for x in writes if x is not None)
        self.ops.append((eng, fn, r, w, dmakey))

    def barrier(self):
        for e in ENGS:
            self.ops.append((e, None, (), (), None))

    def mm(self, out, lhsT, rhs, start=True, stop=True, sgc=False):
        if sgc:
            self.op("pe", lambda e: e.matmul(out, lhsT, rhs, start=start, stop=stop, skip_group_check=True),
                    [lhsT, rhs], [out])
        else:
            self.op("pe", lambda e: e.matmul(out, lhsT, rhs, start=start, stop=stop),
                    [lhsT, rhs], [out])

    def tr(self, out, in_, ident):
        self.op("pe", lambda e: e.transpose(out, in_, ident), [in_, ident], [out])

    def act(self, out, in_, func, bias=None, scale=None, accum_out=None):
        kw = {}
        if bias is not None:
            kw["bias"] = bias
        if scale is not None:
            kw["scale"] = scale
        if accum_out is not None:
            kw["accum_out"] = accum_out
        self.op("act", lambda e: e.activation(out, in_, func, **kw),
                [in_, bias, scale], [out, accum_out])

    def tt(self, out, in0, in1, op, eng="dve"):
        self.op(eng, lambda e: e.tensor_tensor(out, in0, in1, op), [in0, in1], [out])

    def ts(self, out, in0, s1, s2=None, op0=ALU.mult, op1=None, eng="dve"):
        kw = {}
        if op1 is not None:
            kw["op1"] = op1
        self.op(eng, lambda e: e.tensor_scalar(out, in0, s1, s2, op0, **kw), [in0, s1, s2], [out])

    def stt(self, out, in0, scalar, in1, op0, op1, eng="dve"):
        eng = "dve"
        self.op(eng, lambda e: e.scalar_tensor_tensor(out, in0, scalar, in1, op0, op1), [in0, scalar, in1], [out])

    def copy(self, out, in_, eng="dve"):
        self.op(eng, lambda e: e.tensor_copy(out, in_), [in_], [out])

    def memset(self, out, val, eng="dve"):
        self.op(eng, lambda e: e.memset(out, val), [], [out])

    def scan(self, out, d0, d1, init, op0, op1):
        self.op("dve", lambda e: e.tensor_tensor_scan(out, d0, d1, init, op0, op1), [d0, d1, init], [out])

    def recip(self, out, in_):
        self.op("dve", lambda e: e.reciprocal(out, in_), [in_], [out])

    def rsum(self, out, in_):
        self.op("dve", lambda e: e.reduce_sum(out, in_, AX.X), [in_], [out])

    def rmax(self, out, in_):
        self.op("dve", lambda e: e.reduce_max(out, in_, AX.X), [in_], [out])

    def dma(self, out, in_, eng="sp", key=None, reads=None, writes=None):
        r = [in_] if reads is None else list(reads)
        w = [out] if writes is None else list(writes)
        if key is None:
            key = in_.name if "dram" in str(type(out.tensor)).lower() else out.name
            key = key.rsplit("_", 1)[0]
        self.op(eng, lambda e: e.dma_start(out=out, in_=in_), r, w, dmakey=("dma", key))

    def emit(self):
        nc = self.nc
        ops = self.ops
        n = len(ops)
        last_w, readers = {}, {}
        last_eng, last_dma = {}, {}
        deps = [None] * n
        i = 0
        while i < n:
            eng, fn, r, w, dk = ops[i]
            if fn is None:
                snap = set(last_eng.values()) | set(last_dma.values())
                for k in range(len(ENGS)):
                    deps[i + k] = set(snap)
                last_w, readers = {}, {}
                i += len(ENGS)
                continue
            d = set()
            for t in r:
                if t in last_w:
                    d.add(last_w[t])
            for t in w:
                if t in last_w:
                    d.add(last_w[t])
                for j in readers.get(t, ()):
                    d.add(j)
            d.discard(i)
            best = {}
            for j in d:
                sk = ops[j][4] if ops[j][4] is not None else ops[j][0]
                if best.get(sk, -1) < j:
                    best[sk] = j
            d = set(best.values())
            deps[i] = d
            for t in w:
                last_w[t] = i
                readers[t] = []
            for t in r:
                if t not in w:
                    readers.setdefault(t, []).append(i)
            if dk is not None:
                last_dma[dk] = i
            else:
                last_eng[eng] = i
            i += 1

        def needs_wait(i, j):
            ei, ej = ops[i][0], ops[j][0]
            if ops[j][4] is not None:
                return True
            if ei != ej:
                return True
            if ei == "pe":
                return False
            return SAME_ENGINE_SYNC

        waited = [False] * n
        for i in range(n):
            for j in deps[i]:
                if needs_wait(i, j):
                    waited[j] = True
        sems, counts = {}, {}
        semval = [None] * n

        def get_sem(k):
            if k not in sems:
                g = nc.semaphore("s_" + "_".join(str(x) for x in k))
                sems[k] = g.__enter__()
                self.stack.append(g)
                counts[k] = 0
            return sems[k]

        for i, (eng, fn, r, w, dk) in enumerate(ops):
            if fn is None:
                continue
            if dk is not None:
                get_sem(dk)
                counts[dk] += 16
                semval[i] = (dk, counts[dk])
            elif waited[i]:
                k = ("eng", eng)
                get_sem(k)
                counts[k] += 1
                semval[i] = (k, counts[k])
        streams = {e: [] for e in ENGS}
        seen = {e: {} for e in ENGS}
        for i, (eng, fn, r, w, dk) in enumerate(ops):
            need = {}
            for j in deps[i]:
                if not needs_wait(i, j):
                    continue
                k, v = semval[j]
                if need.get(k, 0) < v:
                    need[k] = v
            waits = []
            for k, v in need.items():
                if seen[eng].get(k, 0) < v:
                    seen[eng][k] = v
                    waits.append((k, v))
            streams[eng].append((i, waits))
        self.counts = dict(counts)
        engobj = {"pe": "tensor", "act": "scalar", "dve": "vector", "pool": "gpsimd", "sp": "sync"}
        tail = [(k, v) for k, v in counts.items()]
        with nc.Block() as block:
            for ename in ENGS:
                def body(e, ename=ename):
                    for i, waits in streams[ename]:
                        for k, v in waits:
                            e.wait_ge(sems[k], v)
                        if ops[i][1] is None:
                            continue
                        ins = ops[i][1](e)
                        if semval[i] is not None:
                            k, v = semval[i]
                            ins.then_inc(sems[k], 16 if k[0] == "dma" else 1)
                    if ename == "sp":
                        for k, v in tail:
                            e.wait_ge(sems[k], v)
                getattr(block, engobj[ename])(body)

    def close(self):
        self.release(0)


S, D, T, NG, L = 8192, 1024, 512, 16, 2
DFF = 2816
NFC = DFF // 128
EPS = 1e-6
O_AU, O_AV, O_BX, O_BY, O_CQ, O_CK, O_CV, O_DQ, O_DK, O_DV, O_DF = 0, 512, 1024, 1536, 2048, 2560, 2688, 2816, 3328, 3840, 4352


def build(nlayers=2, nphase=99, dbg=False, ng=NG):
    nc = bass.Bass("TRN2", target_bir_lowering=False)
    P = Prog(nc)

    def din(name, shape):
        return nc.dram_tensor(name, list(shape), F32, kind="ExternalInput").ap()

    def dscr(name, shape, dt):
        kind = "ExternalOutput" if dbg else "Internal"
        return nc.dram_tensor(name, list(shape), dt, kind=kind).ap()

    x = din("x", [S, D]); mem = din("mem", [256, D])
    w_in = din("w_in", [L, D, 4360]); sgu_w = din("sgu_w", [L, 4, 128, 128])
    rg_wa = din("rg_wa", [L, 4, 128, 128]); rg_wx = din("rg_wx", [L, 4, 128, 128])
    w_branch = din("w_branch", [L, 4, 512, D]); w_gate = din("w_gate", [L, 4, D, D]); w_out = din("w_out", [L, D, D])
    wq_c = din("wq_c", [L, D, 512]); wkv_c = din("wkv_c", [L, D, 1024]); wo_c = din("wo_c", [L, 512, D])
    dense_w13 = din("dense_w13", [1, D, 2 * DFF]); dense_w2 = din("dense_w2", [1, DFF, D])
    router_w = din("router_w", [1, D, 8])
    moe_w13 = din("moe_w13", [1, 8, D, 2 * DFF]); moe_w2 = din("moe_w2", [1, 8, DFF, D])
    g_mix = din("g_mix", [128, L, D]); g_cross = din("g_cross", [128, L, D]); g_ffn = din("g_ffn", [128, L, D])
    g_mem = din("g_mem", [128, L, D]); g_final = din("g_final", [128, D])
    sgu_g_b = din("sgu_g_b", [128, L, 512]); sgu_b_b = din("sgu_b_b", [128, L, 512])
    sinks_b = din("sinks_b", [128, L, 8]); bf_b = din("bf_b", [128, L, 8]); rb_b = din("rb_b", [128, 8])
    conv_w_p = din("conv_w_p", [128, L, 4, 4]); conv_b_p = din("conv_b_p", [128, L, 4])
    ba_p = din("ba_p", [128, L, 4]); bx_p = din("bx_p", [128, L, 4]); lam_p = din("lam_p", [128, L, 4])
    bgate_p = din("bgate_p", [128, L, 4, 8])
    pidx_d = din("pidx", [128, 1])
    ident_d = din("ident", [128, 128]); tri_d = din("tri", [128, 128]); mexp_d = din("mexp", [128, 8, 2, 128])
    y = nc.dram_tensor("y", [S, D], F32, kind="ExternalOutput").ap()

    XNT = dscr("XNT", [128, 8, S], BF16)
    QT = dscr("QT", [128, 8, S], BF16)
    KT = dscr("KT", [128, 5, S], BF16)
    VV = dscr("VV", [128, 64, 640], BF16)
    OA = dscr("OA", [128, 4, S], BF16)
    OB = dscr("OB", [128, 4, S], BF16)
    OC = dscr("OC", [128, 64, 512], BF16)
    OD = dscr("OD", [128, 64, 512], BF16)
    H1 = dscr("H1", [S, D], F32)
    H3 = dscr("H3", [S, D], F32)
    I32 = mybir.dt.int32
    NBLK = 40
    WSL = {}
    for sbi in range(6):
        for t_ in range(2):
            WSL[(sbi, t_)] = nc.dram_tensor(f"WSL{sbi}_{t_}", [1024, 8, 512 if sbi < 5 else 256], BF16, kind="Internal").ap()
    W2H = [nc.dram_tensor(f"W2H{hf}", [1024, NFC, 512], BF16, kind="Internal").ap() for hf in range(2)]
    XS = nc.dram_tensor("XS", [NBLK * 512, D], BF16, kind="Internal").ap()
    YS = nc.dram_tensor("YS", [NBLK * 512, D], F32, kind="Internal").ap()
    XN2 = nc.dram_tensor("XN2", [S, D], BF16, kind="Internal").ap()
    W13B = [nc.dram_tensor(f"W13B{e}", [D, 2 * DFF], BF16, kind="Internal").ap() for e in range(1)]
    W2B = [nc.dram_tensor(f"W2B{e}", [DFF, D], BF16, kind="Internal").ap() for e in range(1)]

    ps = [P.ps(f"ps{i}", [128, 512]) for i in range(6)]
    pbs = [P.ps(f"pb{i}", [128, 1024], BF16) for i in range(2)]
    ring = {"i": 0, "b": 0}

    def nps(n=6):
        ring["i"] = (ring["i"] + 1) % n
        return ps[ring["i"]]

    def npb():
        ring["b"] = (ring["b"] + 1) % 2
        return pbs[ring["b"]]

    ident = P.sb("ident", [128, 128], BF16)
    tri_f = P.sb("tri_f", [128, 128]); tri_b = P.sb("tri_b", [128, 128], BF16)
    ones_f = P.sb("ones_f", [128, 128]); ones_b = P.sb("ones_b", [128, 128], BF16)
    CK = P.sb("CK", [128, 64, 8]); CREF = P.sb("CREF", [128, 16, 8])
    epsc = P.sb("epsc", [128, 1])
    P.dma(ident[:], ident_d, eng="pool")
    P.dma(tri_f[:], tri_d)
    P.dma(tri_b[:], tri_d, eng="pool")
    P.memset(ones_f[:], 1.0); P.memset(ones_b[:], 1.0); P.memset(epsc[:], EPS)

    def prep_ffn(e_idx, w13src, w2src):
        for kc in range(8):
            P.dma(W13B[e_idx][kc * 128:(kc + 1) * 128, :], w13src[kc * 128:(kc + 1) * 128, :], eng="pool",
                  key=f"prep{kc % 4}", writes=[f"W13B{e_idx}"])
        for fc in range(0, NFC, 2):
            P.dma(W2B[e_idx][fc * 128:(fc + 2) * 128, :], w2src[fc * 128:(fc + 2) * 128, :], eng="pool",
                  key=f"prep{(fc // 2) % 4}", writes=[f"W2B{e_idx}"])

    def prep_moe(e):
        w13v = moe_w13[0, e].rearrange("(k p) n -> p k n", p=128)
        w2v = moe_w2[0, e].rearrange("(k p) n -> p k n", p=128)
        i = 0
        for sbi in range(6):
            ncol = 512 if sbi < 5 else 256
            for t_ in range(2):
                c0 = t_ * DFF + sbi * 512
                P.dma(WSL[(sbi, t_)][e * 128:(e + 1) * 128, :, :], w13v[:, :, c0:c0 + ncol], eng="pool",
                      key=f"prep{i % 4}", writes=["WP"])
                i += 1
        for hf in range(2):
            for f0 in range(0, NFC, 11):
                P.dma(W2H[hf][e * 128:(e + 1) * 128, f0:f0 + 11, :], w2v[:, f0:f0 + 11, hf * 512:(hf + 1) * 512], eng="pool",
                      key=f"prep{i % 4}", writes=["WP"])
                i += 1

    def rms_p1(hT, gain, xn_tok, junk, ss, rstd):
        for tt in range(4):
            P.act(junk[:], hT[:, tt, :], AF.Square, accum_out=ss[:, tt:tt + 1])
        P.act(rstd[:], ss[:], AF.Sqrt, bias=epsc[:], scale=1.0 / D)
        P.recip(rstd[:], rstd[:])
        for tt in range(4):
            P.stt(xn_tok[:, tt, :], hT[:, tt, :], rstd[:, tt:tt + 1], gain, ALU.mult, ALU.mult,
                  eng="dve" if tt % 2 == 0 else "pool")

    def rms_p2(xn_tok, xT):
        for tt in range(4):
            pb = npb()
            for kc in range(8):
                P.tr(pb[:, kc * 128:(kc + 1) * 128], xn_tok[:, tt, kc * 128:(kc + 1) * 128], ident[:])
            src = pb[:, :].rearrange("p (a b) -> p a b", a=8)
            dst = xT[:, :, tt * 128:(tt + 1) * 128]
            if tt % 2 == 0:
                P.copy(dst, src)
            else:
                P.act(dst, src, AF.Copy)

    def rms_to_T(hT, gain, xn_tok, xT, junk, ss, rstd):
        rms_p1(hT, gain, xn_tok, junk, ss, rstd)
        rms_p2(xn_tok, xT)

    def load_h(hT, hsrc, hname, g):
        P.dma(hT[:], hsrc[g * T:(g + 1) * T, :].rearrange("(t p) d -> p t d", p=128), reads=[(hname, g)])

    def phase_A(l, hsrc, hname):
        mk = P.mark()
        Win = P.sb("Win", [128, 8, 4360], BF16)
        for kc in range(8):
            P.dma(Win[:, kc, :], w_in[l, kc * 128:(kc + 1) * 128, :], eng="pool")
        gain = P.sb("gainA", [128, D]); P.dma(gain[:], g_mix[:, l, :])
        sgug = P.sb("sgug", [128, 512]); P.dma(sgug[:], sgu_g_b[:, l, :])
        sgub = P.sb("sgub", [128, 512]); P.dma(sgub[:], sgu_b_b[:, l, :])
        bfb = P.sb("bfb", [128, 8]); P.dma(bfb[:], bf_b[:, l, :])
        cw = P.sb("cw", [128, 4, 4]); P.dma(cw[:], conv_w_p[:, l])
        cb = P.sb("cb", [128, 4]); P.dma(cb[:], conv_b_p[:, l, :])
        bap = P.sb("bap", [128, 4]); P.dma(bap[:], ba_p[:, l, :])
        bxp = P.sb("bxp", [128, 4]); P.dma(bxp[:], bx_p[:, l, :])
        lam = P.sb("lam", [128, 4]); P.dma(lam[:], lam_p[:, l, :])
        cc = P.sb("cc", [128, 4])
        P.act(cc[:], lam[:], AF.Exp, scale=-1.0)
        P.act(cc[:], cc[:], AF.Ln, bias=1.0)
        P.ts(cc[:], cc[:], -8.0, None, op0=ALU.mult)
        Wa = P.sb("Wa", [128, 4, 128], BF16); Wx = P.sb("Wx", [128, 4, 128], BF16)
        P.dma(Wa[:], rg_wa[l].rearrange("h i o -> i h o"), eng="pool")
        P.dma(Wx[:], rg_wx[l].rearrange("h i o -> i h o"), eng="pool")
        wsb = P.sb("wsb", [128, 4, 128], BF16); WsT = P.sb("WsT", [128, 4, 128], BF16)
        P.dma(wsb[:], sgu_w[l].rearrange("g t s -> t g s"), eng="pool")
        pb = npb()
        for gc in range(4):
            P.tr(pb[:, gc * 128:(gc + 1) * 128], wsb[:, gc, :], ident[:])
        for gc in range(4):
            P.tt(WsT[:, gc, :], pb[:, gc * 128:(gc + 1) * 128], tri_b[:], ALU.mult)

        hT = P.sb("hT", [128, 4, D]); xn_tok = P.sb("xn_tok", [128, 4, D], BF16); xnT = P.sb("xnT", [128, 8, T], BF16)
        junk = P.sb("junk", [128, D]); ss = P.sb("ss", [128, 4]); rstd = P.sb("rstd", [128, 4])
        uT = P.sb("uT", [128, 4, T], BF16); vg = P.sb("vg", [128, 512]); v_tok = P.sb("v_tok", [128, 4, 512], BF16)
        ssv = P.sb("ssv", [128, 1]); rsv = P.sb("rsv", [128, 1])
        bx = P.sb("bx", [128, 4, T + 3]); gy = P.sb("gy", [128, 4, T])
        QTst = P.sb("QTst", [128, 8, T], BF16); KTst = P.sb("KTst", [128, 5, T], BF16); Vst = P.sb("Vst", [128, 4, 640], BF16)
        oaT = P.sb("oaT", [128, 4, T], BF16); obT = P.sb("obT", [128, 4, T], BF16)
        tmpa = P.sb("tmpa", [128, 4, 128])
        xf = P.sb("xf", [128, 8]); ls = P.sb("ls", [128, 8]); carry = P.sb("carry", [128, 8]); hcar = P.sb("hcar", [128, 4])
        xc4 = P.sb("xc4", [128, 4, T]); xcb4 = P.sb("xcb4", [128, 4, T], BF16); rr = P.sb("rr", [128, T]); ii = P.sb("ii", [128, T])
        aa = P.sb("aa", [128, T]); a2 = P.sb("a2", [128, T]); inp = P.sb("inp", [128, T]); hh = P.sb("hh", [128, T])
        P.memset(bx[:], 0.0); P.memset(carry[:], 0.0); P.memset(hcar[:], 0.0)

        xnTs = [xnT, P.sb("xnT2", [128, 8, T], BF16)]
        load_h(hT, hsrc, hname, 0)
        rms_p1(hT, gain[:], xn_tok, junk, ss, rstd)
        if ng > 1:
            load_h(hT, hsrc, hname, 1)
        rms_p2(xn_tok, xnTs[0])
        for g in range(ng):
            xnT = xnTs[g % 2]
            P.dma(XNT[:, :, g * T:(g + 1) * T], xnT[:], writes=[("XNT", g)])
            if g + 1 < ng:
                rms_p1(hT, gain[:], xn_tok, junk, ss, rstd)
                if g + 2 < ng:
                    load_h(hT, hsrc, hname, g + 2)

            def proj(col0, nch, epi):
                for c in range(nch):
                    p_ = nps()
                    for kc in range(8):
                        P.mm(p_[:], Win[:, kc, col0 + c * 128:col0 + (c + 1) * 128], xnT[:, kc, :], start=kc == 0, stop=kc == 7)
                    epi(c, p_)
            proj(O_AU, 4, lambda c, p_: P.act(uT[:, c, :], p_[:], AF.Gelu_apprx_tanh))
            proj(O_BX, 4, lambda c, p_: P.copy(bx[:, c, 3:T + 3], p_[:]))
            proj(O_BY, 4, lambda c, p_: P.act(gy[:, c, :], p_[:], AF.Gelu_apprx_tanh))
            for c in range(4):
                P.act(xc4[:, c, :], bx[:, c, 3:T + 3], AF.Identity, bias=cb[:, c:c + 1], scale=cw[:, 3, c:c + 1])
                for k in range(3):
                    P.stt(xc4[:, c, :], bx[:, c, k:k + T], cw[:, k, c:c + 1], xc4[:, c, :], ALU.mult, ALU.add)
                P.copy(xcb4[:, c, :], xc4[:, c, :], eng="pool")
                P.copy(bx[:, c, 0:3], bx[:, c, T:T + 3], eng="pool")
            proj(O_CQ, 4, lambda c, p_: P.copy(QTst[:, c, :], p_[:]))
            proj(O_DQ, 4, lambda c, p_: P.act(QTst[:, 4 + c, :], p_[:], AF.Copy))
            proj(O_CK, 1, lambda c, p_: P.copy(KTst[:, 0, :], p_[:]))
            proj(O_DK, 4, lambda c, p_: P.act(KTst[:, 1 + c, :], p_[:], AF.Copy))
            if g + 1 < ng:
                rms_p2(xn_tok, xnTs[(g + 1) % 2])
            for tt in range(4):
                tok = slice(tt * 128, (tt + 1) * 128)
                p_ = nps()
                for kc in range(8):
                    P.mm(p_[:], xnT[:, kc, tok], Win[:, kc, O_AV:O_AV + 512], start=kc == 0, stop=kc == 7)
                P.act(vg[:], p_[:], AF.Gelu_apprx_tanh)
                P.act(junk[:, 0:512], vg[:], AF.Square, accum_out=ssv[:])
                P.act(rsv[:], ssv[:], AF.Sqrt, bias=epsc[:], scale=1.0 / 512)
                P.recip(rsv[:], rsv[:])
                P.stt(v_tok[:, tt, :], vg[:], rsv[:, 0:1], sgug[:], ALU.mult, ALU.mult)
                p1 = nps()
                for kc in range(8):
                    P.mm(p1[:], xnT[:, kc, tok], Win[:, kc, O_DV:O_DV + 512], start=kc == 0, stop=kc == 7)
                P.copy(Vst[:, tt, 128:640], p1[:])
                p2 = nps()
                for kc in range(8):
                    P.mm(p2[:, 0:128], xnT[:, kc, tok], Win[:, kc, O_CV:O_CV + 128], start=kc == 0, stop=kc == 7)
                for kc in range(8):
                    P.mm(p2[:, 128:136], xnT[:, kc, tok], Win[:, kc, O_DF:O_DF + 8], start=kc == 0, stop=kc == 7)
                P.act(Vst[:, tt, 0:128], p2[:, 0:128], AF.Copy)
                P.tt(xf[:], p2[:, 128:136], bfb[:], ALU.add)
                P.act(xf[:], xf[:], AF.Exp, scale=-1.0)
                P.act(xf[:], xf[:], AF.Ln, bias=1.0)
                P.ts(ls[:], xf[:], -1.0, None, op0=ALU.mult)
                p3 = nps()
                P.mm(p3[:, 0:8], tri_f[:], ls[:])
                P.mm(p3[:, 8:16], ones_f[:], ls[:])
                kt = 4 * g + tt
                P.tt(CK[:, kt, :], p3[:, 0:8], carry[:], ALU.add)
                P.tt(carry[:], p3[:, 8:16], carry[:], ALU.add)
                if tt == 1:
                    P.copy(CREF[:, g, :], carry[:])
                p4 = nps()
                for gc in range(4):
                    P.mm(p4[:, gc * 128:(gc + 1) * 128], v_tok[:, tt, gc * 128:(gc + 1) * 128], WsT[:, gc, :])
                P.tt(tmpa[:], p4[:, :].rearrange("p (a b) -> p a b", a=4), sgub[:, :].rearrange("p (a b) -> p a b", a=4), ALU.add)
                P.tt(oaT[:, :, tok], tmpa[:], uT[:, :, tok], ALU.mult, eng="pool")
                c = tt
                pr = nps(); P.mm(pr[:], Wa[:, c, :], xcb4[:, c, :])
                pi = nps(); P.mm(pi[:], Wx[:, c, :], xcb4[:, c, :])
                P.act(rr[:], pr[:], AF.Sigmoid, bias=bap[:, c:c + 1])
                P.act(ii[:], pi[:], AF.Sigmoid, bias=bxp[:, c:c + 1])
                P.act(aa[:], rr[:], AF.Exp, scale=cc[:, c:c + 1])
                P.tt(a2[:], aa[:], aa[:], ALU.mult, eng="pool")
                P.act(a2[:], a2[:], AF.Sqrt, bias=1.0, scale=-1.0)
                P.tt(inp[:], xc4[:, c, :], ii[:], ALU.mult)
                P.tt(inp[:], inp[:], a2[:], ALU.mult, eng="pool")
                P.scan(hh[:], aa[:], inp[:], hcar[:, c:c + 1], ALU.mult, ALU.add)
                P.copy(hcar[:, c:c + 1], hh[:, T - 1:T])
                P.tt(obT[:, c, :], hh[:], gy[:, c, :], ALU.mult, eng="pool")
            gs = slice(g * T, (g + 1) * T)
            P.dma(OA[:, :, gs], oaT[:], writes=[("OA", g)])
            P.dma(OB[:, :, gs], obT[:], writes=[("OB", g)])
            P.dma(QT[:, :, gs], QTst[:], writes=[("QT", g)])
            P.dma(KT[:, :, gs], KTst[:], writes=[("KT", g)])
            P.dma(VV[:, 4 * g:4 * g + 4, :], Vst[:], writes=[("VV", g)])
        P.barrier()
        P.release(mk)

    def phase_B(l):
        mk = P.mark()
        if l == 0 and nlayers > 1:
            for e in range(8):
                prep_moe(e)
        mexp = P.sb("mexp", [128, 8, 2, 128]); P.dma(mexp[:], mexp_d)
        snk = P.sb("snk", [128, 8]); P.dma(snk[:], sinks_b[:, l, :])
        P.act(snk[:], snk[:], AF.Exp)
        qh = [P.sb(f"qh{i}", [64, S], BF16) for i in range(2)]
        kh = [P.sb(f"kh{i}", [64, S], BF16) for i in range(2)]
        vh = [P.sb(f"vh{i}", [128, 64, 65], BF16) for i in range(2)]
        for i in range(2):
            P.memset(vh[i][:, :, 64:65], 1.0)
        biasgs = [P.sb(f"biasg{i}", [128, 64]) for i in range(2)]
        pts = [P.sb(f"pt{i}", [128, 512], BF16) for i in range(6)]
        pfs = [P.sb(f"pf{i}", [128, 512]) for i in range(4)]
        dens = [P.sb(f"den{i}", [128, 4]) for i in range(2)]; ods = [P.sb(f"od{i}", [128, 4, 64], BF16) for i in range(2)]
        cnt = 0
        allg = list(range(ng))
        def load_head(hd):
            fox = hd >= 8
            h = hd % 8
            b = hd % 2
            q_, k_, v_ = qh[b], kh[b], vh[b]
            Sg = ng * T
            if fox:
                P.dma(q_[:, 0:Sg], QT[(h % 2) * 64:(h % 2) * 64 + 64, 4 + h // 2, 0:Sg], reads=[("QT", g) for g in allg])
                P.dma(k_[:, 0:Sg], KT[(h % 2) * 64:(h % 2) * 64 + 64, 1 + h // 2, 0:Sg], reads=[("KT", g) for g in allg])
                voff = 128 + h * 64
            else:
                kv = h // 4
                P.dma(q_[:, 0:Sg], QT[(h % 2) * 64:(h % 2) * 64 + 64, h // 2, 0:Sg], reads=[("QT", g) for g in allg])
                P.dma(k_[:, 0:Sg], KT[kv * 64:kv * 64 + 64, 0, 0:Sg], reads=[("KT", g) for g in allg])
                voff = kv * 64
            for k0 in range(0, 4 * ng, 16):
                k1 = min(4 * ng, k0 + 16)
                P.dma(v_[:, k0:k1, 0:64], VV[:, k0:k1, voff:voff + 64], reads=[("VV", g) for g in allg])

        load_head(0)
        for hd in range(16):
            fox = hd >= 8
            h = hd % 8
            b = hd % 2
            q_, k_, v_ = qh[b], kh[b], vh[b]
            if hd + 1 < 16:
                load_head(hd + 1)
            items = []
            for g in range(ng):
                kts = list(range(0, 4 * g + 4)) if fox else list(range(max(0, 4 * g - 1), 4 * g + 4))
                for kt in kts:
                    items.append((g, kt, kts))
            state = {}

            def stage1(it):
                g, kt, kts = it
                if kt == kts[0] and fox:
                    bg_ = biasgs[g % 2]
                    P.ts(bg_[:, 0:4 * g + 4], CK[:, 0:4 * g + 4, h], -1.0, CREF[:, g, h:h + 1], op0=ALU.mult, op1=ALU.add)
                j = kt - 4 * g
                lo = max(0, j)
                hi = 3 if fox else min(3, j + 1)
                cs = slice(lo * 128, (hi + 1) * 128)
                p_ = nps(4)
                P.mm(p_[:, cs], k_[:, kt * 128:(kt + 1) * 128], q_[:, g * T + lo * 128:g * T + (hi + 1) * 128])
                state["cnt"] = state.get("cnt", 0) + 1
                pt = pts[state["cnt"] % 6]
                if fox:
                    P.act(pt[:, cs], p_[:, cs], AF.Exp, bias=biasgs[g % 2][:, kt:kt + 1], scale=0.125)
                    if j >= 0:
                        P.tt(pt[:, j * 128:(j + 1) * 128], pt[:, j * 128:(j + 1) * 128], tri_b[:], ALU.mult)
                else:
                    pf = pfs[state["cnt"] % 4]
                    P.act(pf[:, cs], p_[:, cs], AF.Exp, scale=0.125)
                    for qs in range(lo, hi + 1):
                        idx = 0 if kt == 4 * g + qs else 1
                        P.tt(pt[:, qs * 128:(qs + 1) * 128], pf[:, qs * 128:(qs + 1) * 128], mexp[:, h, idx, :], ALU.mult)
                return (pt, lo, hi)

            def stage2(it, s1):
                g, kt, kts = it
                pt, lo, hi = s1
                po = ps[4 + (g % 2)]
                pov = po[:, 0:260].rearrange("p (a b) -> p a b", a=4)
                for qs in range(lo, hi + 1):
                    last = 4 * g + qs
                    P.mm(pov[:, qs, :], pt[:, qs * 128:(qs + 1) * 128], v_[:, kt, :],
                         start=(kt == kts[0] and qs == lo), stop=kt == last, sgc=True)
                if kt == kts[-1]:
                    od = ods[g % 2]
                    dn = dens[g % 2]
                    if fox:
                        P.copy(dn[:], pov[:, :, 64])
                    else:
                        P.ts(dn[:], pov[:, :, 64], snk[:, h:h + 1], None, op0=ALU.add)
                    P.recip(dn[:], dn[:])
                    for qs in range(4):
                        P.ts(od[:, qs, :], pov[:, qs, 0:64], dn[:, qs:qs + 1], None, op0=ALU.mult)
                    dst = OD if fox else OC
                    P.dma(dst[:, 4 * g:4 * g + 4, h * 64:(h + 1) * 64], od[:], writes=[("OD" if fox else "OC", g, h)])

            LOOK = 3
            s1res = {}
            n_it = len(items)
            for i in range(n_it + LOOK):
                if i < n_it:
                    s1res[i] = stage1(items[i])
                if i - LOOK >= 0:
                    stage2(items[i - LOOK], s1res.pop(i - LOOK))
        P.barrier()
        P.release(mk)

    def phase_C(l, hsrc, hname):
        mk = P.mark()
        Wg = P.sb("Wg", [128, 8, 4096], BF16)
        for br in range(4):
            for kc in range(0, 8, 4):
                P.dma(Wg[:, kc:kc + 4, br * 1024:(br + 1) * 1024],
                      w_gate[l, br, kc * 128:(kc + 4) * 128, :].rearrange("(k p) n -> p k n", p=128), eng="pool")
        Wb = P.sb("Wb", [128, 4, 4, D], BF16)
        for br in range(4):
            P.dma(Wb[:, br], w_branch[l, br].rearrange("(k p) n -> p k n", p=128), eng="pool")
        Wo = P.sb("Wo", [128, 8, D], BF16)
        for kc in range(0, 8, 4):
            P.dma(Wo[:, kc:kc + 4, :], w_out[l, kc * 128:(kc + 4) * 128, :].rearrange("(k p) n -> p k n", p=128), eng="pool")
        bg = P.sb("bg", [128, 4, 8]); P.dma(bg[:], bgate_p[:, l])
        xnT = P.sb("xnT", [128, 8, T], BF16)
        brT = [P.sb(f"brT{i}", [128, 4, T], BF16) for i in range(2)]
        brX = [[P.sb(f"brX{j}_{i}", [128, 4, T], BF16) for i in range(2)] for j in range(2)]
        otokX = [[P.sb(f"otok{j}_{i}", [128, 4, 512], BF16) for i in range(2)] for j in range(2)]

        def prep_cd(g):
            par = g % 2
            P.dma(otokX[par][0][:], OC[:, 4 * g:4 * g + 4, :], reads=[("OC", g, h) for h in range(8)])
            P.dma(otokX[par][1][:], OD[:, 4 * g:4 * g + 4, :], reads=[("OD", g, h) for h in range(8)])
            for i in range(2):
                for tt in range(4):
                    pb = npb()
                    for c in range(4):
                        P.tr(pb[:, c * 128:(c + 1) * 128], otokX[par][i][:, tt, c * 128:(c + 1) * 128], ident[:])
                    P.copy(brX[par][i][:, :, tt * 128:(tt + 1) * 128], pb[:, 0:512].rearrange("p (a b) -> p a b", a=4))
        mT = P.sb("mT", [128, 8, T], BF16)
        gate = [P.sb(f"gate{i}", [128, T]) for i in range(2)]
        macc = P.sb("macc", [128, T]); mtmp = [P.sb(f"mtmp{i}", [128, T]) for i in range(2)]
        hT = P.sb("hT", [128, 4, D])
        for g in range(ng):
            gs = slice(g * T, (g + 1) * T)
            P.dma(xnT[:], XNT[:, :, gs], reads=[("XNT", g)])
            P.dma(brT[0][:], OA[:, :, gs], reads=[("OA", g)])
            P.dma(brT[1][:], OB[:, :, gs], reads=[("OB", g)])
            load_h(hT, hsrc, hname, g)
            if g == 0:
                prep_cd(0)
            brs = [brT[0], brT[1], brX[g % 2][0], brX[g % 2][1]]
            for oc in range(8):
                if oc == 4 and g + 1 < ng:
                    prep_cd(g + 1)
                for br in range(4):
                    pg = nps()
                    for kc in range(8):
                        P.mm(pg[:], Wg[:, kc, br * 1024 + oc * 128:br * 1024 + (oc + 1) * 128], xnT[:, kc, :], start=kc == 0, stop=kc == 7)
                    pr = nps()
                    for kc in range(4):
                        P.mm(pr[:], Wb[:, br, kc, oc * 128:(oc + 1) * 128], brs[br][:, kc, :], start=kc == 0, stop=kc == 3)
                    gt = gate[br % 2]
                    P.act(gt[:], pg[:], AF.Sigmoid, bias=bg[:, br, oc:oc + 1])
                    if br == 0:
                        P.tt(macc[:], gt[:], pr[:], ALU.mult)
                    else:
                        mt_ = mtmp[br % 2]
                        P.tt(mt_[:], gt[:], pr[:], ALU.mult)
                        if br < 3:
                            P.tt(macc[:], macc[:], mt_[:], ALU.add, eng="pool")
                        else:
                            P.tt(mT[:, oc, :], macc[:], mt_[:], ALU.add, eng="pool")
            for tt in range(4):
                for half in range(2):
                    p_ = nps()
                    for kc in range(8):
                        P.mm(p_[:], mT[:, kc, tt * 128:(tt + 1) * 128], Wo[:, kc, half * 512:(half + 1) * 512], start=kc == 0, stop=kc == 7)
                    P.tt(hT[:, tt, half * 512:(half + 1) * 512], hT[:, tt, half * 512:(half + 1) * 512], p_[:], ALU.add)
            P.dma(H1[gs, :].rearrange("(t p) d -> p t d", p=128), hT[:], writes=[("H1", g)], eng="pool")
        P.barrier()
        P.release(mk)

    def phase_D(l, moe, last):
        mk = P.mark()
        gx = P.sb("gx", [128, D]); P.dma(gx[:], g_cross[:, l, :])
        gf = P.sb("gf", [128, D]); P.dma(gf[:], g_ffn[:, l, :])
        Wq = P.sb("Wq", [128, 8, 512], BF16)
        P.dma(Wq[:], wq_c[l].rearrange("(k p) n -> p k n", p=128), eng="pool")
        Woc = P.sb("Woc", [128, 4, D], BF16)
        P.dma(Woc[:], wo_c[l].rearrange("(k p) n -> p k n", p=128), eng="pool")
        kxT = P.sb("kxT", [128, 4, 256], BF16); vx = P.sb("vx", [128, 2, 512], BF16)
        hT = P.sb("hT", [128, 4, D]); xn_tok = P.sb("xn_tok", [128, 4, D], BF16); xT = P.sb("xT", [128, 8, T], BF16)
        junk = P.sb("junk", [128, D]); ss = P.sb("ss", [128, 4]); rstd = P.sb("rstd", [128, 4])
        mk2 = P.mark()
        gm = P.sb("gm", [128, D]); P.dma(gm[:], g_mem[:, l, :])
        Wkv = P.sb("Wkv", [128, 8, D], BF16)
        for kc in range(0, 8, 4):
            P.dma(Wkv[:, kc:kc + 4, :], wkv_c[l, kc * 128:(kc + 4) * 128, :].rearrange("(k p) n -> p k n", p=128), eng="pool")
        mt_ = P.sb("memt", [128, 2, D]); P.dma(mt_[:], mem.rearrange("(t p) d -> p t d", p=128))
        mn = P.sb("memn", [128, 2, D], BF16); memT = P.sb("memT", [128, 8, 256], BF16)
        for t2 in range(2):
            P.act(junk[:], mt_[:, t2, :], AF.Square, accum_out=ss[:, t2:t2 + 1])
        P.act(rstd[:, 0:2], ss[:, 0:2], AF.Sqrt, bias=epsc[:], scale=1.0 / D)
        P.recip(rstd[:, 0:2], rstd[:, 0:2])
        for t2 in range(2):
            P.stt(mn[:, t2, :], mt_[:, t2, :], rstd[:, t2:t2 + 1], gm[:], ALU.mult, ALU.mult)
            pb = npb()
            for kc in range(8):
                P.tr(pb[:, kc * 128:(kc + 1) * 128], mn[:, t2, kc * 128:(kc + 1) * 128], ident[:])
            P.copy(memT[:, :, t2 * 128:(t2 + 1) * 128], pb[:, :].rearrange("p (a b) -> p a b", a=8))
        for hd in range(4):
            p_ = nps()
            for kc in range(8):
                P.mm(p_[:, 0:256], Wkv[:, kc, hd * 128:(hd + 1) * 128], memT[:, kc, :], start=kc == 0, stop=kc == 7)
            P.copy(kxT[:, hd, :], p_[:, 0:256])
        for t2 in range(2):
            p_ = nps()
            for kc in range(8):
                P.mm(p_[:], memT[:, kc, t2 * 128:(t2 + 1) * 128], Wkv[:, kc, 512:1024], start=kc == 0, stop=kc == 7)
            P.copy(vx[:, t2, :], p_[:])
        P.barrier()
        P.release(mk2)
        qxT = P.sb("qxT", [128, 4, T], BF16); ptm = [P.sb(f"ptm{i}", [128, T], BF16) for i in range(2)]
        rden = P.sb("rden", [128, T]); oxT = P.sb("oxT", [128, 4, T], BF16)
        hid = P.sb("hid", [128, NFC, T], BF16)
        w1g = [P.sb(f"w1g{i}", [128, 8, 512], BF16) for i in range(2)]
        w1u = [P.sb(f"w1u{i}", [128, 8, 512], BF16) for i in range(2)]
        w2h = [P.sb(f"w2h{i}", [128, NFC, 512], BF16) for i in range(2)]
        sg = [P.sb(f"sg{i}", [128, T]) for i in range(2)]
        ne = 8 if moe else 1
        if moe:
            Wr = P.sb("Wr", [128, 8, 8], BF16)
            P.dma(Wr[:], router_w[0].rearrange("(k p) n -> p k n", p=128), eng="pool")
            rb = P.sb("rb", [128, 8]); P.dma(rb[:], rb_b)
            lg = P.sb("lg", [128, 8]); lg2 = P.sb("lg2", [128, 8]); eq1 = P.sb("eq1", [128, 8]); eq2 = P.sb("eq2", [128, 8])
            m1 = P.sb("m1", [128, 1]); m2 = P.sb("m2", [128, 1]); dd = P.sb("dd", [128, 1]); w1_ = P.sb("w1_", [128, 1]); w2_ = P.sb("w2_", [128, 1])
            comb = P.sb("comb", [128, 4, 8])
        if last:
            gfin = P.sb("gfin", [128, D]); P.dma(gfin[:], g_final)
            yo = P.sb("yo", [128, 4, D])
        slab_i = 0
        hTs = [hT, P.sb("hT2", [128, 4, D])]
        for g in range(ng):
            gs = slice(g * T, (g + 1) * T)
            hT = hTs[g % 2]
            if g == 0:
                load_h(hT, H1, "H1", g)
                rms_to_T(hT, gx[:], xn_tok, xT, junk, ss, rstd)
            if g + 1 < ng:
                load_h(hTs[(g + 1) % 2], H1, "H1", g + 1)
            for hd in range(4):
                p_ = nps()
                for kc in range(8):
                    P.mm(p_[:], Wq[:, kc, hd * 128:(hd + 1) * 128], xT[:, kc, :], start=kc == 0, stop=kc == 7)
                if hd % 2 == 0:
                    P.copy(qxT[:, hd, :], p_[:])
                else:
                    P.act(qxT[:, hd, :], p_[:], AF.Copy)
            for hd in range(4):
                po = nps(); pd = nps()
                for m_ in range(2):
                    p_ = nps()
                    P.mm(p_[:], kxT[:, hd, m_ * 128:(m_ + 1) * 128], qxT[:, hd, :])
                    pt = ptm[m_]
                    P.act(pt[:], p_[:], AF.Exp, scale=128 ** -0.5)
                    P.mm(po[:], vx[:, m_, hd * 128:(hd + 1) * 128], pt[:], start=m_ == 0, stop=m_ == 1)
                    P.mm(pd[:], ones_b[:], pt[:], start=m_ == 0, stop=m_ == 1)
                P.recip(rden[:], pd[:])
                P.tt(oxT[:, hd, :], po[:], rden[:], ALU.mult)
            for tt in range(4):
                for half in range(2):
                    p_ = nps()
                    for hd in range(4):
                        P.mm(p_[:], oxT[:, hd, tt * 128:(tt + 1) * 128], Woc[:, hd, half * 512:(half + 1) * 512], start=hd == 0, stop=hd == 3)
                    P.tt(hT[:, tt, half * 512:(half + 1) * 512], hT[:, tt, half * 512:(half + 1) * 512], p_[:], ALU.add)
            rms_to_T(hT, gf[:], xn_tok, xT, junk, ss, rstd)
            if moe:
                for tt in range(4):
                    p_ = nps()
                    for kc in range(8):
                        P.mm(p_[:, 0:8], xT[:, kc, tt * 128:(tt + 1) * 128], Wr[:, kc, :], start=kc == 0, stop=kc == 7)
                    P.tt(lg[:], p_[:, 0:8], rb[:], ALU.add)
                    P.rmax(m1[:], lg[:])
                    P.ts(eq1[:], lg[:], m1[:, 0:1], None, op0=ALU.is_equal)
                    P.stt(lg2[:], eq1[:], -1e30, lg[:], ALU.mult, ALU.add)
                    P.rmax(m2[:], lg2[:])
                    P.ts(eq2[:], lg2[:], m2[:, 0:1], None, op0=ALU.is_equal)
                    P.tt(dd[:], m2[:], m1[:], ALU.subtract)
                    P.act(w2_[:], dd[:], AF.Sigmoid)
                    P.act(w1_[:], dd[:], AF.Sigmoid, scale=-1.0)
                    P.ts(eq1[:], eq1[:], w1_[:, 0:1], None, op0=ALU.mult)
                    P.stt(comb[:, tt, :], eq2[:], w2_[:, 0:1], eq1[:], ALU.mult, ALU.add)
            for e in range(ne):
                ei = (1 + e) if moe else 0
                w13 = W13B[ei]; w2 = W2B[ei]
                for sbi in range(6):
                    nfc = 4 if sbi < 5 else 2
                    bi = slab_i % 2; slab_i += 1
                    P.dma(w1g[bi][:, :, 0:nfc * 128], w13[:, sbi * 512:sbi * 512 + nfc * 128].rearrange("(k p) n -> p k n", p=128), reads=[f"W13B{ei}"])
                    P.dma(w1u[bi][:, :, 0:nfc * 128], w13[:, DFF + sbi * 512:DFF + sbi * 512 + nfc * 128].rearrange("(k p) n -> p k n", p=128), reads=[f"W13B{ei}"])
                    if sbi == 0:
                        for half in range(2):
                            P.dma(w2h[half][:], w2[:, half * 512:(half + 1) * 512].rearrange("(k p) n -> p k n", p=128), reads=[f"W2B{ei}"])
                    for fl in range(nfc):
                        fc = sbi * 4 + fl
                        pg = nps(); pu = nps()
                        for kc in range(8):
                            P.mm(pg[:], w1g[bi][:, kc, fl * 128:(fl + 1) * 128], xT[:, kc, :], start=kc == 0, stop=kc == 7)
                        for kc in range(8):
                            P.mm(pu[:], w1u[bi][:, kc, fl * 128:(fl + 1) * 128], xT[:, kc, :], start=kc == 0, stop=kc == 7)
                        s_ = sg[fc % 2]
                        P.act(s_[:], pg[:], AF.Silu)
                        P.tt(hid[:, fc, :], s_[:], pu[:], ALU.mult)
                if e == ne - 1 and g + 1 < ng:
                    rms_to_T(hTs[(g + 1) % 2], gx[:], xn_tok, xT, junk, ss, rstd)
                for half in range(2):
                    for tt in range(4):
                        p_ = nps()
                        for fc in range(NFC):
                            P.mm(p_[:], hid[:, fc, tt * 128:(tt + 1) * 128], w2h[half][:, fc, :], start=fc == 0, stop=fc == NFC - 1)
                        hv = hT[:, tt, half * 512:(half + 1) * 512]
                        if moe:
                            P.stt(hv, p_[:], comb[:, tt, e:e + 1], hv, ALU.mult, ALU.add)
                        else:
                            P.tt(hv, hv, p_[:], ALU.add)
            if last:
                for tt in range(4):
                    P.act(junk[:], hT[:, tt, :], AF.Square, accum_out=ss[:, tt:tt + 1])
                P.act(rstd[:], ss[:], AF.Sqrt, bias=epsc[:], scale=1.0 / D)
                P.recip(rstd[:], rstd[:])
                for tt in range(4):
                    P.stt(yo[:, tt, :], hT[:, tt, :], rstd[:, tt:tt + 1], gfin[:], ALU.mult, ALU.mult, eng="dve" if tt % 2 == 0 else "pool")
                P.dma(y[gs, :].rearrange("(t p) d -> p t d", p=128), yo[:], writes=[("y", g)])
            else:
                P.dma(H3[gs, :].rearrange("(t p) d -> p t d", p=128), hT[:], writes=[("H3", g)])
        P.barrier()
        P.release(mk)

    def phase_D_moe(l):
        mk = P.mark()
        nkt = 4 * ng
        gx = P.sb("gx", [128, D]); P.dma(gx[:], g_cross[:, l, :])
        gf = P.sb("gf", [128, D]); P.dma(gf[:], g_ffn[:, l, :])
        EQ1 = P.sb("EQ1", [128, 64, 8]); EQ2 = P.sb("EQ2", [128, 64, 8])
        RK1 = P.sb("RK1", [128, 64]); RK2 = P.sb("RK2", [128, 64]); WW1 = P.sb("WW1", [128, 64]); WW2 = P.sb("WW2", [128, 64])
        carryM = P.sb("carryM", [128, 8]); P.memset(carryM[:], 0.0)
        SL1 = P.sb("SL1", [128, 64], I32); SL2 = P.sb("SL2", [128, 64], I32); IDXW = P.sb("IDXW", [128, NBLK], I32)
        pidx = P.sb("pidx", [128, 1]); P.dma(pidx[:], pidx_d)
        junk = P.sb("junk", [128, D]); ss = P.sb("ss", [128, 4]); rstd = P.sb("rstd", [128, 4])
        mk1 = P.mark()
        Wq = P.sb("Wq", [128, 8, 512], BF16)
        P.dma(Wq[:], wq_c[l].rearrange("(k p) n -> p k n", p=128), eng="pool")
        Woc = P.sb("Woc", [128, 4, D], BF16)
        P.dma(Woc[:], wo_c[l].rearrange("(k p) n -> p k n", p=128), eng="pool")
        kxT = P.sb("kxT", [128, 4, 256], BF16); vx = P.sb("vx", [128, 2, 512], BF16)
        hT = P.sb("hT", [128, 4, D]); xn_tok = P.sb("xn_tok", [128, 4, D], BF16); xT = P.sb("xT", [128, 8, T], BF16)
        zt = P.sb("zt", [128, 4, D], BF16); P.memset(zt[:], 0.0, eng="pool")
        for b in range(NBLK):
            P.dma(XS[b * 512:(b + 1) * 512, :].rearrange("(t p) d -> p t d", p=128), zt[:], writes=[("XS", b)])
        mk2 = P.mark()
        gm = P.sb("gm", [128, D]); P.dma(gm[:], g_mem[:, l, :])
        Wkv = P.sb("Wkv", [128, 8, D], BF16)
        for kc in range(0, 8, 4):
            P.dma(Wkv[:, kc:kc + 4, :], wkv_c[l, kc * 128:(kc + 4) * 128, :].rearrange("(k p) n -> p k n", p=128), eng="pool")
        mt_ = P.sb("memt", [128, 2, D]); P.dma(mt_[:], mem.rearrange("(t p) d -> p t d", p=128))
        mn = P.sb("memn", [128, 2, D], BF16); memT = P.sb("memT", [128, 8, 256], BF16)
        for t2 in range(2):
            P.act(junk[:], mt_[:, t2, :], AF.Square, accum_out=ss[:, t2:t2 + 1])
        P.act(rstd[:, 0:2], ss[:, 0:2], AF.Sqrt, bias=epsc[:], scale=1.0 / D)
        P.recip(rstd[:, 0:2], rstd[:, 0:2])
        for t2 in range(2):
            P.stt(mn[:, t2, :], mt_[:, t2, :], rstd[:, t2:t2 + 1], gm[:], ALU.mult, ALU.mult)
            pb = npb()
            for kc in range(8):
                P.tr(pb[:, kc * 128:(kc + 1) * 128], mn[:, t2, kc * 128:(kc + 1) * 128], ident[:])
            P.copy(memT[:, :, t2 * 128:(t2 + 1) * 128], pb[:, :].rearrange("p (a b) -> p a b", a=8))
        for hd in range(4):
            p_ = nps()
            for kc in range(8):
                P.mm(p_[:, 0:256], Wkv[:, kc, hd * 128:(hd + 1) * 128], memT[:, kc, :], start=kc == 0, stop=kc == 7)
            P.copy(kxT[:, hd, :], p_[:, 0:256])
        for t2 in range(2):
            p_ = nps()
            for kc in range(8):
                P.mm(p_[:], memT[:, kc, t2 * 128:(t2 + 1) * 128], Wkv[:, kc, 512:1024], start=kc == 0, stop=kc == 7)
            P.copy(vx[:, t2, :], p_[:])
        P.barrier()
        P.release(mk2)
        qxT = P.sb("qxT", [128, 4, T], BF16); ptm = [P.sb(f"ptm{i}", [128, T], BF16) for i in range(2)]
        rden = P.sb("rden", [128, T]); oxT = P.sb("oxT", [128, 4, T], BF16)
        Wr = P.sb("Wr", [128, 8, 8], BF16)
        P.dma(Wr[:], router_w[0].rearrange("(k p) n -> p k n", p=128), eng="pool")
        rb = P.sb("rb", [128, 8]); P.dma(rb[:], rb_b)
        lg = P.sb("lg", [128, 8]); lg2 = P.sb("lg2", [128, 8]); msk = P.sb("msk", [128, 8]); rk = P.sb("rk", [128, 8]); t8 = P.sb("t8", [128, 8])
        m1 = P.sb("m1", [128, 1]); m2 = P.sb("m2", [128, 1]); dd = P.sb("dd", [128, 1])
        hTs = [hT, P.sb("hT2m", [128, 4, D])]
        for g in range(ng):
            gs = slice(g * T, (g + 1) * T)
            hT = hTs[g % 2]
            if g == 0:
                load_h(hT, H1, "H1", 0)
            if g + 1 < ng:
                load_h(hTs[(g + 1) % 2], H1, "H1", g + 1)
            rms_to_T(hT, gx[:], xn_tok, xT, junk, ss, rstd)
            for hd in range(4):
                p_ = nps()
                for kc in range(8):
                    P.mm(p_[:], Wq[:, kc, hd * 128:(hd + 1) * 128], xT[:, kc, :], start=kc == 0, stop=kc == 7)
                if hd % 2 == 0:
                    P.copy(qxT[:, hd, :], p_[:])
                else:
                    P.act(qxT[:, hd, :], p_[:], AF.Copy)
            for hd in range(4):
                po = nps(); pd = nps()
                for m_ in range(2):
                    p_ = nps()
                    P.mm(p_[:], kxT[:, hd, m_ * 128:(m_ + 1) * 128], qxT[:, hd, :])
                    pt = ptm[m_]
                    P.act(pt[:], p_[:], AF.Exp, scale=128 ** -0.5)
                    P.mm(po[:], vx[:, m_, hd * 128:(hd + 1) * 128], pt[:], start=m_ == 0, stop=m_ == 1)
                    P.mm(pd[:], ones_b[:], pt[:], start=m_ == 0, stop=m_ == 1)
                P.recip(rden[:], pd[:])
                P.tt(oxT[:, hd, :], po[:], rden[:], ALU.mult)
            for tt in range(4):
                for half in range(2):
                    p_ = nps()
                    for hd in range(4):
                        P.mm(p_[:], oxT[:, hd, tt * 128:(tt + 1) * 128], Woc[:, hd, half * 512:(half + 1) * 512], start=hd == 0, stop=hd == 3)
                    P.tt(hT[:, tt, half * 512:(half + 1) * 512], hT[:, tt, half * 512:(half + 1) * 512], p_[:], ALU.add)
            P.dma(H3[gs, :].rearrange("(t p) d -> p t d", p=128), hT[:], writes=[("H3", g)])
            rms_to_T(hT, gf[:], xn_tok, xT, junk, ss, rstd)
            P.dma(XN2[gs, :].rearrange("(t p) d -> p t d", p=128), xn_tok[:], writes=[("XN2", g)])
            for tt in range(4):
                kt = 4 * g + tt
                p_ = nps()
                for kc in range(8):
                    P.mm(p_[:, 0:8], xT[:, kc, tt * 128:(tt + 1) * 128], Wr[:, kc, :], start=kc == 0, stop=kc == 7)
                P.tt(lg[:], p_[:, 0:8], rb[:], ALU.add)
                P.rmax(m1[:], lg[:])
                P.ts(EQ1[:, kt, :], lg[:], m1[:, 0:1], None, op0=ALU.is_equal)
                P.stt(lg2[:], EQ1[:, kt, :], -1e30, lg[:], ALU.mult, ALU.add)
                P.rmax(m2[:], lg2[:])
                P.ts(EQ2[:, kt, :], lg2[:], m2[:, 0:1], None, op0=ALU.is_equal)
                P.tt(dd[:], m2[:], m1[:], ALU.subtract)
                P.act(WW2[:, kt:kt + 1], dd[:], AF.Sigmoid)
                P.act(WW1[:, kt:kt + 1], dd[:], AF.Sigmoid, scale=-1.0)
                P.tt(msk[:], EQ1[:, kt, :], EQ2[:, kt, :], ALU.add)
                p3 = nps()
                P.mm(p3[:, 0:8], tri_f[:], msk[:])
                P.mm(p3[:, 8:16], ones_f[:], msk[:])
                P.tt(rk[:], p3[:, 0:8], carryM[:], ALU.add)
                P.tt(rk[:], rk[:], msk[:], ALU.subtract)
                P.tt(carryM[:], p3[:, 8:16], carryM[:], ALU.add)
                P.tt(t8[:], EQ1[:, kt, :], rk[:], ALU.mult)
                P.rsum(RK1[:, kt:kt + 1], t8[:])
                P.tt(t8[:], EQ2[:, kt, :], rk[:], ALU.mult)
                P.rsum(RK2[:, kt:kt + 1], t8[:])
        P.barrier()
        P.release(mk1)
        nblk = P.sb("nblk", [128, 8]); pad = P.sb("pad", [128, 8]); incl = P.sb("incl", [128, 8]); base = P.sb("base", [128, 8])
        t8 = P.sb("t8b", [128, 8]); eb = P.sb("eb", [128, NBLK]); sf1 = P.sb("sf1", [128, 64]); sf2 = P.sb("sf2", [128, 64])
        P.memset(nblk[:], 0.0)
        for j in range(16):
            P.stt(nblk[:], carryM[:], float(512 * j), nblk[:], ALU.is_gt, ALU.add)
        P.ts(pad[:], nblk[:], 512.0, None, op0=ALU.mult)
        P.scan(incl[:], ones_f[:, 0:8], pad[:], 0.0, ALU.mult, ALU.add)
        P.tt(base[:], incl[:], pad[:], ALU.subtract)
        for b in range(NBLK):
            P.ts(t8[:], incl[:], float(512 * b), None, op0=ALU.is_le)
            P.rsum(eb[:, b:b + 1], t8[:])
        P.ts(eb[:], eb[:], 7.0, None, op0=ALU.min)
        P.ts(eb[:], eb[:], 128.0, pidx[:, 0:1], op0=ALU.mult, op1=ALU.add)
        P.copy(IDXW[:], eb[:])
        P.copy(sf1[:], RK1[:]); P.copy(sf2[:], RK2[:])
        for e in range(8):
            P.stt(sf1[:, 0:nkt], EQ1[:, 0:nkt, e], base[:, e:e + 1], sf1[:, 0:nkt], ALU.mult, ALU.add)
            P.stt(sf2[:, 0:nkt], EQ2[:, 0:nkt, e], base[:, e:e + 1], sf2[:, 0:nkt], ALU.mult, ALU.add)
        P.copy(SL1[:], sf1[:]); P.copy(SL2[:], sf2[:])
        xr = [P.sb(f"xr{i}", [128, D], BF16) for i in range(2)]
        XSB = NBLK * 512 - 1
        for kt in range(nkt):
            x_ = xr[kt % 2]
            P.dma(x_[:], XN2[kt * 128:(kt + 1) * 128, :], reads=[("XN2", kt // 4)])
            for SL in (SL1, SL2):
                P.op("pool", lambda e, x_=x_, SL=SL, kt=kt: e.indirect_dma_start(
                    out=XS, out_offset=bass.IndirectOffsetOnAxis(ap=SL[:, kt:kt + 1], axis=0), in_=x_[:], in_offset=None,
                    bounds_check=None), [x_, SL] + [("XS", b) for b in range(NBLK)], ["XSs"], dmakey=("dma", f"xr{kt % 2}"))
        P.barrier()
        mkb = P.mark()
        xs_toks = [P.sb(f"xs_tok{i}", [128, 4, D], BF16) for i in range(2)]; xTs = [P.sb(f"xTb{i}", [128, 8, T], BF16) for i in range(2)]

        def prep_blk(b):
            xs_tok = xs_toks[b % 2]; xT = xTs[b % 2]
            P.dma(xs_tok[:], XS[b * 512:(b + 1) * 512, :].rearrange("(t p) d -> p t d", p=128), reads=["XSs"])
            for tt in range(4):
                pb = npb()
                for kc in range(8):
                    P.tr(pb[:, kc * 128:(kc + 1) * 128], xs_tok[:, tt, kc * 128:(kc + 1) * 128], ident[:])
                src = pb[:, :].rearrange("p (a b) -> p a b", a=8)
                dst = xT[:, :, tt * 128:(tt + 1) * 128]
                if tt % 2 == 0:
                    P.copy(dst, src)
                else:
                    P.act(dst, src, AF.Copy)
        hid = P.sb("hid", [128, NFC, T], BF16)
        w1g = [P.sb(f"w1g{i}", [128, 8, 512], BF16) for i in range(2)]
        w1u = [P.sb(f"w1u{i}", [128, 8, 512], BF16) for i in range(2)]
        w2h = [P.sb(f"w2h{i}", [128, NFC, 512], BF16) for i in range(2)]
        w1gL = P.sb("w1gL", [128, 8, 256], BF16); w1uL = P.sb("w1uL", [128, 8, 256], BF16)
        sg = [P.sb(f"sg{i}", [128, T]) for i in range(2)]
        yo = P.sb("yo", [128, 4, D])
        slab_i = 0

        def wgather(dst, src, b, key):
            P.op("pool", lambda e: e.indirect_dma_start(
                out=dst, out_offset=None, in_=src, in_offset=bass.IndirectOffsetOnAxis(ap=IDXW[:, b:b + 1], axis=0),
                bounds_check=None), [IDXW, "WP"], [dst], dmakey=("dma", key))
        prep_blk(0)
        for b in range(NBLK):
            xT = xTs[b % 2]
            for sbi in range(6):
                if sbi == 4 and b + 1 < NBLK:
                    prep_blk(b + 1)
                nfc = 4 if sbi < 5 else 2
                bi = slab_i % 2; slab_i += 1
                if sbi < 5:
                    g_t, u_t, kg, ku = w1g[bi], w1u[bi], f"w1g{bi}", f"w1u{bi}"
                else:
                    g_t, u_t, kg, ku = w1gL, w1uL, "w1gL", "w1uL"
                wgather(g_t[:].rearrange("p k n -> p (k n)"), WSL[(sbi, 0)].rearrange("r k n -> r (k n)"), b, kg)
                wgather(u_t[:].rearrange("p k n -> p (k n)"), WSL[(sbi, 1)].rearrange("r k n -> r (k n)"), b, ku)
                if sbi == 0:
                    for half in range(2):
                        wgather(w2h[half][:].rearrange("p k n -> p (k n)"), W2H[half].rearrange("r k n -> r (k n)"), b, f"w2h{half}")
                for fl in range(nfc):
                    fc = sbi * 4 + fl
                    pg = nps(); pu = nps()
                    for kc in range(8):
                        P.mm(pg[:], g_t[:, kc, fl * 128:(fl + 1) * 128], xT[:, kc, :], start=kc == 0, stop=kc == 7)
                    for kc in range(8):
                        P.mm(pu[:], u_t[:, kc, fl * 128:(fl + 1) * 128], xT[:, kc, :], start=kc == 0, stop=kc == 7)
                    s_ = sg[fc % 2]
                    P.act(s_[:], pg[:], AF.Silu)
                    P.tt(hid[:, fc, :], s_[:], pu[:], ALU.mult)
            for half in range(2):
                for tt in range(4):
                    p_ = nps()
                    for fc in range(NFC):
                        P.mm(p_[:], hid[:, fc, tt * 128:(tt + 1) * 128], w2h[half][:, fc, :], start=fc == 0, stop=fc == NFC - 1)
                    if tt % 2 == 0:
                        P.copy(yo[:, tt, half * 512:(half + 1) * 512], p_[:])
                    else:
                        P.act(yo[:, tt, half * 512:(half + 1) * 512], p_[:], AF.Copy)
            P.dma(YS[b * 512:(b + 1) * 512, :].rearrange("(t p) d -> p t d", p=128), yo[:], writes=["YSs"])
        P.barrier()
        P.release(mkb)
        gfin = P.sb("gfin", [128, D]); P.dma(gfin[:], g_final)
        ya = [P.sb(f"ya{i}", [128, D]) for i in range(2)]; yb = [P.sb(f"yb{i}", [128, D]) for i in range(2)]
        h2 = [P.sb(f"h2{i}", [128, D]) for i in range(2)]; yo2 = [P.sb(f"yo2{i}", [128, D]) for i in range(2)]
        ssc = P.sb("ssc", [128, 64]); rsc = P.sb("rsc", [128, 64])
        def fetch_c(kt):
            i2 = kt % 2
            for dst_, SL, nm in ((ya[i2], SL1, "ya"), (yb[i2], SL2, "yb")):
                P.op("pool", lambda e, dst_=dst_, SL=SL, kt=kt: e.indirect_dma_start(
                    out=dst_[:], out_offset=None, in_=YS, in_offset=bass.IndirectOffsetOnAxis(ap=SL[:, kt:kt + 1], axis=0),
                    bounds_check=None), [SL, "YSs"], [dst_], dmakey=("dma", f"{nm}{i2}"))
            P.dma(h2[i2][:], H3[kt * 128:(kt + 1) * 128, :], reads=[("H3", kt // 4)])

        fetch_c(0)
        for kt in range(nkt):
            i2 = kt % 2
            if kt + 1 < nkt:
                fetch_c(kt + 1)
            P.stt(h2[i2][:], ya[i2][:], WW1[:, kt:kt + 1], h2[i2][:], ALU.mult, ALU.add)
            P.stt(h2[i2][:], yb[i2][:], WW2[:, kt:kt + 1], h2[i2][:], ALU.mult, ALU.add)
            P.act(junk[:], h2[i2][:], AF.Square, accum_out=ssc[:, kt:kt + 1])
            P.act(rsc[:, kt:kt + 1], ssc[:, kt:kt + 1], AF.Sqrt, bias=epsc[:], scale=1.0 / D)
            P.recip(rsc[:, kt:kt + 1], rsc[:, kt:kt + 1])
            P.stt(yo2[i2][:], h2[i2][:], rsc[:, kt:kt + 1], gfin[:], ALU.mult, ALU.mult)
            P.dma(y[kt * 128:(kt + 1) * 128, :], yo2[i2][:], writes=[("y", kt)])
        P.barrier()
        P.release(mk)

    phases = []
    prep_ffn(0, dense_w13[0], dense_w2[0])
    np_ = 0
    for l in range(nlayers):
        hsrc, hname = (x, "x") if l == 0 else (H3, "H3")
        for ph in "ABCD":
            if np_ >= nphase:
                break
            np_ += 1
            if ph == "A":
                phase_A(l, hsrc, hname)
            elif ph == "B":
                phase_B(l)
            elif ph == "C":
                phase_C(l, hsrc, hname)
            else:
                if l % 2 == 1:
                    phase_D_moe(l)
                else:
                    phase_D(l, moe=False, last=(l == nlayers - 1))
    P.emit()
    P.close()
    return nc, P


def host_inputs(inputs, b):
    f = lambda a: np.ascontiguousarray(np.asarray(a, dtype=np.float32))
    rep = lambda a: f(np.broadcast_to(np.asarray(a)[None], (128,) + np.asarray(a).shape))
    m = {}
    m["x"] = f(inputs["x"][b]); m["mem"] = f(inputs["mem"][b])
    for k in ("w_in", "sgu_w", "rg_wa", "rg_wx", "w_branch", "w_gate", "w_out", "wq_c", "wkv_c", "wo_c",
              "dense_w13", "dense_w2", "router_w", "moe_w13", "moe_w2"):
        m[k] = f(inputs[k])
    m["g_mix"] = rep(inputs["norm_mix"]); m["g_cross"] = rep(inputs["norm_cross"]); m["g_ffn"] = rep(inputs["norm_ffn"])
    m["g_mem"] = rep(inputs["norm_mem"]); m["g_final"] = rep(inputs["norm_final"])
    m["sgu_g_b"] = rep(inputs["sgu_g"]); m["sgu_b_b"] = rep(np.asarray(inputs["sgu_b"]).reshape(L, 512))
    m["sinks_b"] = rep(inputs["swa_sinks"]); m["bf_b"] = rep(inputs["fox_bf"]); m["rb_b"] = rep(np.asarray(inputs["router_b"])[0])
    pp = lambda a: f(np.asarray(a).reshape(L, 4, 128).transpose(2, 0, 1))
    m["conv_w_p"] = f(np.asarray(inputs["conv_w"]).reshape(L, 4, 4, 128).transpose(3, 0, 1, 2))
    m["conv_b_p"] = pp(inputs["conv_b"]); m["ba_p"] = pp(inputs["rg_ba"]); m["bx_p"] = pp(inputs["rg_bx"]); m["lam_p"] = pp(inputs["rg_lambda"])
    m["bgate_p"] = f(np.asarray(inputs["b_gate"]).reshape(L, 4, 8, 128).transpose(3, 0, 1, 2))
    m["ident"] = np.eye(128, dtype=np.float32)
    m["pidx"] = np.arange(128, dtype=np.float32).reshape(128, 1)
    k = np.arange(128)[:, None]; q = np.arange(128)[None, :]
    m["tri"] = (k <= q).astype(np.float32)
    slopes = 2.0 ** (-(np.arange(1, 9, dtype=np.float64)))
    me = np.zeros((128, 8, 2, 128), np.float64)
    for h in range(8):
        me[:, h, 0, :] = np.where(k <= q, np.exp(-slopes[h] * (q - k)), 0.0)
        me[:, h, 1, :] = np.where(k > q, np.exp(-slopes[h] * (q + 128 - k)), 0.0)
    m["mexp"] = me.astype(np.float32)
    return m


_CACHE = {}


def kernel(**inputs):
    if "nc" not in _CACHE:
        _CACHE["nc"] = build()[0]
    nc = _CACHE["nc"]
    in_maps = [host_inputs(inputs, b) for b in range(8)]
    res = run_bass_kernel_spmd(nc, in_maps, core_ids=list(range(8)))
    return np.stack([np.asarray(r["y"], dtype=np.float32) for r in res.results], axis=0)
```
